# Optimizing a Trainium2 kernel written in Bass

```python
import math
import jax, jax.numpy as jnp
from jax import lax
import numpy as np

D_MODEL = 1024
BATCH = 16
SEQ = 2048
DEPTH = 1

CHUNK = 64
Q_BLOCK = 128
A_QK_DIM = 64
A_V_DIM = 2 * A_QK_DIM
A_HEADS = D_MODEL // 256
A_WIDTH = A_HEADS * A_V_DIM
B_HEAD_DIM = 64
B_HEADS = D_MODEL // 128
B_WIDTH = B_HEADS * B_HEAD_DIM
B_KV_RANK = D_MODEL // 8
IDX_HEADS = 8
IDX_DIM = 64
TOPK_MAX = 256
N_ALIBI_HEADS = A_HEADS + B_HEADS
IN_SPLITS = (
    A_HEADS * 2 * A_QK_DIM,
    A_HEADS * 2 * A_QK_DIM,
    A_WIDTH,
    B_WIDTH,
    B_KV_RANK,
    IDX_HEADS * IDX_DIM,
    IDX_DIM,
    IDX_HEADS,
    2 * D_MODEL,
)
IN_COLS = sum(IN_SPLITS)
N_GROUPS = 4
EXPERTS_PER_GROUP = 4
N_EXPERTS = N_GROUPS * EXPERTS_PER_GROUP
TOP_K_IN_GROUP = 2
D_FF_EXPERT = D_MODEL // 4
LN_EPS = 1e-5
RMS_EPS = 1e-5
DEEPNORM_ALPHA = (2.0 * DEPTH) ** 0.25
DEEPNORM_BETA = (8.0 * DEPTH) ** -0.25

kernel_name = "hybrid_diffattn_dsa_hmoe_block"


def _layer_norm(x, g, b):
    xf = x.astype(jnp.float32)
    mu = jnp.mean(xf, axis=-1, keepdims=True)
    var = jnp.mean(jnp.square(xf - mu), axis=-1, keepdims=True)
    return ((xf - mu) * lax.rsqrt(var + LN_EPS) * g.astype(jnp.float32) + b.astype(jnp.float32)).astype(x.dtype)


def _rms_norm(x, g):
    xf = x.astype(jnp.float32)
    y = xf * lax.rsqrt(jnp.mean(xf * xf, axis=-1, keepdims=True) + RMS_EPS)
    return (y * g.astype(jnp.float32)).astype(x.dtype)


def _alibi_slopes():
    n = N_ALIBI_HEADS
    slopes = (2.0 ** (-8.0 * np.arange(1, n + 1) / n)).astype(np.float32)
    a_idx = np.arange(A_HEADS) * (n // A_HEADS)
    b_idx = np.setdiff1d(np.arange(n), a_idx)
    return jnp.asarray(slopes[a_idx]), jnp.asarray(slopes[b_idx])


def _to_blocks(t):
    b, s = t.shape[:2]
    t = t.reshape((b, s // Q_BLOCK, Q_BLOCK) + t.shape[2:])
    return jnp.moveaxis(t, 1, 0)


def _from_blocks(t):
    t = jnp.moveaxis(t, 0, 1)
    return t.reshape((t.shape[0], t.shape[1] * t.shape[2]) + t.shape[3:])


def _diff_attention(q, k, v, slopes, lam, lam_init, subln_g):
    b, s = q.shape[:2]
    n_blk = s // Q_BLOCK
    scale = A_QK_DIM ** -0.5
    k_pos = jnp.arange(s)

    def block(args):
        q_blk, blk = args
        q_pos = blk * Q_BLOCK + jnp.arange(Q_BLOCK)
        logits = jnp.einsum('bqhmd,bshmd->bhmqs', q_blk, k).astype(jnp.float32) * scale
        dist = jnp.abs(q_pos[:, None] - k_pos[None, :]).astype(jnp.float32)
        bias = -slopes[:, None, None] * dist
        visible = (k_pos // CHUNK)[None, :] <= (q_pos // CHUNK)[:, None]
        logits = jnp.where(visible, logits + bias[None, :, None], -jnp.inf)
        p = jax.nn.softmax(logits, axis=-1)
        attn = p[:, :, 0] - lam * p[:, :, 1]
        return jnp.einsum('bhqs,bshe->bqhe', attn.astype(v.dtype), v)

    o = _from_blocks(lax.map(block, (_to_blocks(q), jnp.arange(n_blk))))
    o = _rms_norm(o, subln_g) * (1.0 - lam_init)
    return o.reshape(b, s, A_WIDTH)


def _sparse_indexed_attention(q_abs, c_kv, idx_q, idx_k, idx_w, slopes):
    b, s = q_abs.shape[:2]
    n_blk = s // Q_BLOCK
    top_k = min(TOPK_MAX, s // 4)
    scale = B_HEAD_DIM ** -0.5
    k_pos = jnp.arange(s)

    def block(args):
        qa, iq, iw, blk = args
        q_pos = blk * Q_BLOCK + jnp.arange(Q_BLOCK)
        idx_logits = jnp.einsum('bqhd,bsd->bqhs', iq, idx_k).astype(jnp.float32)
        score = jnp.einsum('bqh,bqhs->bqs', iw.astype(jnp.float32), jax.nn.relu(idx_logits))
        visible = (k_pos // CHUNK)[None, :] <= (q_pos // CHUNK)[:, None]
        score = jnp.where(visible[None], score, -jnp.inf)
        _, sel = lax.top_k(score, top_k)
        kv_sel = jax.vmap(lambda kv, ix: kv[ix])(c_kv, sel)
        logits = jnp.einsum('bqhr,bqkr->bhqk', qa, kv_sel).astype(jnp.float32) * scale
        dist = jnp.abs(q_pos[None, :, None] - sel).astype(jnp.float32)
        sel_ok = (sel // CHUNK) <= (q_pos // CHUNK)[None, :, None]
        logits = jnp.where(sel_ok[:, None], logits - slopes[None, :, None, None] * dist[:, None], -jnp.inf)
        p = jax.nn.softmax(logits, axis=-1)
        return jnp.einsum('bhqk,bqkr->bqhr', p.astype(kv_sel.dtype), kv_sel)

    args = (_to_blocks(q_abs), _to_blocks(idx_q), _to_blocks(idx_w), jnp.arange(n_blk))
    return _from_blocks(lax.map(block, args))


def _hier_moe(u, w_group, b_group, w_expert_router, b_expert_router, w_gate, w_up, w_down):
    g_logits = (jnp.einsum('bsd,dg->bsg', u, w_group) + b_group).astype(jnp.float32)
    g_prob = jax.nn.softmax(g_logits, axis=-1)
    g_top_p, g_top_i = lax.top_k(g_prob, 1)
    e_logits = (jnp.einsum('bsd,de->bse', u, w_expert_router) + b_expert_router).astype(jnp.float32)
    e_logits = e_logits.reshape(u.shape[0], u.shape[1], N_GROUPS, EXPERTS_PER_GROUP)
    e_logits = jnp.einsum('bsge,bsg->bse', e_logits, jax.nn.one_hot(g_top_i[..., 0], N_GROUPS, dtype=jnp.float32))
    e_prob = jax.nn.softmax(e_logits, axis=-1)
    e_top_p, e_top_i = lax.top_k(e_prob, TOP_K_IN_GROUP)
    e_top_p = e_top_p / jnp.sum(e_top_p, axis=-1, keepdims=True)
    expert_id = g_top_i * EXPERTS_PER_GROUP + e_top_i
    weight = g_top_p * e_top_p
    comb = jnp.einsum('bsk,bske->bse', weight, jax.nn.one_hot(expert_id, N_EXPERTS, dtype=jnp.float32))
    comb = comb.astype(u.dtype)
    y = jnp.zeros_like(u)
    for e in range(N_EXPERTS):
        h = jax.nn.silu(u @ w_gate[e]) * (u @ w_up[e])
        y = y + comb[..., e:e + 1] * (h @ w_down[e])
    return y


def setup_inputs(seed: int = 0) -> dict:
    key = jax.random.key(seed)
    ks = jax.random.split(key, 27)
    f32 = jnp.float32
    L, D = DEPTH, D_MODEL

    def nrm(k, shape, scale):
        return jax.random.normal(k, shape, f32) * scale

    return {
        "x": nrm(ks[0], (BATCH, SEQ, D), 1.0),
        "c": nrm(ks[1], (BATCH, D), 1.0),
        "ada_w": nrm(ks[2], (L, D, 6 * D), 0.5 * D ** -0.5),
        "ada_b": nrm(ks[3], (L, 6 * D), 0.02),
        "w_in": nrm(ks[4], (L, D, IN_COLS), D ** -0.5),
        "lambda_q1": nrm(ks[5], (L, A_QK_DIM), 0.1),
        "lambda_k1": nrm(ks[6], (L, A_QK_DIM), 0.1),
        "lambda_q2": nrm(ks[7], (L, A_QK_DIM), 0.1),
        "lambda_k2": nrm(ks[8], (L, A_QK_DIM), 0.1),
        "a_subln_g": 1.0 + nrm(ks[9], (L, A_V_DIM), 0.02),
        "kv_norm_g": 1.0 + nrm(ks[10], (L, B_KV_RANK), 0.02),
        "w_uk": nrm(ks[11], (L, B_KV_RANK, B_HEADS, B_HEAD_DIM), B_KV_RANK ** -0.5),
        "w_uv": nrm(ks[12], (L, B_KV_RANK, B_HEADS, B_HEAD_DIM), B_KV_RANK ** -0.5),
        "w_a_proj": nrm(ks[13], (L, A_WIDTH, D), A_WIDTH ** -0.5),
        "w_b_proj": nrm(ks[14], (L, B_WIDTH, D), B_WIDTH ** -0.5),
        "w_o": nrm(ks[15], (L, D, D), DEEPNORM_BETA * D ** -0.5),
        "ln1_g": 1.0 + nrm(ks[16], (L, D), 0.02),
        "ln1_b": nrm(ks[17], (L, D), 0.02),
        "w_group": nrm(ks[18], (L, D, N_GROUPS), D ** -0.5),
        "b_group": nrm(ks[19], (L, N_GROUPS), 0.01),
        "w_expert_router": nrm(ks[20], (L, D, N_EXPERTS), D ** -0.5),
        "b_expert_router": nrm(ks[21], (L, N_EXPERTS), 0.01),
        "w_exp_gate": nrm(ks[22], (L, N_EXPERTS, D, D_FF_EXPERT), D ** -0.5),
        "w_exp_up": nrm(ks[23], (L, N_EXPERTS, D, D_FF_EXPERT), D ** -0.5),
        "w_exp_down": nrm(ks[24], (L, N_EXPERTS, D_FF_EXPERT, D), DEEPNORM_BETA * D_FF_EXPERT ** -0.5),
        "ln2_g": 1.0 + nrm(ks[25], (L, D), 0.02),
        "ln2_b": nrm(ks[26], (L, D), 0.02),
    }


def reference(x, c, ada_w, ada_b, w_in, lambda_q1, lambda_k1, lambda_q2, lambda_k2, a_subln_g,
              kv_norm_g, w_uk, w_uv, w_a_proj, w_b_proj, w_o, ln1_g, ln1_b, w_group, b_group,
              w_expert_router, b_expert_router, w_exp_gate, w_exp_up, w_exp_down, ln2_g, ln2_b):
    b, s, d = x.shape
    slopes_a, slopes_b = _alibi_slopes()
    cond = jax.nn.silu(c)
    offsets = [int(o) for o in np.cumsum(IN_SPLITS)[:-1]]
    for l in range(DEPTH):
        mod = cond @ ada_w[l] + ada_b[l]
        shift1, scale1, gate1, shift2, scale2, gate2 = [m[:, None, :] for m in jnp.split(mod, 6, axis=-1)]

        u = x * (1.0 + scale1) + shift1
        proj = u @ w_in[l]
        a_q, a_k, a_v, b_q, b_kv, i_q, i_k, i_w, gates = jnp.split(proj, offsets, axis=-1)

        lam_init = 0.8 - 0.6 * math.exp(-0.3 * l)
        lam = (jnp.exp(jnp.sum(lambda_q1[l].astype(jnp.float32) * lambda_k1[l].astype(jnp.float32)))
               - jnp.exp(jnp.sum(lambda_q2[l].astype(jnp.float32) * lambda_k2[l].astype(jnp.float32)))
               + lam_init)
        y_a = _diff_attention(a_q.reshape(b, s, A_HEADS, 2, A_QK_DIM),
                              a_k.reshape(b, s, A_HEADS, 2, A_QK_DIM),
                              a_v.reshape(b, s, A_HEADS, A_V_DIM),
                              slopes_a, lam, lam_init, a_subln_g[l])

        c_kv = _rms_norm(b_kv, kv_norm_g[l])
        q_abs = jnp.einsum('bshe,rhe->bshr', b_q.reshape(b, s, B_HEADS, B_HEAD_DIM), w_uk[l])
        o_lat = _sparse_indexed_attention(q_abs, c_kv, i_q.reshape(b, s, IDX_HEADS, IDX_DIM),
                                          i_k, i_w, slopes_b)
        y_b = jnp.einsum('bshr,rhe->bshe', o_lat, w_uv[l]).reshape(b, s, B_WIDTH)

        g = jax.nn.sigmoid(gates).reshape(b, s, 2, d)
        mixed = g[:, :, 0] * (y_a @ w_a_proj[l]) + g[:, :, 1] * (y_b @ w_b_proj[l])
        x = _layer_norm(DEEPNORM_ALPHA * x + gate1 * (mixed @ w_o[l]), ln1_g[l], ln1_b[l])

        u2 = x * (1.0 + scale2) + shift2
        y = _hier_moe(u2, w_group[l], b_group[l], w_expert_router[l], b_expert_router[l],
                      w_exp_gate[l], w_exp_up[l], w_exp_down[l])
        x = _layer_norm(DEEPNORM_ALPHA * x + gate2 * y, ln2_g[l], ln2_b[l])
    return x
```

```python
import math
import types
import numpy as np
import ml_dtypes
import concourse.bass as bass
import concourse.mybir as mybir
from concourse.bass_utils import run_bass_kernel_spmd

F32 = mybir.dt.float32
BF16 = mybir.dt.bfloat16
ALU = mybir.AluOpType
AF = mybir.ActivationFunctionType
AX = mybir.AxisListType

SEM_WRAP = 30000
N_DMA_SLOTS = 16
CELL = 32
ENGS = ("pe", "act", "dve", "pool", "sp")
EIDX = {e: i for i, e in enumerate(ENGS)}


def _esize(dt):
    return {F32: 4, BF16: 2}.get(dt, 4)


def _freeze(fn):
    if not getattr(fn, "__closure__", None):
        return fn
    cells = []
    for c in fn.__closure__:
        try:
            cells.append(types.CellType(c.cell_contents))
        except ValueError:
            cells.append(c)
    g = types.FunctionType(fn.__code__, fn.__globals__, fn.__name__, fn.__defaults__, tuple(cells))
    g.__kwdefaults__ = fn.__kwdefaults__
    return g


class Prog:
    def __init__(self, nc):
        self.nc = nc
        self.ops = []
        self.reg = {}
        self.sb_top = 16512
        self.sb_limit = 229344
        self.spaces = {}
        self.seq = {e: 0 for e in ENGS}
        self.dma_reads = []
        self._space("sb", nc.SBUF_PARTITION_SIZE_BYTES)

    def _space(self, name, nbytes):
        n = (nbytes + CELL - 1) // CELL
        self.spaces[name] = dict(
            lw=np.full((len(ENGS), n), -1, np.int64),
            lr=np.full((len(ENGS), n), -1, np.int64),
            dw=np.full(n, -1, np.int64),
        )

    def sb(self, name, shape, dtype, offset=None):
        es = _esize(dtype)
        nbytes = int(np.prod(shape[1:])) * es
        if offset is None:
            offset = (self.sb_top + 63) // 64 * 64
            self.sb_top = offset + nbytes
        assert offset + nbytes <= self.sb_limit, (name, offset, nbytes)
        h = self.nc.alloc_sbuf_tensor_at(name, list(shape), dtype, offset=offset)
        self.reg[h.name] = ("sb", offset, es, int(np.prod(shape[1:])))
        return h

    def ps(self, name, shape, dtype=F32):
        es = _esize(dtype)
        h = self.nc.alloc_psum_tensor(name, list(shape), dtype)
        sp = "ps:" + h.name
        self._space(sp, int(np.prod(shape[1:])) * es)
        self.reg[h.name] = (sp, 0, es, int(np.prod(shape[1:])))
        return h

    def _ranges(self, ap):
        r = self.reg.get(ap.name)
        if r is None:
            return None
        space, base, es, row = r
        if space.startswith("ps:"):
            return space, [(0, (row * es + CELL - 1) // CELL)]
        off = int(ap.offset) % row
        dims = [(int(s), int(c)) for (s, c) in ap.ap[1:]]
        runs = []
        if len(dims) == 0:
            runs = [(off, off + 1)]
        else:
            inner = dims[-1]
            ext = abs(inner[0]) * (inner[1] - 1) + 1
            outer = dims[:-1]
            n_outer = int(np.prod([c for _, c in outer])) if outer else 1
            if n_outer > 64:
                lo = off
                hi = off + 1
                for s, c in dims:
                    if s >= 0:
                        hi += s * (c - 1)
                    else:
                        lo += s * (c - 1)
                runs = [(lo, hi)]
            else:
                offs = [off]
                for s, c in outer:
                    offs = [o + i * s for o in offs for i in range(c)]
                for o in offs:
                    if inner[0] >= 0:
                        runs.append((o, o + ext))
                    else:
                        runs.append((o - ext + 1, o + 1))
        out = []
        for lo, hi in runs:
            out.append(((base + lo * es) // CELL, (base + hi * es + CELL - 1) // CELL))
        out.sort()
        merged = []
        for lo, hi in out:
            if merged and lo <= merged[-1][1]:
                merged[-1] = (merged[-1][0], max(merged[-1][1], hi))
            else:
                merged.append((lo, hi))
        return space, merged

    def op(self, eng, fns, ins=(), outs=(), dma=False):
        if callable(fns):
            fns = [fns]
        fns = [_freeze(f) for f in fns]
        oid = len(self.ops)
        ei = EIDX[eng]
        waits = {}
        dwaits = set()
        rd = [self._ranges(a) for a in ins]
        wr = [self._ranges(a) for a in outs]

        def need(e_i, s):
            if s >= 0 and waits.get(e_i, -1) < s:
                waits[e_i] = s

        for fp in rd:
            if fp is None:
                continue
            st = self.spaces[fp[0]]
            for lo, hi in fp[1]:
                m = st["lw"][:, lo:hi].max(axis=1)
                for e_i in range(len(ENGS)):
                    need(e_i, int(m[e_i]))
                d = st["dw"][lo:hi]
                if (d >= 0).any():
                    dwaits.update(int(v) for v in np.unique(d[d >= 0]))
        for fp in wr:
            if fp is None:
                continue
            st = self.spaces[fp[0]]
            for lo, hi in fp[1]:
                m = np.maximum(st["lw"][:, lo:hi].max(axis=1), st["lr"][:, lo:hi].max(axis=1))
                for e_i in range(len(ENGS)):
                    need(e_i, int(m[e_i]))
                d = st["dw"][lo:hi]
                if (d >= 0).any():
                    dwaits.update(int(v) for v in np.unique(d[d >= 0]))
                if self.dma_reads:
                    keep = []
                    for (did, dsp, dlo, dhi) in self.dma_reads:
                        if dsp == fp[0] and dlo < hi and lo < dhi:
                            dwaits.add(did)
                        else:
                            keep.append((did, dsp, dlo, dhi))
                    self.dma_reads = keep
        if dma:
            for fp in rd:
                if fp is None:
                    continue
                for lo, hi in fp[1]:
                    self.dma_reads.append((oid, fp[0], lo, hi))
            for fp in wr:
                if fp is None:
                    continue
                st = self.spaces[fp[0]]
                for lo, hi in fp[1]:
                    st["dw"][lo:hi] = oid
                    st["lw"][:, lo:hi] = -1
                    st["lr"][:, lo:hi] = -1
            seq = None
        else:
            seq = self.seq[eng]
            self.seq[eng] += 1
            for fp in rd:
                if fp is None:
                    continue
                st = self.spaces[fp[0]]
                for lo, hi in fp[1]:
                    st["lr"][ei, lo:hi] = seq
            for fp in wr:
                if fp is None:
                    continue
                st = self.spaces[fp[0]]
                for lo, hi in fp[1]:
                    st["lw"][:, lo:hi] = -1
                    st["lr"][:, lo:hi] = -1
                    st["lw"][ei, lo:hi] = seq
                    st["dw"][lo:hi] = -1
        self.ops.append(dict(eng=eng, fns=fns, waits=waits, dwaits=dwaits, dma=dma, id=oid, seq=seq))
        return oid

    def emit(self, final_wait_ops=()):
        nc = self.nc
        ops = self.ops
        by_eng = {e: [o for o in ops if o["eng"] == e] for e in ENGS}
        dma_cnt = {e: 0 for e in ENGS}
        last_slot = {}
        for o in ops:
            if o["dma"]:
                k = dma_cnt[o["eng"]]
                dma_cnt[o["eng"]] += 1
                o["slot"] = k % N_DMA_SLOTS
                o["sval"] = 16 * (k // N_DMA_SLOTS + 1)
                key = (o["eng"], o["slot"])
                if key in last_slot:
                    o["dwaits"].add(last_slot[key])
                last_slot[key] = o["id"]
        signal = {e: set() for e in ENGS}
        for e in ENGS:
            waited = {}
            dwaited = {}
            for o in by_eng[e]:
                w2 = {}
                for e_i, s in o["waits"].items():
                    if waited.get(e_i, -1) >= s:
                        continue
                    w2[e_i] = s
                    waited[e_i] = s
                    signal[ENGS[e_i]].add(s)
                o["waits2"] = w2
                d2 = []
                for d in sorted(o["dwaits"]):
                    od = ops[d]
                    key = (od["eng"], od["slot"])
                    if dwaited.get(key, 0) >= od["sval"]:
                        continue
                    dwaited[key] = od["sval"]
                    d2.append(d)
                o["dwaits2"] = d2
        sig_sorted = {e: sorted(signal[e]) for e in ENGS}
        sig_rank = {e: {s: i for i, s in enumerate(sig_sorted[e])} for e in ENGS}
        sems = {}
        for e in ENGS:
            nsem = len(sig_sorted[e]) // SEM_WRAP + 1
            sems[e] = [nc.alloc_semaphore(f"s_{e}_{i}") for i in range(nsem)]
        dsems = {}
        for e in ENGS:
            if dma_cnt[e]:
                dsems[e] = [nc.alloc_semaphore(f"d_{e}_{i}") for i in range(min(N_DMA_SLOTS, dma_cnt[e]))]

        def csig(e, s):
            r = sig_rank[e][s]
            return sems[e][r // SEM_WRAP], r % SEM_WRAP + 1

        self.n_waits = 0
        self.n_sig = sum(len(v) for v in sig_sorted.values())
        self.n_ins = 0
        finals = [(dsems[ops[d]["eng"]][ops[d]["slot"]], ops[d]["sval"]) for d in final_wait_ops]
        with nc.Block() as block:
            hw = {"pe": block.tensor, "act": block.scalar, "dve": block.vector,
                  "pool": block.gpsimd, "sp": block.sync}

            def make(e):
                def body(eng):
                    for o in by_eng[e]:
                        for e_i, s in o["waits2"].items():
                            sem, val = csig(ENGS[e_i], s)
                            eng.wait_ge(sem, val)
                            self.n_waits += 1
                        for d in o["dwaits2"]:
                            od = ops[d]
                            eng.wait_ge(dsems[od["eng"]][od["slot"]], od["sval"])
                            self.n_waits += 1
                        ins = None
                        for fn in o["fns"]:
                            ins = fn(eng)
                            self.n_ins += 1
                        if o["dma"]:
                            ins.then_inc(dsems[e][o["slot"]], 16)
                        elif o["seq"] in sig_rank[e]:
                            sem, val = csig(e, o["seq"])
                            ins.then_inc(sem, 1)
                    if e == "sp":
                        for sem, val in finals:
                            eng.wait_ge(sem, val)
                return body

            for e in ENGS:
                if by_eng[e] or (e == "sp" and finals):
                    hw[e](make(e))


D = 1024
NIT = 20
LN_EPS = 1e-5
RMS_EPS = 1e-5
ALPHA = 2.0 ** 0.25
LAM_INIT = 0.8 - 0.6 * math.exp(0.0)
NEG = -30000.0
C_AQ, C_AK, C_AV, C_BQ, C_BKV, C_IQ, C_IK, C_IW, C_G = 0, 512, 1024, 1536, 2048, 2176, 2688, 2752, 2760


def _slopes():
    n = 12
    s = (2.0 ** (-8.0 * np.arange(1, n + 1) / n)).astype(np.float32).astype(np.float64)
    a_idx = np.arange(4) * 3
    b_idx = np.setdiff1d(np.arange(n), a_idx)
    return np.concatenate([s[a_idx], s[b_idx]])


def _bf16_split3(v):
    hi = v.astype(ml_dtypes.bfloat16).astype(np.float64)
    r = v - hi
    mid = r.astype(ml_dtypes.bfloat16).astype(np.float64)
    lo = (r - mid).astype(ml_dtypes.bfloat16).astype(np.float64)
    return np.stack([hi, mid, lo], 0)


def host_constants():
    sl = _slopes()
    qrel = np.arange(512, dtype=np.float64)
    qaug = np.stack([_bf16_split3(-8.0 * sl[h] * qrel) for h in range(12)], 1)
    p = np.arange(128, dtype=np.float64)
    dp = np.arange(-3, 13, dtype=np.float64)
    kbias = sl[None, :, None] * (p[:, None, None] - 128.0 * dp[None, None, :])
    qq = np.arange(128)
    vis = (p[:, None].astype(int) // 64) <= (qq[None, :] // 64)
    dist = np.abs(qq[None, :] - p[:, None])
    dbias = np.where(vis[:, None, :], -sl[None, :, None] * dist[:, None, :], NEG)
    idxmask = np.where((p[None, :].astype(int) // 64) <= (qq[:, None] // 64), 0.0, -1e30)
    pow2 = np.tile((2.0 ** -(np.arange(NIT + 1) + 1.0))[None, :], (128, 1))
    return dict(
        ident=np.eye(128, dtype=np.float32),
        qaug=qaug.astype(np.float32),
        kbias=kbias.astype(np.float32),
        dbias=dbias.astype(np.float32),
        idxmask=idxmask.astype(np.float32),
        pow2=pow2.astype(np.float32),
    )


def build(NB=2, S=2048, dbg=(), stop=None):
    nc = bass.Bass("TRN2", target_bir_lowering=False)
    P = Prog(nc)
    NT, NG, NBLK = S // 128, S // 512, S // 1024
    TOPK = min(256, S // 4)
    dumps = {}

    def din(name, shape):
        return nc.dram_tensor(name, list(shape), F32, kind="ExternalInput").ap()

    x_d = din("x", [NB * S, D])
    cT_d = din("cT", [128, 8, NB])
    adaw_d = din("ada_w", [D, 6 * D])
    adabT_d = din("ada_bT", [128, 48])
    win_d = din("w_in", [D, 4808])
    wik2_d = din("w_ik2", [D, 128])
    lamA_d = din("lamA", [128, 2, 64])
    lamB_d = din("lamB", [128, 2, 64])
    asub_d = din("asub", [128, 1])
    kvg_d = din("kvg_bc", [128, 128])
    wukT_d = din("w_ukTz", [128, 8, 128])
    wuv_d = din("w_uvz", [128, 8, 128])
    wap_d = din("w_a_proj", [512, D])
    wbp_d = din("w_b_proj", [512, D])
    wo_d = din("w_o", [D, D])
    lnbc_d = din("ln_bc", [128, 4, D])
    wrt_d = din("w_router", [D, 20])
    brt_d = din("b_router_bc", [128, 20])
    weg_d = din("w_exp_gate", [16, D, 256])
    weu_d = din("w_exp_up", [16, D, 256])
    wed_d = din("w_exp_down", [16, 256, D])
    ident_d = din("ident", [128, 128])
    qaug_d = din("qaug", [3, 12, 512])
    kbias_d = din("kbias", [128, 12, 16])
    dbias_d = din("dbias", [128, 12, 128])
    idxmask_d = din("idxmask", [128, 128])
    pow2_d = din("pow2", [128, NIT + 1])
    y_d = nc.dram_tensor("y", [NB * S, D], F32, kind="ExternalOutput").ap()

    identf = P.sb("identf", [128, 128], F32)
    identb = P.sb("identb", [128, 128], BF16)
    onesb = P.sb("onesb", [128, 128], BF16)
    onesf = P.sb("onesf", [128, 128], F32)
    qaug = P.sb("qaug_sb", [128, 12, 512], BF16)
    augl = P.sb("augl", [128, 128], BF16)
    kbias = P.sb("kbias_sb", [128, 12, 16], F32)
    dbias = P.sb("dbias_sb", [128, 12, 128], F32)
    idxmask = P.sb("idxmask_sb", [128, 128], F32)
    pow2 = P.sb("pow2_sb", [128, NIT + 1], F32)
    modT = P.sb("modT", [128, 48, NB], F32)
    lamneg = P.sb("lamneg", [128, 1], F32)
    gsc = P.sb("gsc", [128, 1], F32)
    kvg = P.sb("kvg", [128, 128], F32)
    brt = P.sb("brt", [128, 20], F32)
    wukT = P.sb("wukT", [128, 8, 128], BF16)
    wuv = P.sb("wuv", [128, 8, 128], BF16)
    wrt = P.sb("wrt", [128, 8, 20], F32)
    small = P.sb("small", [128, 64], F32)
    epsc = P.sb("epsc", [128, 2], F32)
    R_K = (P.sb_top + 63) // 64 * 64
    aKT = P.sb("aKT", [128, 4, S], BF16)
    aV = P.sb("aV", [128, NT, 512], BF16)
    cKV = P.sb("cKV", [128, NT, 128], BF16)
    cKVT = P.sb("cKVT", [128, S], BF16)
    iKT = P.sb("iKT", [128, S], BF16)
    P.sb_top = max(P.sb_top, R_K + 8 * D * 4 + 2 * D * 4 + 128)
    yAT = P.sb("yAT", [128, 4, S], BF16)
    yBT = P.sb("yBT", [128, 4, S], BF16)
    R_W = (P.sb_top + 63) // 64 * 64

    def region(start):
        P.sb_top = start

    region(R_W)
    cT_sb = P.sb("cT_sb", [128, 8, NB], F32)
    condT = P.sb("condT", [128, 8, NB], BF16)
    adabT = P.sb("adabT", [128, 48], F32)
    lamA = P.sb("lamA_sb", [128, 2, 64], F32)
    lamB = P.sb("lamB_sb", [128, 2, 64], F32)
    asub = P.sb("asub_sb", [128, 1], F32)
    adaw = [P.sb(f"adaw{i}", [128, 8, 1536], BF16) for i in range(2)]
    region(R_W)
    xin = P.sb("xin", [128, 4, D], F32)
    uT = P.sb("uT", [128, 8, 512], BF16)
    R_12 = P.sb_top
    wK = P.sb("wK", [128, 8, 1280], BF16)
    sq1 = P.sb("sq1", [128, 512], F32)
    t1 = P.sb("t1", [128, 4, 128], F32)
    region(R_12)
    xin2, uT2 = xin, uT
    xin_off = P.reg[xin.name][1]
    score = P.sb("score", [128, S], F32, offset=xin_off)
    score1 = P.sb("score1", [128, S], F32, offset=P.reg[uT.name][1])
    assert S * 4 <= 8 * 512 * 2
    junk = P.sb("junk", [128, S], BF16, offset=xin_off + S * 4)
    maskQ = P.sb("maskQ", [128, S], BF16, offset=xin_off + S * 6)
    assert S * 8 <= 4 * D * 4
    wQ = P.sb("wQ", [128, 8, 1544], BF16)
    aQT = P.sb("aQT", [128, 4, 2, 512], BF16)
    ov1 = P.sb_top
    bQT = P.sb("bQT", [128, 4, 512], BF16)
    o01 = [P.sb(f"o01_{i}", [128, 512], F32, offset=(ov1 + 63) // 64 * 64 + i * 2048) for i in range(2)]
    iQT = P.sb("iQT", [128, 8, 512], BF16)
    qabsT = P.sb("qabsT", [128, 8, 512], BF16)
    iW = P.sb("iW", [128, 4, 8], F32)
    maskT = P.sb("maskT", [128, NT, 512], BF16)
    ov3 = P.sb_top
    rl = [P.sb(f"rl{i}", [128, 512], F32) for i in range(2)]
    ov3b = (ov3 + 63) // 64 * 64
    PTm = [P.sb(f"PTm{i}", [128, 512], BF16, offset=ov3b + i * 1024) for i in range(2)]
    olT = [P.sb(f"olT{i}", [128, 512], BF16, offset=ov3b + 2048 + i * 1024) for i in range(2)]
    dtmp = [P.sb(f"dtmp{i}", [128, 128], F32) for i in range(2)]
    rinv = P.sb("rinv", [128, 512], F32)
    wtab = P.sb("wtab", [128, NIT + 1], F32)
    bis = P.sb("bis", [128, 8], F32)
    PT = [P.sb(f"PT{i}", [128, 512], BF16) for i in range(4)]
    p2_end = P.sb_top
    region(R_K)
    x1blk = P.sb("x1blk", [128, 8, D], F32)
    lnv = P.sb("lnv", [128, 2, D], F32)
    assert P.sb_top <= P.reg[yAT.name][1]
    region(R_W)
    u2T = P.sb("u2T", [128, 8, 1024], BF16)
    comb = P.sb("comb", [128, 8, 16], F32)
    gatebc = P.sb("gatebc", [128, 2, D], F32)
    mixedT = P.sb("mixedT", [128, 8, 1024], BF16)
    R_34 = P.sb_top
    uT3 = P.sb("uT3", [128, 8, 1024], BF16)
    xin3 = P.sb("xin3", [128, 4, D], F32)
    wap = P.sb("wap", [128, 4, D], BF16)
    wbp = P.sb("wbp", [128, 4, D], BF16)
    wgt = [P.sb(f"wgt{i}", [128, 8, 2, 128], BF16) for i in range(2)]
    g01 = [P.sb(f"g01_{i}", [128, 512], BF16) for i in range(4)]
    tt01 = [P.sb(f"tt01_{i}", [128, 512], F32) for i in range(2)]
    p3a_end = P.sb_top
    region(R_34)
    wo = P.sb("wo", [128, 8, D], BF16)
    NSLOT = 3
    xrow = [P.sb(f"xrow{i}", [128, D], F32) for i in range(NSLOT)]
    r1 = [P.sb(f"r1_{i}", [128, D], F32) for i in range(NSLOT)]
    u2fs = [P.sb(f"u2f{i}", [128, 8, 128], F32) for i in range(NSLOT)]
    rtrs = [P.sb(f"rtr{i}", [128, 96], F32) for i in range(NSLOT)]
    dg = [P.sb(f"dg{i}", [128, 128], F32) for i in range(2)]
    p3_end = max(P.sb_top, p3a_end)
    region(P.reg[mixedT.name][1])
    yacc = P.sb("yacc", [128, 8, D], F32)
    wg = [P.sb(f"wg{i}", [128, 8, 256], BF16) for i in range(2)]
    wu = [P.sb(f"wu{i}", [128, 8, 256], BF16) for i in range(2)]
    wd = [P.sb(f"wd{i}", [128, 2, D], BF16) for i in range(2)]
    hT = [P.sb(f"hT{i}", [128, 2, 1024], BF16) for i in range(2)]
    sg = [P.sb(f"sg{i}", [128, 512], BF16) for i in range(2)]
    r2 = [P.sb("r2_0", [128, D], F32)]
    otile = [P.sb(f"otile{i}", [128, D], F32) for i in range(2)]
    p4_end = P.sb_top
    mem_report = dict(R_K=R_K, R_W=R_W, p2_end=p2_end, p3_end=p3_end, p4_end=p4_end, limit=P.sb_limit)

    pf = [P.ps(f"pf{i}", [128, 512], F32) for i in range(8)]
    pb = pf[7].bitcast(BF16)
    rr = {"i": 0}

    def bank():
        rr["i"] = (rr["i"] + 1) % 8
        return pf[rr["i"]]

    ev = {"i": 0}

    def evac(out, in_, scale=None):
        ev["i"] += 1
        if ev["i"] % 2 == 0:
            P.op("act", lambda e: e.activation(out=out, in_=in_, func=AF.Copy), ins=[in_], outs=[out])
        else:
            P.op("dve", lambda e: e.tensor_copy(out=out, in_=in_), ins=[in_], outs=[out])

    def dump(name, ap, shape):
        if name not in dbg:
            return
        t = nc.dram_tensor("dbg_" + name, list(shape), ap.dtype, kind="ExternalOutput").ap()
        dumps[name] = P.op("sp", lambda e: e.dma_start(out=t, in_=ap), ins=[ap], dma=True)

    def dma(eng, out, in_):
        if eng == "pool":
            return P.op(eng, lambda e: e.dma_start(out=out, in_=in_, max_dma_last_dim=8192), ins=[in_], outs=[out],
                        dma=True)
        return P.op(eng, lambda e: e.dma_start(out=out, in_=in_), ins=[in_], outs=[out], dma=True)

    class _Stop(Exception):
        pass

    hits = {}

    def checkpoint(name):
        hits[name] = hits.get(name, -1) + 1
        if stop == name or stop == f"{name}@{hits[name]}":
            raise _Stop()

    def wview(w_ap, c0, c1):
        return w_ap[:, c0:c1].rearrange("(k p) f -> p k f", p=128)

    try:
        dma("sp", identf[:], ident_d)
        dma("pool", identb[:], ident_d)
        P.op("pool", lambda e: e.memset(qaug[:], 0.0), outs=[qaug[:]])
        dma("pool", qaug[0:3, :, :], qaug_d)
        P.op("pool", lambda e: e.memset(augl[:], 0.0), outs=[augl[:]])
        P.op("pool", lambda e: e.memset(augl[0:3, :], 1.0), outs=[augl[0:3, :]])
        dma("sp", kbias[:], kbias_d)
        dma("sp", dbias[:], dbias_d)
        dma("sp", idxmask[:], idxmask_d)
        dma("sp", pow2[:], pow2_d)
        dma("sp", kvg[:], kvg_d)
        dma("sp", brt[:], brt_d)
        dma("pool", wukT[:], wukT_d)
        dma("pool", wuv[:], wuv_d)
        dma("sp", wrt[:], wrt_d.rearrange("(k p) f -> p k f", p=128))
        P.op("pool", lambda e: e.memset(onesb[:], 1.0), outs=[onesb[:]])
        P.op("pool", lambda e: e.memset(onesf[:], 1.0), outs=[onesf[:]])
        P.op("pool", lambda e: e.memset(epsc[:], RMS_EPS), outs=[epsc[:]])

        dma("sp", cT_sb[:], cT_d)
        dma("sp", adabT[:], adabT_d)
        dma("sp", lamA[:], lamA_d)
        dma("sp", lamB[:], lamB_d)
        dma("sp", asub[:], asub_d)
        P.op("act", lambda e: e.activation(out=condT[:], in_=cT_sb[:], func=AF.Silu), ins=[cT_sb[:]], outs=[condT[:]])
        for pc in range(4):
            aw = adaw[pc % 2]
            dma("pool", aw[:], wview(adaw_d, pc * 1536, (pc + 1) * 1536))
            bk = bank()
            fns = []
            for jj in range(12):
                for k in range(8):
                    fns.append(lambda e, jj=jj, k=k, aw=aw, bk=bk: e.matmul(
                        bk[:, jj * NB:(jj + 1) * NB], aw[:, k, jj * 128:(jj + 1) * 128], condT[:, k, :],
                        start=(k == 0), stop=(k == 7)))
            P.op("pe", fns, ins=[aw[:], condT[:]], outs=[bk[:, 0:12 * NB]])
            j0 = pc * 12
            P.op("dve", lambda e, bk=bk, j0=j0: e.tensor_tensor(
                out=modT[:, j0:j0 + 12, :], in0=bk[:, 0:12 * NB].rearrange("p (j b) -> p j b", b=NB),
                in1=adabT[:, j0:j0 + 12].unsqueeze(2).to_broadcast([128, 12, NB]), op=ALU.add),
                ins=[bk[:, 0:12 * NB], adabT[:]], outs=[modT[:, j0:j0 + 12, :]])
        for (a, b_) in ((8, 16), (32, 40)):
            P.op("dve", lambda e, a=a, b_=b_: e.tensor_scalar(out=modT[:, a:b_, :], in0=modT[:, a:b_, :], scalar1=1.0,
                                                             scalar2=None, op0=ALU.add),
                 ins=[modT[:, a:b_, :]], outs=[modT[:, a:b_, :]])
        dump("modT", modT[:], [128, 48, NB])
        checkpoint("p0")
        lp = small[:, 0:2]
        P.op("dve", lambda e: e.tensor_tensor(out=lamA[:], in0=lamA[:], in1=lamB[:], op=ALU.mult),
             ins=[lamA[:], lamB[:]], outs=[lamA[:]])
        P.op("dve", lambda e: e.tensor_reduce(out=lp, in_=lamA[:], axis=AX.X, op=ALU.add), ins=[lamA[:]], outs=[lp])
        P.op("act", lambda e: e.activation(out=small[:, 2:4], in_=lp, func=AF.Exp), ins=[lp], outs=[small[:, 2:4]])
        P.op("dve", lambda e: e.tensor_tensor(out=small[:, 4:5], in0=small[:, 3:4], in1=small[:, 2:3], op=ALU.subtract),
             ins=[small[:, 2:4]], outs=[small[:, 4:5]])
        P.op("dve", lambda e: e.tensor_scalar(out=lamneg[:], in0=small[:, 4:5], scalar1=-LAM_INIT, scalar2=None, op0=ALU.add),
             ins=[small[:, 4:5]], outs=[lamneg[:]])
        P.op("dve", lambda e: e.tensor_scalar(out=gsc[:], in0=asub[:], scalar1=1.0 - LAM_INIT, scalar2=None, op0=ALU.mult),
             ins=[asub[:]], outs=[gsc[:]])

        def load_uT(b, g, xt, ut, ucol0=0):
            r0 = b * S + g * 512
            dma("sp", xt[:], x_d[r0:r0 + 512, :].rearrange("(t p) d -> p t d", p=128))
            for k in range(8):
                bk = bank()
                P.op("pe", [lambda e, tt=tt, k=k, bk=bk: e.transpose(bk[:, tt * 128:(tt + 1) * 128],
                                                                  xt[:, tt, k * 128:(k + 1) * 128], identf[:])
                            for tt in range(4)], ins=[xt[:, :, k * 128:(k + 1) * 128], identf[:]], outs=[bk[:]])
                P.op("act", lambda e, k=k, bk=bk: e.activation(
                    out=ut[:, k, ucol0:ucol0 + 512], in_=bk[:], func=AF.Identity,
                    bias=modT[:, k, b:b + 1], scale=modT[:, 8 + k, b:b + 1]),
                    ins=[bk[:], modT[:]], outs=[ut[:, k, ucol0:ucol0 + 512]])

        def proj_fm(w, c0, ut, out_ap, ucol0=0, act=None):
            bk = bank()
            P.op("pe", [lambda e, k=k, bk=bk: e.matmul(bk[:], w[:, k, c0:c0 + 128], ut[:, k, ucol0:ucol0 + 512],
                                                    start=(k == 0), stop=(k == 7)) for k in range(8)],
                 ins=[w[:, :, c0:c0 + 128], ut[:, :, ucol0:ucol0 + 512]], outs=[bk[:]])
            if act is None:
                evac(out_ap, bk[:])
            else:
                P.op("act", lambda e: e.activation(out=out_ap, in_=bk[:], func=act), ins=[bk[:]], outs=[out_ap])

        def proj_fm_split(w, c0, ut, out_lo, out_hi):
            bk = bank()
            P.op("pe", [lambda e, k=k, bk=bk: e.matmul(bk[:], w[:, k, c0:c0 + 128], ut[:, k, 0:512],
                                                    start=(k == 0), stop=(k == 7)) for k in range(8)],
                 ins=[w[:, :, c0:c0 + 128], ut[:, :, 0:512]], outs=[bk[:]])
            P.op("act", lambda e: e.activation(out=out_lo, in_=bk[0:64, :], func=AF.Copy), ins=[bk[0:64, :]], outs=[out_lo])
            P.op("dve", lambda e: e.tensor_copy(out=out_hi, in_=bk[64:128, :]), ins=[bk[64:128, :]], outs=[out_hi])

        for b in range(NB):
            dma("pool", wK[:, :, 0:512], wview(win_d, C_AK, C_AK + 512))
            dma("pool", wK[:, :, 512:1024], wview(win_d, C_AV, C_AV + 512))
            dma("pool", wK[:, :, 1024:1152], wview(win_d, C_BKV, C_BKV + 128))
            dma("pool", wK[:, :, 1152:1280], wik2_d.rearrange("(k p) f -> p k f", p=128))
            for g in range(NG):
                load_uT(b, g, xin, uT)
                for h in range(4):
                    proj_fm(wK, h * 128, uT, aKT[:, h, g * 512:(g + 1) * 512])
                proj_fm(wK, 1152, uT, iKT[:, g * 512:(g + 1) * 512])
                for tt in range(4):
                    bk = bank()
                    P.op("pe", [lambda e, k=k, bk=bk, tt=tt: e.matmul(bk[:], uT[:, k, tt * 128:(tt + 1) * 128],
                                                                  wK[:, k, 512:1024], start=(k == 0), stop=(k == 7))
                                for k in range(8)], ins=[uT[:], wK[:, :, 512:1024]], outs=[bk[:]])
                    evac(aV[:, g * 4 + tt, :], bk[:])
                bk = bank()
                fns = []
                for tt in range(4):
                    for k in range(8):
                        fns.append(lambda e, k=k, bk=bk, tt=tt: e.matmul(
                            bk[:, tt * 128:(tt + 1) * 128], uT[:, k, tt * 128:(tt + 1) * 128], wK[:, k, 1024:1152],
                            start=(k == 0), stop=(k == 7)))
                P.op("pe", fns, ins=[uT[:], wK[:, :, 1024:1152]], outs=[bk[:]])
                P.op("act", lambda e, bk=bk: e.activation(out=sq1[:], in_=bk[:], func=AF.Square), ins=[bk[:]], outs=[sq1[:]])
                ssq = small[:, 8:12]
                P.op("dve", lambda e: e.tensor_reduce(out=ssq, in_=sq1[:].rearrange("p (t r) -> p t r", t=4), axis=AX.X,
                                                      op=ALU.add), ins=[sq1[:]], outs=[ssq])
                P.op("dve", lambda e: e.tensor_scalar(out=ssq, in0=ssq, scalar1=1.0 / 128.0, scalar2=RMS_EPS,
                                                      op0=ALU.mult, op1=ALU.add), ins=[ssq], outs=[ssq])
                P.op("act", lambda e: e.activation(out=ssq, in_=ssq, func=AF.Sqrt), ins=[ssq], outs=[ssq])
                P.op("dve", lambda e: e.reciprocal(out=ssq, in_=ssq), ins=[ssq], outs=[ssq])
                P.op("dve", lambda e, bk=bk: e.tensor_tensor(
                    out=t1[:], in0=bk[:].rearrange("p (t r) -> p t r", t=4),
                    in1=ssq.unsqueeze(2).to_broadcast([128, 4, 128]), op=ALU.mult),
                    ins=[bk[:], ssq], outs=[t1[:]])
                P.op("pool", lambda e, g=g: e.tensor_tensor(
                    out=cKV[:, g * 4:(g + 1) * 4, :], in0=t1[:],
                    in1=kvg[:].unsqueeze(1).to_broadcast([128, 4, 128]), op=ALU.mult),
                    ins=[t1[:], kvg[:]], outs=[cKV[:, g * 4:(g + 1) * 4, :]])
                pbh = pb[:, (g % 2) * 512:(g % 2) * 512 + 512]
                P.op("pe", [lambda e, tt=tt, g=g, pbh=pbh: e.transpose(pbh[:, tt * 128:(tt + 1) * 128],
                                                                   cKV[:, g * 4 + tt, :], identb[:])
                            for tt in range(4)], ins=[cKV[:, g * 4:(g + 1) * 4, :], identb[:]], outs=[pbh])
                evac(cKVT[:, g * 512:(g + 1) * 512], pbh)
            dump(f"aKT{b}", aKT[:], [128, 4, S])
            dump(f"aV{b}", aV[:], [128, NT, 512])
            dump(f"cKV{b}", cKV[:], [128, NT, 128])
            dump(f"cKVT{b}", cKVT[:], [128, S])
            dump(f"iKT{b}", iKT[:], [128, S])
            checkpoint("p1")

            dma("pool", wQ[:, :, 0:512], wview(win_d, C_AQ, C_AQ + 512))
            dma("pool", wQ[:, :, 512:1024], wview(win_d, C_BQ, C_BQ + 512))
            dma("pool", wQ[:, :, 1024:1536], wview(win_d, C_IQ, C_IQ + 512))
            dma("pool", wQ[:, :, 1536:1544], wview(win_d, C_IW, C_IW + 8))
            P.op("pool", lambda e: e.memset(aQT[:], 0.0), outs=[aQT[:]])
            P.op("pool", lambda e: e.memset(iQT[:], 0.0), outs=[iQT[:]])
            for g in range(NG):
                T0 = 4 * g
                load_uT(b, g, xin2, uT2)
                for h in range(4):
                    proj_fm_split(wQ, h * 128, uT2, aQT[0:64, h, 0, :], aQT[64:128, h, 1, :])
                    proj_fm(wQ, 512 + h * 128, uT2, bQT[:, h, :])
                    proj_fm_split(wQ, 1024 + h * 128, uT2, iQT[0:64, 2 * h, :], iQT[64:128, 2 * h + 1, :])
                bk = bank()
                fns = []
                for tt in range(4):
                    for k in range(8):
                        fns.append(lambda e, k=k, bk=bk, tt=tt: e.matmul(
                            bk[:, tt * 8:(tt + 1) * 8], uT2[:, k, tt * 128:(tt + 1) * 128], wQ[:, k, 1536:1544],
                            start=(k == 0), stop=(k == 7)))
                P.op("pe", fns, ins=[uT2[:], wQ[:, :, 1536:1544]], outs=[bk[:, 0:32]])
                P.op("dve", lambda e, bk=bk: e.tensor_copy(out=iW[:].rearrange("p t h -> p (t h)"), in_=bk[:, 0:32]),
                     ins=[bk[:, 0:32]], outs=[iW[:]])
                for h in range(8):
                    bk = bank()
                    P.op("pe", lambda e, bk=bk, h=h: e.matmul(bk[:], wukT[:, h, :], bQT[:, h // 2, :],
                                                           start=True, stop=True),
                         ins=[wukT[:, h, :], bQT[:, h // 2, :]], outs=[bk[:]])
                    evac(qabsT[:, h, :], bk[:])
                if b == 0 and g == NG - 1:
                    dump("qabsT", qabsT[:], [128, 8, 512])
                    dump("iW", iW[:], [128, 4, 8])
                    checkpoint("p2a")
                mx, mn, rg, thr, cnt, ee, lo = [bis[:, i:i + 1] for i in range(7)]

                def index_gen(T0=T0, b=b, g=g):
                    scoreB = [score, score1]

                    def l_units(qt):
                        T = T0 + qt
                        nk = (T + 1) * 128
                        scb = scoreB[qt % 2]
                        units = []
                        for kc in range((nk + 511) // 512):
                            ncol = min(512, nk - kc * 512)
                            for h in range(8):
                                def unit(kc=kc, ncol=ncol, h=h, qt=qt, scb=scb):
                                    bk = pf[6 + h % 2]
                                    P.op("pe", lambda e: e.matmul(
                                        bk[:, 0:ncol], iQT[:, h, qt * 128:(qt + 1) * 128],
                                        iKT[:, kc * 512:kc * 512 + ncol], start=True, stop=True),
                                        ins=[iQT[:, h, qt * 128:(qt + 1) * 128], iKT[:, kc * 512:kc * 512 + ncol]],
                                        outs=[bk[:, 0:ncol]])
                                    r = rl[h % 2]
                                    P.op("act", lambda e: e.activation(out=r[:, 0:ncol], in_=bk[:, 0:ncol], func=AF.Relu),
                                         ins=[bk[:, 0:ncol]], outs=[r[:, 0:ncol]])
                                    sc = scb[:, kc * 512:kc * 512 + ncol]
                                    if h == 0:
                                        P.op("act", lambda e: e.activation(out=sc, in_=r[:, 0:ncol], func=AF.Identity,
                                                                           scale=iW[:, qt, 0:1]),
                                             ins=[r[:, 0:ncol], iW[:]], outs=[sc])
                                    else:
                                        P.op("act", lambda e: e.activation(out=r[:, 0:ncol], in_=r[:, 0:ncol], func=AF.Identity,
                                                                           scale=iW[:, qt, h:h + 1]),
                                             ins=[r[:, 0:ncol], iW[:]], outs=[r[:, 0:ncol]])
                                        P.op("pool", lambda e: e.tensor_tensor(out=sc, in0=sc, in1=r[:, 0:ncol], op=ALU.add),
                                             ins=[sc, r[:, 0:ncol]], outs=[sc])
                                units.append(unit)
                        return units

                    def bz_steps(qt):
                        T = T0 + qt
                        nk = (T + 1) * 128
                        sv = scoreB[qt % 2][:, 0:nk]
                        sd = scoreB[qt % 2][:, T * 128:(T + 1) * 128]
                        steps = []

                        def setup():
                            if T >= 2:
                                P.op("dve", lambda e: e.tensor_reduce(out=mx, in_=sv, axis=AX.X, op=ALU.max), ins=[sv], outs=[mx])
                                P.op("dve", lambda e: e.tensor_reduce(out=mn, in_=sv, axis=AX.X, op=ALU.min), ins=[sv], outs=[mn])
                            P.op("dve", lambda e: e.tensor_tensor(out=sd, in0=sd, in1=idxmask[:], op=ALU.add),
                                 ins=[sd, idxmask[:]], outs=[sd])
                            if T >= 2:
                                P.op("dve", lambda e: e.tensor_tensor(out=rg, in0=mx, in1=mn, op=ALU.subtract), ins=[mx, mn], outs=[rg])
                                P.op("dve", lambda e: e.tensor_scalar(out=wtab[:], in0=pow2[:], scalar1=rg, scalar2=None,
                                                                      op0=ALU.mult), ins=[pow2[:], rg], outs=[wtab[:]])
                                P.op("dve", lambda e: e.tensor_tensor(out=thr, in0=mn, in1=wtab[:, 0:1], op=ALU.add),
                                     ins=[mn, wtab[:, 0:1]], outs=[thr])
                        steps.append(setup)
                        if T >= 2:
                            for it in range(NIT):
                                def iteration(it=it):
                                    P.op("dve", lambda e: e.tensor_scalar(
                                        out=junk[:, 0:nk], in0=sv, scalar1=thr, scalar2=None, op0=ALU.is_ge, op1=ALU.add,
                                        accum_out=cnt), ins=[sv, thr], outs=[junk[:, 0:nk], cnt])
                                    P.op("dve", lambda e: e.tensor_scalar(out=ee, in0=cnt, scalar1=TOPK - 0.5, scalar2=0.5,
                                                                          op0=ALU.is_ge, op1=ALU.subtract), ins=[cnt], outs=[ee])
                                    P.op("dve", lambda e: e.scalar_tensor_tensor(out=thr, in0=ee, scalar=wtab[:, it:it + 1], in1=thr,
                                                                                 op0=ALU.mult, op1=ALU.add),
                                         ins=[ee, wtab[:, it:it + 1], thr], outs=[thr])
                                steps.append(iteration)

                        def final():
                            if T >= 2:
                                P.op("dve", lambda e: e.scalar_tensor_tensor(out=lo, in0=wtab[:, NIT:NIT + 1], scalar=-1.5, in1=thr,
                                                                             op0=ALU.mult, op1=ALU.add),
                                     ins=[wtab[:, NIT:NIT + 1], thr], outs=[lo])
                                P.op("dve", lambda e: e.tensor_scalar(out=maskQ[:, 0:nk], in0=sv, scalar1=lo, scalar2=None,
                                                                      op0=ALU.is_ge), ins=[sv, lo],
                                     outs=[maskQ[:, 0:nk]])
                            else:
                                P.op("dve", lambda e: e.tensor_scalar(out=maskQ[:, 0:nk], in0=sv, scalar1=-1e29, scalar2=None,
                                                                      op0=ALU.is_ge), ins=[sv],
                                     outs=[maskQ[:, 0:nk]])
                            for kb0 in range(0, T + 1, 4):
                                nb = min(4, T + 1 - kb0)
                                half = (kb0 // 4) % 2
                                pbh = pb[:, half * 512:half * 512 + nb * 128]
                                P.op("pe", [lambda e, i=i, kb0=kb0, pbh=pbh: e.transpose(
                                    pbh[:, i * 128:(i + 1) * 128], maskQ[:, (kb0 + i) * 128:(kb0 + i + 1) * 128], identb[:])
                                    for i in range(nb)], ins=[maskQ[:, kb0 * 128:(kb0 + nb) * 128], identb[:]], outs=[pbh])
                                mo = maskT[:, kb0:kb0 + nb, qt * 128:(qt + 1) * 128]
                                P.op("act", lambda e, mo=mo, pbh=pbh, nb=nb: e.activation(
                                    out=mo, in_=pbh.rearrange("p (n q) -> p n q", n=nb), func=AF.Copy), ins=[pbh], outs=[mo])
                        steps.append(final)
                        return steps

                    for u in l_units(0):
                        u()
                        yield
                    for qt in range(4):
                        nxt = l_units(qt + 1) if qt < 3 else []
                        steps = bz_steps(qt)
                        nu = 0
                        for i, st_ in enumerate(steps[:-1]):
                            st_()
                            den = max(1, (6 * (len(steps) - 1)) // 10)
                            want = (len(nxt) * (i + 1) + den - 1) // den
                            while nu < min(want, len(nxt)):
                                nxt[nu]()
                                nu += 1
                            yield
                        while nu < len(nxt):
                            nxt[nu]()
                            nu += 1
                            yield
                        steps[-1]()
                        yield

                blocks = [("off", kb, 0) for kb in range(T0)] + [("diag", T0 + j, j) for j in range(4)]
                nblk = len(blocks)
                gcol = slice(g * 512, (g + 1) * 512)
                passes = []
                for h in range(4):
                    for m in range(2):
                        passes.append(dict(kind="A", h=h, m=m, hh=h,
                                           qk_lhsT=lambda kb, h=h: aKT[:, h, kb * 128:(kb + 1) * 128],
                                           qk_rhs=lambda c0, h=h, m=m: aQT[:, h, m, c0:512],
                                           pv_lhsT=lambda kb, h=h: aV[:, kb, h * 128:(h + 1) * 128],
                                           masked=False))
                for h in range(8):
                    passes.append(dict(kind="B", h=h, hh=4 + h,
                                       qk_lhsT=lambda kb: cKVT[:, kb * 128:(kb + 1) * 128],
                                       qk_rhs=lambda c0, h=h: qabsT[:, h, c0:512],
                                       pv_lhsT=lambda kb: cKV[:, kb, :],
                                       masked=True))
                items = [(pi, bi) for pi in range(len(passes)) for bi in range(nblk)]
                NI = len(items)

                def geom(n):
                    pi, bi = items[n]
                    kind, kb, j = blocks[bi]
                    c0 = j * 128 if kind == "diag" else 0
                    a0 = c0 + 128 if kind == "diag" else 0
                    return pi, bi, passes[pi], kind, kb, j, c0, a0

                NA = 8 * nblk
                SB4 = [pf[0], pf[1], pf[6], pf[7]]

                def sbank(n):
                    return pf[n % 2] if n < NA else SB4[n % 4]

                def st_qk(n):
                    pi, bi, ps_, kind, kb, j, c0, a0 = geom(n)
                    sb_ = sbank(n)
                    hh = ps_["hh"]
                    kl, qr = ps_["qk_lhsT"](kb), ps_["qk_rhs"](c0)
                    fns = [lambda e: e.matmul(sb_[:, c0:512], kl, qr, start=True, stop=(a0 >= 512))]
                    ins = [kl, qr]
                    if a0 < 512:
                        fns.append(lambda e: e.matmul(sb_[:, a0:512], augl[:], qaug[:, hh, a0:512], start=False, stop=True))
                        ins += [augl[:], qaug[:, hh, a0:512]]
                    P.op("pe", fns, ins=ins, outs=[sb_[:, c0:512]])

                def st_sm(n):
                    pi, bi, ps_, kind, kb, j, c0, a0 = geom(n)
                    sb_ = sbank(n)
                    hh = ps_["hh"]
                    pt = PT[n % 4]
                    if kind == "diag":
                        dt_ = dtmp[n % 2]
                        P.op("dve", lambda e: e.scalar_tensor_tensor(
                            out=dt_[:], in0=sb_[:, c0:c0 + 128], scalar=0.125, in1=dbias[:, hh, :],
                            op0=ALU.mult, op1=ALU.add), ins=[sb_[:, c0:c0 + 128], dbias[:, hh, :]], outs=[dt_[:]])
                        P.op("act", lambda e: e.activation(out=pt[:, c0:c0 + 128], in_=dt_[:], func=AF.Exp),
                             ins=[dt_[:]], outs=[pt[:, c0:c0 + 128]])
                    if a0 < 512:
                        dpi = (T0 - kb) + 3
                        P.op("act", lambda e: e.activation(
                            out=pt[:, a0:512], in_=sb_[:, a0:512], func=AF.Exp, scale=0.125,
                            bias=kbias[:, hh, dpi:dpi + 1]),
                            ins=[sb_[:, a0:512], kbias[:, hh, dpi:dpi + 1]], outs=[pt[:, a0:512]])

                def st_mask(n):
                    pi, bi, ps_, kind, kb, j, c0, a0 = geom(n)
                    if ps_["masked"]:
                        pt = PT[n % 4]
                        P.op("dve", lambda e: e.tensor_tensor(
                            out=pt[:, c0:512], in0=pt[:, c0:512], in1=maskT[:, kb, c0:512], op=ALU.mult),
                            ins=[pt[:, c0:512], maskT[:, kb, c0:512]], outs=[pt[:, c0:512]])

                def st_pv(n):
                    pi, bi, ps_, kind, kb, j, c0, a0 = geom(n)
                    src = PT[n % 4]
                    OA, RS = pf[2 + pi % 2], pf[4 + pi % 2]
                    first, last = (bi == 0), (bi == nblk - 1)
                    vl = ps_["pv_lhsT"](kb)
                    P.op("pe", [lambda e: e.matmul(OA[:, c0:512], vl, src[:, c0:512], start=first, stop=last),
                                lambda e: e.matmul(RS[:, c0:512], onesb[:], src[:, c0:512], start=first, stop=last)],
                         ins=[vl, src[:, c0:512], onesb[:]], outs=[OA[:, c0:512], RS[:, c0:512]])

                pending = []

                def fin_a(pi, n):
                    ps_ = passes[pi]
                    OA, RS = pf[2 + pi % 2], pf[4 + pi % 2]
                    P.op("act", lambda e: e.activation(out=rinv[:], in_=RS[:], func=AF.Ln), ins=[RS[:]], outs=[rinv[:]])
                    P.op("act", lambda e: e.activation(out=rinv[:], in_=rinv[:], func=AF.Exp, scale=-1.0),
                         ins=[rinv[:]], outs=[rinv[:]])
                    if ps_["kind"] == "A":
                        h, m = ps_["h"], ps_["m"]
                        om = o01[m]
                        od = o01[0]
                        dfr = 2 if nblk >= 8 else 0
                        sqb = o01[1] if dfr else rinv

                        def fin_dve():
                            P.op("dve", lambda e: e.tensor_tensor(out=om[:], in0=OA[:], in1=rinv[:], op=ALU.mult),
                                 ins=[OA[:], rinv[:]], outs=[om[:]])
                            if m == 1:
                                P.op("dve", lambda e: e.scalar_tensor_tensor(out=od[:], in0=o01[1][:], scalar=lamneg[:, 0:1],
                                                                             in1=od[:], op0=ALU.mult, op1=ALU.add),
                                     ins=[o01[1][:], lamneg[:], od[:]], outs=[od[:]])
                                P.op("act", lambda e: e.activation(out=sqb[:], in_=od[:], func=AF.Square),
                                     ins=[od[:]], outs=[sqb[:]])
                        if dfr:
                            pending.append((n + dfr, fin_dve))
                        else:
                            fin_dve()
                        if m == 1:
                            def fin_pe():
                                P.op("pe", lambda e: e.matmul(RS[:], onesf[:], sqb[:], start=True, stop=True),
                                     ins=[onesf[:], sqb[:]], outs=[RS[:]])

                            def fin_b():
                                P.op("act", lambda e: e.activation(out=sqb[:], in_=RS[:], func=AF.Ln, scale=1.0 / 128.0,
                                                                   bias=epsc[:, 0:1]),
                                     ins=[RS[:], epsc[:]], outs=[sqb[:]])
                                P.op("act", lambda e: e.activation(out=sqb[:], in_=sqb[:], func=AF.Exp, scale=-0.5),
                                     ins=[sqb[:]], outs=[sqb[:]])
                                yo = yAT[:, h, gcol]
                                P.op("dve", lambda e: e.scalar_tensor_tensor(out=yo, in0=od[:], scalar=gsc[:, 0:1], in1=sqb[:],
                                                                             op0=ALU.mult, op1=ALU.mult),
                                     ins=[od[:], gsc[:], sqb[:]], outs=[yo])
                            pending.append((n + 2 + dfr, fin_pe))
                            pending.append((n + 3 + dfr, fin_b))
                    else:
                        h = ps_["h"]
                        ot = olT[h % 2]
                        P.op("dve", lambda e: e.tensor_tensor(out=ot[:], in0=OA[:], in1=rinv[:], op=ALU.mult),
                             ins=[OA[:], rinv[:]], outs=[ot[:]])

                        def fin_pe():
                            P.op("pe", lambda e: e.matmul(RS[:], wuv[:, h, :], ot[:], start=True, stop=True),
                                 ins=[wuv[:, h, :], ot[:]], outs=[RS[:]])
                        pending.append((n + 2, fin_pe))

                        def fin_b():
                            p0 = (h % 2) * 64
                            yo = yBT[p0:p0 + 64, h // 2, gcol]
                            src_ = RS[p0:p0 + 64, :]
                            if h % 2 == 0:
                                P.op("act", lambda e: e.activation(out=yo, in_=src_, func=AF.Copy), ins=[src_], outs=[yo])
                            else:
                                P.op("dve", lambda e: e.tensor_copy(out=yo, in_=src_), ins=[src_], outs=[yo])
                        pending.append((n + 3, fin_b))

                def attn_gen():
                    st_qk(0)
                    nq = 1
                    for n in range(NI):
                        tgt = min(NI - 1, n + (3 if n >= NA else 1))
                        while nq <= tgt:
                            st_qk(nq)
                            nq += 1
                        st_sm(n)
                        st_mask(n)
                        st_pv(n)
                        if items[n][1] == nblk - 1:
                            fin_a(items[n][0], n)
                        while pending and pending[0][0] <= n:
                            pending.pop(0)[1]()
                        yield
                    while pending:
                        pending.pop(0)[1]()

                ig, ag = index_gen(), attn_gen()
                NY = 8 * (((T0 + 1) * 128 + 511) // 512) + sum((NIT + 2) if T0 + qt >= 2 else 2 for qt in range(4))
                ia = iy = 0
                for _ in ig:
                    iy += 1
                    while ia < NA - 1 and ia * NY < iy * NA:
                        next(ag)
                        ia += 1
                while ia < NA:
                    next(ag)
                    ia += 1
                for _ in ag:
                    pass
            dump(f"yAT{b}", yAT[:], [128, 4, S])
            dump(f"yBT{b}", yBT[:], [128, 4, S])
            checkpoint("p2")

            for blk in range(NBLK):
                dma("pool", wap[:], wap_d.rearrange("(k p) f -> p k f", p=128))
                dma("pool", wbp[:], wbp_d.rearrange("(k p) f -> p k f", p=128))
                for gl in range(2):
                    load_uT(b, blk * 2 + gl, xin3, uT3, ucol0=gl * 512)
                tok0 = blk * 1024
                for j in range(8):
                    wj = wgt[j % 2]
                    dma("pool", wj[:, :, 0, :], wview(win_d, C_G + j * 128, C_G + (j + 1) * 128))
                    dma("pool", wj[:, :, 1, :], wview(win_d, C_G + 1024 + j * 128, C_G + 1024 + (j + 1) * 128))
                    for gl in range(2):
                        gs = []
                        for gi in range(2):
                            bk = bank()
                            P.op("pe", [lambda e, k=k, bk=bk, wj=wj, gi=gi, gl=gl: e.matmul(
                                bk[:], wj[:, k, gi, :], uT3[:, k, gl * 512:(gl + 1) * 512], start=(k == 0), stop=(k == 7))
                                for k in range(8)], ins=[wj[:, :, gi, :], uT3[:, :, gl * 512:(gl + 1) * 512]], outs=[bk[:]])
                            gt = g01[(gl * 2 + gi) % 4]
                            P.op("act", lambda e, gt=gt, bk=bk: e.activation(out=gt[:], in_=bk[:], func=AF.Sigmoid),
                                 ins=[bk[:]], outs=[gt[:]])
                            gs.append(gt)
                        ts_ = []
                        for gi, (wp, yT) in enumerate(((wap, yAT), (wbp, yBT))):
                            bk = bank()
                            c_lo = tok0 + gl * 512
                            P.op("pe", [lambda e, c=c, bk=bk, wp=wp, yT=yT, c_lo=c_lo, j=j: e.matmul(
                                bk[:], wp[:, c, j * 128:(j + 1) * 128], yT[:, c, c_lo:c_lo + 512], start=(c == 0), stop=(c == 3))
                                for c in range(4)], ins=[wp[:, :, j * 128:(j + 1) * 128], yT[:, :, c_lo:c_lo + 512]],
                                outs=[bk[:]])
                            tt_ = tt01[gi]
                            P.op("dve", lambda e, tt_=tt_, bk=bk, gt=gs[gi]: e.tensor_tensor(out=tt_[:], in0=bk[:], in1=gt[:],
                                                                                          op=ALU.mult),
                                 ins=[bk[:], gs[gi][:]], outs=[tt_[:]])
                            ts_.append(tt_)
                        mo = mixedT[:, j, gl * 512:(gl + 1) * 512]
                        P.op("pool", lambda e, mo=mo, a=ts_[0], c=ts_[1]: e.tensor_tensor(out=mo, in0=a[:], in1=c[:], op=ALU.add),
                             ins=[ts_[0][:], ts_[1][:]], outs=[mo])
                if b == 0 and blk == 0:
                    dump("mixedT", mixedT[:], [128, 8, 1024])
                dma("pool", wo[:], wo_d.rearrange("(k p) f -> p k f", p=128))
                dma("sp", lnv[:], lnbc_d[:, 0:2, :])
                for gi, ch0 in enumerate((16, 40)):
                    for half in range(2):
                        bk = bank()
                        for jj in range(4):
                            j = half * 4 + jj
                            dgt = dg[jj % 2]
                            P.op("dve", lambda e, dgt=dgt, j=j, ch0=ch0: e.tensor_scalar(
                                out=dgt[:], in0=identf[:], scalar1=modT[:, ch0 + j, b:b + 1], scalar2=None, op0=ALU.mult),
                                ins=[identf[:], modT[:]], outs=[dgt[:]])
                            P.op("pe", lambda e, dgt=dgt, bk=bk, jj=jj: e.matmul(bk[:, jj * 128:(jj + 1) * 128], onesf[:], dgt[:],
                                                                              start=True, stop=True),
                                 ins=[onesf[:], dgt[:]], outs=[bk[:, jj * 128:(jj + 1) * 128]])
                        evac(gatebc[:, gi, half * 512:(half + 1) * 512], bk[:])
                if blk == 0:
                    dump(f"gatebc{b}", gatebc[:], [128, 2, D])
                def layer_norm(src, gidx, out_ap, scratch, st):
                    s1, s2, mean, msq, var = st[0], st[1], st[2], st[3], st[4]
                    P.op("act", lambda e: e.activation(out=scratch, in_=src, func=AF.Copy, accum_out=s1),
                         ins=[src], outs=[scratch, s1])
                    P.op("act", lambda e: e.activation(out=scratch, in_=src, func=AF.Square, accum_out=s2),
                         ins=[src], outs=[scratch, s2])
                    yield
                    P.op("dve", lambda e: e.tensor_scalar(out=mean, in0=s1, scalar1=1.0 / D, scalar2=None, op0=ALU.mult),
                         ins=[s1], outs=[mean])
                    P.op("dve", lambda e: e.tensor_tensor(out=msq, in0=mean, in1=mean, op=ALU.mult), ins=[mean], outs=[msq])
                    P.op("dve", lambda e: e.scalar_tensor_tensor(out=var, in0=s2, scalar=1.0 / D, in1=msq, op0=ALU.mult,
                                                                 op1=ALU.subtract), ins=[s2, msq], outs=[var])
                    P.op("dve", lambda e: e.tensor_scalar(out=var, in0=var, scalar1=LN_EPS, scalar2=None, op0=ALU.add),
                         ins=[var], outs=[var])
                    P.op("act", lambda e: e.activation(out=var, in_=var, func=AF.Sqrt), ins=[var], outs=[var])
                    yield
                    P.op("dve", lambda e: e.reciprocal(out=var, in_=var), ins=[var], outs=[var])
                    P.op("dve", lambda e: e.tensor_scalar(out=src, in0=src, scalar1=mean, scalar2=var, op0=ALU.subtract,
                                                          op1=ALU.mult), ins=[src, mean, var], outs=[src])
                    P.op("dve", lambda e: e.tensor_tensor(out=src, in0=src, in1=lnv[:, 0, :], op=ALU.mult),
                         ins=[src, lnv[:, 0, :]], outs=[src])
                    P.op("pool", lambda e: e.tensor_tensor(out=out_ap, in0=src, in1=lnv[:, 1, :], op=ALU.add),
                         ins=[src, lnv[:, 1, :]], outs=[out_ap])
                    yield

                def tile_gen(t, b=b, tok0=tok0):
                    slot = t % NSLOT
                    row0 = b * S + tok0 + t * 128
                    xr = xrow[slot]
                    rt_ = r1[slot]
                    u2f = u2fs[slot]
                    rtr = rtrs[slot]
                    st = [small[:, 16 + 8 * slot + i:17 + 8 * slot + i] for i in range(8)]
                    dma("sp", xr[:], x_d[row0:row0 + 128, :])
                    for half in range(2):
                        bk = bank()
                        cs = slice(half * 512, (half + 1) * 512)
                        P.op("pe", [lambda e, k=k, bk=bk, t=t, cs=cs: e.matmul(
                            bk[:], mixedT[:, k, t * 128:(t + 1) * 128], wo[:, k, cs], start=(k == 0), stop=(k == 7))
                            for k in range(8)], ins=[mixedT[:, :, t * 128:(t + 1) * 128], wo[:, :, cs]], outs=[bk[:]])
                        P.op("dve", lambda e, bk=bk, rt_=rt_, cs=cs: e.tensor_tensor(out=rt_[:, cs], in0=bk[:],
                                                                                  in1=gatebc[:, 0, cs], op=ALU.mult),
                             ins=[bk[:], gatebc[:, 0, cs]], outs=[rt_[:, cs]])
                    P.op("dve", lambda e, xr=xr, rt_=rt_: e.scalar_tensor_tensor(out=rt_[:], in0=xr[:], scalar=ALPHA, in1=rt_[:],
                                                                                op0=ALU.mult, op1=ALU.add),
                         ins=[xr[:], rt_[:]], outs=[rt_[:]])
                    yield
                    yield from layer_norm(rt_[:], 0, x1blk[:, t, :], xr[:], st)
                    for kh in range(2):
                        bk = bank()
                        P.op("pe", [lambda e, kk=kk, bk=bk, t=t, kh=kh: e.transpose(
                            bk[:, kk * 128:(kk + 1) * 128], x1blk[:, t, (kh * 4 + kk) * 128:(kh * 4 + kk + 1) * 128], identf[:])
                            for kk in range(4)], ins=[x1blk[:, t, kh * 512:(kh + 1) * 512], identf[:]], outs=[bk[:]])
                        for kk in range(4):
                            k = kh * 4 + kk
                            P.op("act", lambda e, bk=bk, kk=kk, k=k: e.activation(
                                out=u2f[:, k, :], in_=bk[:, kk * 128:(kk + 1) * 128], func=AF.Identity,
                                bias=modT[:, 24 + k, b:b + 1], scale=modT[:, 32 + k, b:b + 1]),
                                ins=[bk[:, kk * 128:(kk + 1) * 128], modT[:]], outs=[u2f[:, k, :]])
                    P.op("pool", lambda e, t=t: e.tensor_copy(out=u2T[:, :, t * 128:(t + 1) * 128], in_=u2f[:]),
                         ins=[u2f[:]], outs=[u2T[:, :, t * 128:(t + 1) * 128]])
                    bk = bank()
                    P.op("pe", [lambda e, k=k, bk=bk: e.matmul(bk[:, 0:20], u2f[:, k, :], wrt[:, k, :], start=(k == 0),
                                                              stop=(k == 7)) for k in range(8)],
                         ins=[u2f[:], wrt[:]], outs=[bk[:, 0:20]])
                    yield
                    lg = rtr[:, 0:20]
                    GL, EL = rtr[:, 0:4], rtr[:, 4:20]
                    gmax, ngmax, gsum, gtop = rtr[:, 20:21], rtr[:, 21:22], rtr[:, 22:23], rtr[:, 23:24]
                    ohg, gexp = rtr[:, 24:28], rtr[:, 28:32]
                    t44, els, oh1, els2, oh2 = rtr[:, 32:48], rtr[:, 48:52], rtr[:, 52:56], rtr[:, 56:60], rtr[:, 60:64]
                    m1, m2, dd, ex, w1, w2 = [rtr[:, 64 + i:65 + i] for i in range(6)]
                    we = rtr[:, 72:76]
                    P.op("dve", lambda e, bk=bk: e.tensor_tensor(out=lg, in0=bk[:, 0:20], in1=brt[:], op=ALU.add),
                         ins=[bk[:, 0:20], brt[:]], outs=[lg])
                    P.op("dve", lambda e: e.tensor_reduce(out=gmax, in_=GL, axis=AX.X, op=ALU.max), ins=[GL], outs=[gmax])
                    P.op("dve", lambda e: e.tensor_scalar(out=ohg, in0=GL, scalar1=gmax, scalar2=None, op0=ALU.is_ge),
                         ins=[GL, gmax], outs=[ohg])
                    P.op("dve", lambda e: e.tensor_scalar(out=ngmax, in0=gmax, scalar1=-1.0, scalar2=None, op0=ALU.mult),
                         ins=[gmax], outs=[ngmax])
                    P.op("act", lambda e: e.activation(out=gexp, in_=GL, func=AF.Exp, bias=ngmax, scale=1.0, accum_out=gsum),
                         ins=[GL, ngmax], outs=[gexp, gsum])
                    yield
                    P.op("dve", lambda e: e.reciprocal(out=gtop, in_=gsum), ins=[gsum], outs=[gtop])
                    P.op("dve", lambda e: e.tensor_tensor(
                        out=t44.rearrange("p (g e) -> p g e", g=4), in0=EL.rearrange("p (g e) -> p g e", g=4),
                        in1=ohg.unsqueeze(2).to_broadcast([128, 4, 4]), op=ALU.mult), ins=[EL, ohg], outs=[t44])
                    P.op("dve", lambda e: e.tensor_reduce(out=els, in_=t44.rearrange("p (g e) -> p e g", g=4), axis=AX.X,
                                                          op=ALU.add), ins=[t44], outs=[els])
                    P.op("dve", lambda e: e.tensor_reduce(out=m1, in_=els, axis=AX.X, op=ALU.max), ins=[els], outs=[m1])
                    P.op("dve", lambda e: e.tensor_scalar(out=oh1, in0=els, scalar1=m1, scalar2=None, op0=ALU.is_ge),
                         ins=[els, m1], outs=[oh1])
                    P.op("dve", lambda e: e.scalar_tensor_tensor(out=els2, in0=oh1, scalar=-1e30, in1=els, op0=ALU.mult,
                                                                 op1=ALU.add), ins=[oh1, els], outs=[els2])
                    P.op("dve", lambda e: e.tensor_reduce(out=m2, in_=els2, axis=AX.X, op=ALU.max), ins=[els2], outs=[m2])
                    P.op("dve", lambda e: e.tensor_scalar(out=oh2, in0=els2, scalar1=m2, scalar2=None, op0=ALU.is_ge),
                         ins=[els2, m2], outs=[oh2])
                    P.op("dve", lambda e: e.tensor_tensor(out=dd, in0=m2, in1=m1, op=ALU.subtract), ins=[m2, m1], outs=[dd])
                    P.op("act", lambda e: e.activation(out=ex, in_=dd, func=AF.Exp), ins=[dd], outs=[ex])
                    P.op("dve", lambda e: e.tensor_scalar(out=w1, in0=ex, scalar1=1.0, scalar2=None, op0=ALU.add),
                         ins=[ex], outs=[w1])
                    P.op("dve", lambda e: e.reciprocal(out=w1, in_=w1), ins=[w1], outs=[w1])
                    P.op("dve", lambda e: e.tensor_tensor(out=w2, in0=ex, in1=w1, op=ALU.mult), ins=[ex, w1], outs=[w2])
                    P.op("dve", lambda e: e.tensor_scalar(out=we, in0=oh1, scalar1=w1, scalar2=None, op0=ALU.mult),
                         ins=[oh1, w1], outs=[we])
                    P.op("dve", lambda e: e.scalar_tensor_tensor(out=we, in0=oh2, scalar=w2, in1=we, op0=ALU.mult, op1=ALU.add),
                         ins=[oh2, w2, we], outs=[we])
                    P.op("dve", lambda e: e.tensor_scalar(out=we, in0=we, scalar1=gtop, scalar2=None, op0=ALU.mult),
                         ins=[we, gtop], outs=[we])
                    P.op("dve", lambda e, t=t: e.tensor_tensor(
                        out=comb[:, t, :].rearrange("p (g e) -> p g e", g=4),
                        in0=ohg.unsqueeze(2).to_broadcast([128, 4, 4]),
                        in1=we.unsqueeze(1).to_broadcast([128, 4, 4]), op=ALU.mult), ins=[ohg, we], outs=[comb[:, t, :]])
                    yield

                tgens = [tile_gen(t) for t in range(8)]
                active = []
                nstart = 0
                while nstart < 8 or active:
                    if nstart < 8 and len(active) < NSLOT:
                        active.append(tgens[nstart])
                        nstart += 1
                    for tg in list(active):
                        try:
                            next(tg)
                        except StopIteration:
                            active.remove(tg)
                if b == 0 and blk == 0:
                    dump("x1blk", x1blk[:], [128, 8, D])
                    dump("comb", comb[:], [128, 8, 16])
                    dump("u2T", u2T[:], [128, 8, 1024])
                    checkpoint("p3")

                def moe_gu(ex_):
                    wg_, wu_, wd_ = wg[ex_ % 2], wu[ex_ % 2], wd[ex_ % 2]
                    dma("pool", wg_[:], weg_d[ex_].rearrange("(k p) f -> p k f", p=128))
                    dma("pool", wu_[:], weu_d[ex_].rearrange("(k p) f -> p k f", p=128))
                    dma("pool", wd_[:], wed_d[ex_].rearrange("(k p) f -> p k f", p=128))
                    hT_ = hT[ex_ % 2]
                    for gl in range(2):
                        for fh in range(2):
                            bg, bu = bank(), bank()
                            for (bk, w_) in ((bg, wg_), (bu, wu_)):
                                P.op("pe", [lambda e, k=k, bk=bk, w_=w_, fh=fh, gl=gl: e.matmul(
                                    bk[:], w_[:, k, fh * 128:(fh + 1) * 128], u2T[:, k, gl * 512:(gl + 1) * 512],
                                    start=(k == 0), stop=(k == 7)) for k in range(8)],
                                    ins=[w_[:, :, fh * 128:(fh + 1) * 128], u2T[:, :, gl * 512:(gl + 1) * 512]], outs=[bk[:]])
                            s_ = sg[(gl * 2 + fh) % 2]
                            P.op("act", lambda e, s_=s_, bg=bg: e.activation(out=s_[:], in_=bg[:], func=AF.Silu),
                                 ins=[bg[:]], outs=[s_[:]])
                            ho = hT_[:, fh, gl * 512:(gl + 1) * 512]
                            P.op("dve", lambda e, ho=ho, s_=s_, bu=bu: e.tensor_tensor(out=ho, in0=bu[:], in1=s_[:], op=ALU.mult),
                                 ins=[bu[:], s_[:]], outs=[ho])

                def moe_dn(ex_):
                    wd_, hT_ = wd[ex_ % 2], hT[ex_ % 2]
                    for t in range(8):
                        for half in range(2):
                            bk = bank()
                            cs = slice(half * 512, (half + 1) * 512)
                            P.op("pe", [lambda e, fh=fh, bk=bk, t=t, cs=cs: e.matmul(
                                bk[:], hT_[:, fh, t * 128:(t + 1) * 128], wd_[:, fh, cs], start=(fh == 0), stop=(fh == 1))
                                for fh in range(2)], ins=[hT_[:, :, t * 128:(t + 1) * 128], wd_[:, :, cs]], outs=[bk[:]])
                            ya = yacc[:, t, cs]
                            if ex_ == 0:
                                P.op("dve", lambda e, ya=ya, bk=bk, t=t: e.tensor_scalar(
                                    out=ya, in0=bk[:], scalar1=comb[:, t, 0:1], scalar2=None, op0=ALU.mult),
                                    ins=[bk[:], comb[:, t, 0:1]], outs=[ya])
                            else:
                                P.op("dve", lambda e, ya=ya, bk=bk, t=t, ex_=ex_: e.scalar_tensor_tensor(
                                    out=ya, in0=bk[:], scalar=comb[:, t, ex_:ex_ + 1], in1=ya, op0=ALU.mult, op1=ALU.add),
                                    ins=[bk[:], comb[:, t, ex_:ex_ + 1], ya], outs=[ya])

                moe_gu(0)
                for ex_ in range(16):
                    if ex_ + 1 < 16:
                        moe_gu(ex_ + 1)
                    moe_dn(ex_)
                if b == 0 and blk == 0:
                    dump("yacc", yacc[:], [128, 8, D])
                dma("sp", lnv[:], lnbc_d[:, 2:4, :])
                def ln2_gen(t, b=b, tok0=tok0):
                    row0 = b * S + tok0 + t * 128
                    r_ = yacc[:, t, :]
                    ot = otile[t % 2]
                    st2 = [small[:, 40 + 8 * (t % 2) + i:41 + 8 * (t % 2) + i] for i in range(8)]
                    P.op("dve", lambda e: e.tensor_tensor(out=r_, in0=r_, in1=gatebc[:, 1, :], op=ALU.mult),
                         ins=[r_, gatebc[:, 1, :]], outs=[r_])
                    P.op("dve", lambda e: e.scalar_tensor_tensor(out=r_, in0=x1blk[:, t, :], scalar=ALPHA,
                                                                 in1=r_, op0=ALU.mult, op1=ALU.add),
                         ins=[x1blk[:, t, :], r_], outs=[r_])
                    yield
                    yield from layer_norm(r_, 2, ot[:], ot[:], st2)
                    fin = dma("sp", y_d[row0:row0 + 128, :], ot[:])
                    finals.append(fin)

                lgens = [ln2_gen(t) for t in range(8)]
                active = []
                nstart = 0
                while nstart < 8 or active:
                    if nstart < 8 and len(active) < 2:
                        active.append(lgens[nstart])
                        nstart += 1
                    for tg in list(active):
                        try:
                            next(tg)
                        except StopIteration:
                            active.remove(tg)
    except _Stop:
        pass
    P.emit(final_wait_ops=finals + list(dumps.values()))
    return nc, P, mem_report


finals = []


def prep_inputs(inputs, core, NB=2, S=2048):
    f = lambda a: np.ascontiguousarray(np.asarray(a, dtype=np.float32))
    b0 = core * NB
    x = f(inputs["x"])[b0:b0 + NB, :S].reshape(NB * S, D)
    c = f(inputs["c"])[b0:b0 + NB]
    cT = f(c.T.reshape(8, 128, NB).transpose(1, 0, 2))
    ada_b = f(inputs["ada_b"])[0]
    rep = lambda v: f(np.broadcast_to(np.asarray(v, np.float32)[None, :], (128, len(v))))
    w_in = f(inputs["w_in"])[0]
    w_uk = f(inputs["w_uk"])[0]
    w_uv = f(inputs["w_uv"])[0]
    w_ukTz = np.zeros((128, 8, 128), np.float32)
    w_uvz = np.zeros((128, 8, 128), np.float32)
    for h in range(8):
        w_ukTz[(h % 2) * 64:(h % 2) * 64 + 64, h, :] = w_uk[:, h, :].T
        w_uvz[:, h, (h % 2) * 64:(h % 2) * 64 + 64] = w_uv[:, h, :]
    d = dict(
        x=x, cT=cT, ada_w=f(inputs["ada_w"])[0], ada_bT=f(ada_b.reshape(48, 128).T),
        w_in=w_in, w_ik2=f(np.concatenate([w_in[:, C_IK:C_IK + 64], w_in[:, C_IK:C_IK + 64]], 1)),
        lamA=f(np.stack([rep(f(inputs["lambda_q1"])[0]), rep(f(inputs["lambda_q2"])[0])], 1)),
        lamB=f(np.stack([rep(f(inputs["lambda_k1"])[0]), rep(f(inputs["lambda_k2"])[0])], 1)),
        asub=f(f(inputs["a_subln_g"])[0].reshape(128, 1)),
        kvg_bc=rep(f(inputs["kv_norm_g"])[0]),
        w_ukTz=w_ukTz, w_uvz=w_uvz,
        w_a_proj=f(inputs["w_a_proj"])[0], w_b_proj=f(inputs["w_b_proj"])[0], w_o=f(inputs["w_o"])[0],
        ln_bc=f(np.stack([rep(f(inputs["ln1_g"])[0]), rep(f(inputs["ln1_b"])[0]),
                          rep(f(inputs["ln2_g"])[0]), rep(f(inputs["ln2_b"])[0])], 1)),
        w_router=f(np.concatenate([f(inputs["w_group"])[0], f(inputs["w_expert_router"])[0]], 1)),
        b_router_bc=rep(np.concatenate([f(inputs["b_group"])[0], f(inputs["b_expert_router"])[0]])),
        w_exp_gate=f(inputs["w_exp_gate"])[0], w_exp_up=f(inputs["w_exp_up"])[0], w_exp_down=f(inputs["w_exp_down"])[0],
    )
    d.update(host_constants())
    return d


_CACHE = {}


def kernel(**inputs):
    NB, S = 2, 2048
    if "nc" not in _CACHE:
        finals.clear()
        _CACHE["nc"] = build(NB, S)[0]
    nc = _CACHE["nc"]
    in_maps = [prep_inputs(inputs, c, NB, S) for c in range(8)]
    res = run_bass_kernel_spmd(nc, in_maps, core_ids=list(range(8)))
    out = np.concatenate([np.asarray(r["y"], dtype=np.float32).reshape(NB, S, D) for r in res.results], axis=0)
    return out
```

```python
import math
import types
import numpy as np
import ml_dtypes
import concourse.bass as bass
import concourse.mybir as mybir
from concourse.bass_utils import run_bass_kernel_spmd

F32 = mybir.dt.float32
BF16 = mybir.dt.bfloat16
ALU = mybir.AluOpType
AF = mybir.ActivationFunctionType
AX = mybir.AxisListType

SEM_WRAP = 30000
N_DMA_SLOTS = 16
CELL = 32
ENGS = ("pe", "act", "dve", "pool", "sp")
EIDX = {e: i for i, e in enumerate(ENGS)}


def _esize(dt):
    return {F32: 4, BF16: 2}.get(dt, 4)


def _freeze(fn):
    if not getattr(fn, "__closure__", None):
        return fn
    cells = []
    for c in fn.__closure__:
        try:
            cells.append(types.CellType(c.cell_contents))
        except ValueError:
            cells.append(c)
    g = types.FunctionType(fn.__code__, fn.__globals__, fn.__name__, fn.__defaults__, tuple(cells))
    g.__kwdefaults__ = fn.__kwdefaults__
    return g


class Prog:
    def __init__(self, nc):
        self.nc = nc
        self.ops = []
        self.reg = {}
        self.sb_top = 16512
        self.sb_limit = 229344
        self.spaces = {}
        self.seq = {e: 0 for e in ENGS}
        self.dma_reads = []
        self._space("sb", nc.SBUF_PARTITION_SIZE_BYTES)

    def _space(self, name, nbytes):
        n = (nbytes + CELL - 1) // CELL
        self.spaces[name] = dict(
            lw=np.full((len(ENGS), n), -1, np.int64),
            lr=np.full((len(ENGS), n), -1, np.int64),
            dw=np.full(n, -1, np.int64),
        )

    def sb(self, name, shape, dtype, offset=None):
        es = _esize(dtype)
        nbytes = int(np.prod(shape[1:])) * es
        if offset is None:
            offset = (self.sb_top + 63) // 64 * 64
            self.sb_top = offset + nbytes
        assert offset + nbytes <= self.sb_limit, (name, offset, nbytes)
        h = self.nc.alloc_sbuf_tensor_at(name, list(shape), dtype, offset=offset)
        self.reg[h.name] = ("sb", offset, es, int(np.prod(shape[1:])))
        return h

    def ps(self, name, shape, dtype=F32):
        es = _esize(dtype)
        h = self.nc.alloc_psum_tensor(name, list(shape), dtype)
        sp = "ps:" + h.name
        self._space(sp, int(np.prod(shape[1:])) * es)
        self.reg[h.name] = (sp, 0, es, int(np.prod(shape[1:])))
        return h

    def _ranges(self, ap):
        r = self.reg.get(ap.name)
        if r is None:
            return None
        space, base, es, row = r
        if space.startswith("ps:"):
            return space, [(0, (row * es + CELL - 1) // CELL)]
        off = int(ap.offset) % row
        dims = [(int(s), int(c)) for (s, c) in ap.ap[1:]]
        runs = []
        if len(dims) == 0:
            runs = [(off, off + 1)]
        else:
            inner = dims[-1]
            ext = abs(inner[0]) * (inner[1] - 1) + 1
            outer = dims[:-1]
            n_outer = int(np.prod([c for _, c in outer])) if outer else 1
            if n_outer > 64:
                lo = off
                hi = off + 1
                for s, c in dims:
                    if s >= 0:
                        hi += s * (c - 1)
                    else:
                        lo += s * (c - 1)
                runs = [(lo, hi)]
            else:
                offs = [off]
                for s, c in outer:
                    offs = [o + i * s for o in offs for i in range(c)]
                for o in offs:
                    if inner[0] >= 0:
                        runs.append((o, o + ext))
                    else:
                        runs.append((o - ext + 1, o + 1))
        out = []
        for lo, hi in runs:
            out.append(((base + lo * es) // CELL, (base + hi * es + CELL - 1) // CELL))
        out.sort()
        merged = []
        for lo, hi in out:
            if merged and lo <= merged[-1][1]:
                merged[-1] = (merged[-1][0], max(merged[-1][1], hi))
            else:
                merged.append((lo, hi))
        return space, merged

    def op(self, eng, fns, ins=(), outs=(), dma=False):
        if callable(fns):
            fns = [fns]
        fns = [_freeze(f) for f in fns]
        oid = len(self.ops)
        ei = EIDX[eng]
        waits = {}
        dwaits = set()
        rd = [self._ranges(a) for a in ins]
        wr = [self._ranges(a) for a in outs]

        def need(e_i, s):
            if s >= 0 and waits.get(e_i, -1) < s:
                waits[e_i] = s

        for fp in rd:
            if fp is None:
                continue
            st = self.spaces[fp[0]]
            for lo, hi in fp[1]:
                m = st["lw"][:, lo:hi].max(axis=1)
                for e_i in range(len(ENGS)):
                    need(e_i, int(m[e_i]))
                d = st["dw"][lo:hi]
                if (d >= 0).any():
                    dwaits.update(int(v) for v in np.unique(d[d >= 0]))
        for fp in wr:
            if fp is None:
                continue
            st = self.spaces[fp[0]]
            for lo, hi in fp[1]:
                m = np.maximum(st["lw"][:, lo:hi].max(axis=1), st["lr"][:, lo:hi].max(axis=1))
                for e_i in range(len(ENGS)):
                    need(e_i, int(m[e_i]))
                d = st["dw"][lo:hi]
                if (d >= 0).any():
                    dwaits.update(int(v) for v in np.unique(d[d >= 0]))
                if self.dma_reads:
                    keep = []
                    for (did, dsp, dlo, dhi) in self.dma_reads:
                        if dsp == fp[0] and dlo < hi and lo < dhi:
                            dwaits.add(did)
                        else:
                            keep.append((did, dsp, dlo, dhi))
                    self.dma_reads = keep
        if dma:
            for fp in rd:
                if fp is None:
                    continue
                for lo, hi in fp[1]:
                    self.dma_reads.append((oid, fp[0], lo, hi))
            for fp in wr:
                if fp is None:
                    continue
                st = self.spaces[fp[0]]
                for lo, hi in fp[1]:
                    st["dw"][lo:hi] = oid
                    st["lw"][:, lo:hi] = -1
                    st["lr"][:, lo:hi] = -1
            seq = None
        else:
            seq = self.seq[eng]
            self.seq[eng] += 1
            for fp in rd:
                if fp is None:
                    continue
                st = self.spaces[fp[0]]
                for lo, hi in fp[1]:
                    st["lr"][ei, lo:hi] = seq
            for fp in wr:
                if fp is None:
                    continue
                st = self.spaces[fp[0]]
                for lo, hi in fp[1]:
                    st["lw"][:, lo:hi] = -1
                    st["lr"][:, lo:hi] = -1
                    st["lw"][ei, lo:hi] = seq
                    st["dw"][lo:hi] = -1
        self.ops.append(dict(eng=eng, fns=fns, waits=waits, dwaits=dwaits, dma=dma, id=oid, seq=seq))
        return oid

    def emit(self, final_wait_ops=()):
        nc = self.nc
        ops = self.ops
        by_eng = {e: [o for o in ops if o["eng"] == e] for e in ENGS}
        dma_cnt = {e: 0 for e in ENGS}
        last_slot = {}
        for o in ops:
            if o["dma"]:
                k = dma_cnt[o["eng"]]
                dma_cnt[o["eng"]] += 1
                o["slot"] = k % N_DMA_SLOTS
                o["sval"] = 16 * (k // N_DMA_SLOTS + 1)
                key = (o["eng"], o["slot"])
                if key in last_slot:
                    o["dwaits"].add(last_slot[key])
                last_slot[key] = o["id"]
        signal = {e: set() for e in ENGS}
        for e in ENGS:
            waited = {}
            dwaited = {}
            for o in by_eng[e]:
                w2 = {}
                for e_i, s in o["waits"].items():
                    if waited.get(e_i, -1) >= s:
                        continue
                    w2[e_i] = s
                    waited[e_i] = s
                    signal[ENGS[e_i]].add(s)
                o["waits2"] = w2
                d2 = []
                for d in sorted(o["dwaits"]):
                    od = ops[d]
                    key = (od["eng"], od["slot"])
                    if dwaited.get(key, 0) >= od["sval"]:
                        continue
                    dwaited[key] = od["sval"]
                    d2.append(d)
                o["dwaits2"] = d2
        sig_sorted = {e: sorted(signal[e]) for e in ENGS}
        sig_rank = {e: {s: i for i, s in enumerate(sig_sorted[e])} for e in ENGS}
        sems = {}
        for e in ENGS:
            nsem = len(sig_sorted[e]) // SEM_WRAP + 1
            sems[e] = [nc.alloc_semaphore(f"s_{e}_{i}") for i in range(nsem)]
        dsems = {}
        for e in ENGS:
            if dma_cnt[e]:
                dsems[e] = [nc.alloc_semaphore(f"d_{e}_{i}") for i in range(min(N_DMA_SLOTS, dma_cnt[e]))]

        def csig(e, s):
            r = sig_rank[e][s]
            return sems[e][r // SEM_WRAP], r % SEM_WRAP + 1

        self.n_waits = 0
        self.n_sig = sum(len(v) for v in sig_sorted.values())
        self.n_ins = 0
        finals = [(dsems[ops[d]["eng"]][ops[d]["slot"]], ops[d]["sval"]) for d in final_wait_ops]
        with nc.Block() as block:
            hw = {"pe": block.tensor, "act": block.scalar, "dve": block.vector,
                  "pool": block.gpsimd, "sp": block.sync}

            def make(e):
                def body(eng):
                    for o in by_eng[e]:
                        for e_i, s in o["waits2"].items():
                            sem, val = csig(ENGS[e_i], s)
                            eng.wait_ge(sem, val)
                            self.n_waits += 1
                        for d in o["dwaits2"]:
                            od = ops[d]
                            eng.wait_ge(dsems[od["eng"]][od["slot"]], od["sval"])
                            self.n_waits += 1
                        ins = None
                        for fn in o["fns"]:
                            ins = fn(eng)
                            self.n_ins += 1
                        if o["dma"]:
                            ins.then_inc(dsems[e][o["slot"]], 16)
                        elif o["seq"] in sig_rank[e]:
                            sem, val = csig(e, o["seq"])
                            ins.then_inc(sem, 1)
                    if e == "sp":
                        for sem, val in finals:
                            eng.wait_ge(sem, val)
                return body

            for e in ENGS:
                if by_eng[e] or (e == "sp" and finals):
                    hw[e](make(e))


D = 1024
NIT = 20
LN_EPS = 1e-5
RMS_EPS = 1e-5
ALPHA = 2.0 ** 0.25
LAM_INIT = 0.8 - 0.6 * math.exp(0.0)
NEG = -30000.0
C_AQ, C_AK, C_AV, C_BQ, C_BKV, C_IQ, C_IK, C_IW, C_G = 0, 512, 1024, 1536, 2048, 2176, 2688, 2752, 2760


def _slopes():
    n = 12
    s = (2.0 ** (-8.0 * np.arange(1, n + 1) / n)).astype(np.float32).astype(np.float64)
    a_idx = np.arange(4) * 3
    b_idx = np.setdiff1d(np.arange(n), a_idx)
    return np.concatenate([s[a_idx], s[b_idx]])


def _bf16_split3(v):
    hi = v.astype(ml_dtypes.bfloat16).astype(np.float64)
    r = v - hi
    mid = r.astype(ml_dtypes.bfloat16).astype(np.float64)
    lo = (r - mid).astype(ml_dtypes.bfloat16).astype(np.float64)
    return np.stack([hi, mid, lo], 0)


def _hl(v):
    hi = v.astype(ml_dtypes.bfloat16).astype(np.float64)
    lo = (v - hi).astype(ml_dtypes.bfloat16).astype(np.float64)
    return np.stack([hi, lo], 1).astype(np.float32)


def host_constants():
    sl = _slopes()
    qrel = np.arange(512, dtype=np.float64)
    qaug = np.stack([_bf16_split3(-8.0 * sl[h] * qrel) for h in range(12)], 1)
    p = np.arange(128, dtype=np.float64)
    dp = np.arange(-3, 13, dtype=np.float64)
    kbias = sl[None, :, None] * (p[:, None, None] - 128.0 * dp[None, None, :])
    qq = np.arange(128)
    vis = (p[:, None].astype(int) // 64) <= (qq[None, :] // 64)
    dist = np.abs(qq[None, :] - p[:, None])
    dbias = np.where(vis[:, None, :], -sl[None, :, None] * dist[:, None, :], NEG)
    idxmask = np.where((p[None, :].astype(int) // 64) <= (qq[:, None] // 64), 0.0, -1e30)
    pow2 = np.tile((2.0 ** -(np.arange(NIT + 1) + 1.0))[None, :], (128, 1))
    return dict(
        ident=np.eye(128, dtype=np.float32),
        qaug=qaug.astype(np.float32),
        kbias=kbias.astype(np.float32),
        dbias=_hl(dbias * 8.0),
        idxmask=idxmask.astype(np.float32),
        pow2=pow2.astype(np.float32),
    )


def build(NB=2, S=2048, dbg=(), stop=None):
    nc = bass.Bass("TRN2", target_bir_lowering=False)
    P = Prog(nc)
    NT, NG, NBLK = S // 128, S // 512, S // 1024
    TOPK = min(256, S // 4)
    dumps = {}

    def din(name, shape):
        return nc.dram_tensor(name, list(shape), F32, kind="ExternalInput").ap()

    x_d = din("x", [NB * S, D])
    cT_d = din("cT", [128, 8, NB])
    adaw_d = din("ada_w", [D, 6 * D])
    adabT_d = din("ada_bT", [128, 48])
    win_d = din("w_in", [D, 4808])
    wik2_d = din("w_ik2", [D, 128])
    lamA_d = din("lamA", [128, 2, 64])
    lamB_d = din("lamB", [128, 2, 64])
    asub_d = din("asub", [128, 1])
    kvg_d = din("kvg_bc", [128, 128])
    wukT_d = din("w_ukTz", [128, 8, 128])
    wuv_d = din("w_uvz", [128, 8, 128])
    wap_d = din("w_a_proj", [512, D])
    wbp_d = din("w_b_proj", [512, D])
    wo_d = din("w_o", [D, D])
    lnbc_d = din("ln_bc", [128, 4, D])
    wrt_d = din("w_router", [D, 20])
    brt_d = din("b_router_bc", [128, 20])
    weg_d = din("w_exp_gate", [16, D, 256])
    weu_d = din("w_exp_up", [16, D, 256])
    wed_d = din("w_exp_down", [16, 256, D])
    ident_d = din("ident", [128, 128])
    qaug_d = din("qaug", [3, 12, 512])
    kbias_d = din("kbias", [128, 12, 16])
    dbias_d = din("dbias", [128, 2, 12, 128])
    idxmask_d = din("idxmask", [128, 128])
    pow2_d = din("pow2", [128, NIT + 1])
    y_d = nc.dram_tensor("y", [NB * S, D], F32, kind="ExternalOutput").ap()

    identf = P.sb("identf", [128, 128], F32)
    identb = P.sb("identb", [128, 128], BF16)
    onesb = P.sb("onesb", [128, 128], BF16)
    onesf = P.sb("onesf", [128, 128], F32)
    qaug = P.sb("qaug_sb", [128, 12, 512], BF16)
    augl = P.sb("augl", [128, 128], BF16)
    kbias = P.sb("kbias_sb", [128, 12, 16], F32)
    dbias = P.sb("dbias_sb", [128, 2, 12, 128], BF16)
    idxmask = P.sb("idxmask_sb", [128, 128], F32)
    pow2 = P.sb("pow2_sb", [128, NIT + 1], F32)
    modT = P.sb("modT", [128, 48, NB], F32)
    lamneg = P.sb("lamneg", [128, 1], F32)
    gsc = P.sb("gsc", [128, 1], F32)
    kvg = P.sb("kvg", [128, 128], F32)
    brt = P.sb("brt", [128, 20], F32)
    wukT = P.sb("wukT", [128, 8, 128], BF16)
    wuv = P.sb("wuv", [128, 8, 128], BF16)
    wrt = P.sb("wrt", [128, 8, 20], F32)
    small = P.sb("small", [128, 64], F32)
    epsc = P.sb("epsc", [128, 2], F32)
    R_K = (P.sb_top + 63) // 64 * 64
    aKT = P.sb("aKT", [128, 4, S], BF16)
    aV = P.sb("aV", [128, NT, 512], BF16)
    cKV = P.sb("cKV", [128, NT, 128], BF16)
    cKVT = P.sb("cKVT", [128, S], BF16)
    iKT = P.sb("iKT", [128, S], BF16)
    P.sb_top = max(P.sb_top, R_K + 8 * D * 4 + 2 * D * 4 + 128)
    yAT = P.sb("yAT", [128, 4, S], BF16)
    yBT = P.sb("yBT", [128, 4, S], BF16)
    R_W = (P.sb_top + 63) // 64 * 64

    def region(start):
        P.sb_top = start

    region(R_W)
    cT_sb = P.sb("cT_sb", [128, 8, NB], F32)
    condT = P.sb("condT", [128, 8, NB], BF16)
    adabT = P.sb("adabT", [128, 48], F32)
    lamA = P.sb("lamA_sb", [128, 2, 64], F32)
    lamB = P.sb("lamB_sb", [128, 2, 64], F32)
    asub = P.sb("asub_sb", [128, 1], F32)
    adaw = [P.sb(f"adaw{i}", [128, 8, 1536], BF16) for i in range(2)]
    region(R_W)
    xin = P.sb("xin", [128, 4, D], F32)
    uT = P.sb("uT", [128, 8, 512], BF16)
    R_12 = P.sb_top
    wK = P.sb("wK", [128, 8, 1280], BF16)
    sq1 = P.sb("sq1", [128, 512], F32)
    t1 = P.sb("t1", [128, 4, 128], F32)
    region(R_12)
    xin2, uT2 = xin, uT
    xin_off = P.reg[xin.name][1]
    score = P.sb("score", [128, S], F32, offset=xin_off)
    score1 = P.sb("score1", [128, S], F32, offset=P.reg[uT.name][1])
    assert S * 4 <= 8 * 512 * 2
    junk = P.sb("junk", [128, S], BF16, offset=xin_off + S * 4)
    maskQ = P.sb("maskQ", [128, S], BF16, offset=xin_off + S * 6)
    assert S * 8 <= 4 * D * 4
    wQ = P.sb("wQ", [128, 8, 1544], BF16)
    aQT = P.sb("aQT", [128, 4, 2, 512], BF16)
    ov1 = P.sb_top
    bQT = P.sb("bQT", [128, 4, 512], BF16)
    o01 = [P.sb(f"o01_{i}", [128, 512], F32, offset=(ov1 + 63) // 64 * 64 + i * 2048) for i in range(2)]
    iQT = P.sb("iQT", [128, 8, 512], BF16)
    qabsT = P.sb("qabsT", [128, 8, 512], BF16)
    iW = P.sb("iW", [128, 4, 8], F32)
    maskT = P.sb("maskT", [128, NT, 512], BF16)
    ov3 = P.sb_top
    rl = [P.sb(f"rl{i}", [128, 512], F32) for i in range(2)]
    ov3b = (ov3 + 63) // 64 * 64
    PTm = [P.sb(f"PTm{i}", [128, 512], BF16, offset=ov3b + i * 1024) for i in range(2)]
    olT = [P.sb(f"olT{i}", [128, 512], BF16, offset=ov3b + 2048 + i * 1024) for i in range(2)]
    dtmp = [P.sb(f"dtmp{i}", [128, 128], F32) for i in range(2)]
    rinv = P.sb("rinv", [128, 512], F32)
    wtab = P.sb("wtab", [128, NIT + 1], F32)
    bis = P.sb("bis", [128, 8], F32)
    PT = [P.sb(f"PT{i}", [128, 512], BF16) for i in range(4)]
    p2_end = P.sb_top
    region(R_K)
    x1blk = P.sb("x1blk", [128, 8, D], F32)
    lnv = P.sb("lnv", [128, 2, D], F32)
    assert P.sb_top <= P.reg[yAT.name][1]
    region(R_W)
    u2T = P.sb("u2T", [128, 8, 1024], BF16)
    comb = P.sb("comb", [128, 8, 16], F32)
    gatebc = P.sb("gatebc", [128, 2, D], F32)
    mixedT = P.sb("mixedT", [128, 8, 1024], BF16)
    R_34 = P.sb_top
    uT3 = P.sb("uT3", [128, 8, 1024], BF16)
    xin3 = P.sb("xin3", [128, 4, D], F32)
    wap = P.sb("wap", [128, 4, D], BF16)
    wbp = P.sb("wbp", [128, 4, D], BF16)
    wgt = [P.sb(f"wgt{i}", [128, 8, 2, 128], BF16) for i in range(2)]
    g01 = [P.sb(f"g01_{i}", [128, 512], BF16) for i in range(4)]
    tt01 = [P.sb(f"tt01_{i}", [128, 512], F32) for i in range(2)]
    p3a_end = P.sb_top
    region(R_34)
    wo = P.sb("wo", [128, 8, D], BF16)
    NSLOT = 3
    xrow = [P.sb(f"xrow{i}", [128, D], F32) for i in range(NSLOT)]
    r1 = [P.sb(f"r1_{i}", [128, D], F32) for i in range(NSLOT)]
    u2fs = [P.sb(f"u2f{i}", [128, 8, 128], F32) for i in range(NSLOT)]
    rtrs = [P.sb(f"rtr{i}", [128, 96], F32) for i in range(NSLOT)]
    dg = [P.sb(f"dg{i}", [128, 128], F32) for i in range(2)]
    p3_end = max(P.sb_top, p3a_end)
    region(P.reg[mixedT.name][1])
    yacc = P.sb("yacc", [128, 8, D], F32)
    wg = [P.sb(f"wg{i}", [128, 8, 256], BF16) for i in range(2)]
    wu = [P.sb(f"wu{i}", [128, 8, 256], BF16) for i in range(2)]
    wd = [P.sb(f"wd{i}", [128, 2, D], BF16) for i in range(2)]
    hT = [P.sb(f"hT{i}", [128, 2, 1024], BF16) for i in range(2)]
    sg = [P.sb(f"sg{i}", [128, 512], BF16) for i in range(2)]
    r2 = [P.sb("r2_0", [128, D], F32)]
    otile = [P.sb(f"otile{i}", [128, D], F32) for i in range(2)]
    p4_end = P.sb_top
    mem_report = dict(R_K=R_K, R_W=R_W, p2_end=p2_end, p3_end=p3_end, p4_end=p4_end, limit=P.sb_limit)

    pf = [P.ps(f"pf{i}", [128, 512], F32) for i in range(8)]
    pb = pf[7].bitcast(BF16)
    rr = {"i": 0}

    def bank():
        rr["i"] = (rr["i"] + 1) % 8
        return pf[rr["i"]]

    ev = {"i": 0}

    def evac(out, in_, scale=None):
        ev["i"] += 1
        if ev["i"] % 2 == 0:
            P.op("act", lambda e: e.activation(out=out, in_=in_, func=AF.Copy), ins=[in_], outs=[out])
        else:
            P.op("dve", lambda e: e.tensor_copy(out=out, in_=in_), ins=[in_], outs=[out])

    def dump(name, ap, shape):
        if name not in dbg:
            return
        t = nc.dram_tensor("dbg_" + name, list(shape), ap.dtype, kind="ExternalOutput").ap()
        dumps[name] = P.op("sp", lambda e: e.dma_start(out=t, in_=ap), ins=[ap], dma=True)

    def dma(eng, out, in_):
        if eng == "pool":
            return P.op(eng, lambda e: e.dma_start(out=out, in_=in_, max_dma_last_dim=8192), ins=[in_], outs=[out],
                        dma=True)
        return P.op(eng, lambda e: e.dma_start(out=out, in_=in_), ins=[in_], outs=[out], dma=True)

    class _Stop(Exception):
        pass

    hits = {}

    def checkpoint(name):
        hits[name] = hits.get(name, -1) + 1
        if stop == name or stop == f"{name}@{hits[name]}":
            raise _Stop()

    def wview(w_ap, c0, c1):
        return w_ap[:, c0:c1].rearrange("(k p) f -> p k f", p=128)

    try:
        dma("sp", identf[:], ident_d)
        dma("pool", identb[:], ident_d)
        P.op("pool", lambda e: e.memset(qaug[:], 0.0), outs=[qaug[:]])
        dma("pool", qaug[0:3, :, :], qaug_d)
        P.op("pool", lambda e: e.memset(augl[:], 0.0), outs=[augl[:]])
        P.op("pool", lambda e: e.memset(augl[0:3, :], 1.0), outs=[augl[0:3, :]])
        dma("sp", kbias[:], kbias_d)
        dma("pool", dbias[:], dbias_d)
        dma("sp", idxmask[:], idxmask_d)
        dma("sp", pow2[:], pow2_d)
        dma("sp", kvg[:], kvg_d)
        dma("sp", brt[:], brt_d)
        dma("pool", wukT[:], wukT_d)
        dma("pool", wuv[:], wuv_d)
        dma("sp", wrt[:], wrt_d.rearrange("(k p) f -> p k f", p=128))
        P.op("pool", lambda e: e.memset(onesb[:], 1.0), outs=[onesb[:]])
        P.op("pool", lambda e: e.memset(onesf[:], 1.0), outs=[onesf[:]])
        P.op("pool", lambda e: e.memset(epsc[:], RMS_EPS), outs=[epsc[:]])

        dma("sp", cT_sb[:], cT_d)
        dma("sp", adabT[:], adabT_d)
        dma("sp", lamA[:], lamA_d)
        dma("sp", lamB[:], lamB_d)
        dma("sp", asub[:], asub_d)
        P.op("act", lambda e: e.activation(out=condT[:], in_=cT_sb[:], func=AF.Silu), ins=[cT_sb[:]], outs=[condT[:]])
        for pc in range(4):
            aw = adaw[pc % 2]
            dma("pool", aw[:], wview(adaw_d, pc * 1536, (pc + 1) * 1536))
            bk = bank()
            fns = []
            for jj in range(12):
                for k in range(8):
                    fns.append(lambda e, jj=jj, k=k, aw=aw, bk=bk: e.matmul(
                        bk[:, jj * NB:(jj + 1) * NB], aw[:, k, jj * 128:(jj + 1) * 128], condT[:, k, :],
                        start=(k == 0), stop=(k == 7)))
            P.op("pe", fns, ins=[aw[:], condT[:]], outs=[bk[:, 0:12 * NB]])
            j0 = pc * 12
            P.op("dve", lambda e, bk=bk, j0=j0: e.tensor_tensor(
                out=modT[:, j0:j0 + 12, :], in0=bk[:, 0:12 * NB].rearrange("p (j b) -> p j b", b=NB),
                in1=adabT[:, j0:j0 + 12].unsqueeze(2).to_broadcast([128, 12, NB]), op=ALU.add),
                ins=[bk[:, 0:12 * NB], adabT[:]], outs=[modT[:, j0:j0 + 12, :]])
        for (a, b_) in ((8, 16), (32, 40)):
            P.op("dve", lambda e, a=a, b_=b_: e.tensor_scalar(out=modT[:, a:b_, :], in0=modT[:, a:b_, :], scalar1=1.0,
                                                             scalar2=None, op0=ALU.add),
                 ins=[modT[:, a:b_, :]], outs=[modT[:, a:b_, :]])
        dump("modT", modT[:], [128, 48, NB])
        checkpoint("p0")
        lp = small[:, 0:2]
        P.op("dve", lambda e: e.tensor_tensor(out=lamA[:], in0=lamA[:], in1=lamB[:], op=ALU.mult),
             ins=[lamA[:], lamB[:]], outs=[lamA[:]])
        P.op("dve", lambda e: e.tensor_reduce(out=lp, in_=lamA[:], axis=AX.X, op=ALU.add), ins=[lamA[:]], outs=[lp])
        P.op("act", lambda e: e.activation(out=small[:, 2:4], in_=lp, func=AF.Exp), ins=[lp], outs=[small[:, 2:4]])
        P.op("dve", lambda e: e.tensor_tensor(out=small[:, 4:5], in0=small[:, 3:4], in1=small[:, 2:3], op=ALU.subtract),
             ins=[small[:, 2:4]], outs=[small[:, 4:5]])
        P.op("dve", lambda e: e.tensor_scalar(out=lamneg[:], in0=small[:, 4:5], scalar1=-LAM_INIT, scalar2=None, op0=ALU.add),
             ins=[small[:, 4:5]], outs=[lamneg[:]])
        P.op("dve", lambda e: e.tensor_scalar(out=gsc[:], in0=asub[:], scalar1=1.0 - LAM_INIT, scalar2=None, op0=ALU.mult),
             ins=[asub[:]], outs=[gsc[:]])

        def load_uT(b, g, xt, ut, ucol0=0):
            r0 = b * S + g * 512
            dma("sp", xt[:], x_d[r0:r0 + 512, :].rearrange("(t p) d -> p t d", p=128))
            for k in range(8):
                bk = bank()
                P.op("pe", [lambda e, tt=tt, k=k, bk=bk: e.transpose(bk[:, tt * 128:(tt + 1) * 128],
                                                                  xt[:, tt, k * 128:(k + 1) * 128], identf[:])
                            for tt in range(4)], ins=[xt[:, :, k * 128:(k + 1) * 128], identf[:]], outs=[bk[:]])
                P.op("act", lambda e, k=k, bk=bk: e.activation(
                    out=ut[:, k, ucol0:ucol0 + 512], in_=bk[:], func=AF.Identity,
                    bias=modT[:, k, b:b + 1], scale=modT[:, 8 + k, b:b + 1]),
                    ins=[bk[:], modT[:]], outs=[ut[:, k, ucol0:ucol0 + 512]])

        def proj_fm(w, c0, ut, out_ap, ucol0=0, act=None):
            bk = bank()
            P.op("pe", [lambda e, k=k, bk=bk: e.matmul(bk[:], w[:, k, c0:c0 + 128], ut[:, k, ucol0:ucol0 + 512],
                                                    start=(k == 0), stop=(k == 7)) for k in range(8)],
                 ins=[w[:, :, c0:c0 + 128], ut[:, :, ucol0:ucol0 + 512]], outs=[bk[:]])
            if act is None:
                evac(out_ap, bk[:])
            else:
                P.op("act", lambda e: e.activation(out=out_ap, in_=bk[:], func=act), ins=[bk[:]], outs=[out_ap])

        def proj_fm_split(w, c0, ut, out_lo, out_hi):
            bk = bank()
            P.op("pe", [lambda e, k=k, bk=bk: e.matmul(bk[:], w[:, k, c0:c0 + 128], ut[:, k, 0:512],
                                                    start=(k == 0), stop=(k == 7)) for k in range(8)],
                 ins=[w[:, :, c0:c0 + 128], ut[:, :, 0:512]], outs=[bk[:]])
            P.op("act", lambda e: e.activation(out=out_lo, in_=bk[0:64, :], func=AF.Copy), ins=[bk[0:64, :]], outs=[out_lo])
            P.op("dve", lambda e: e.tensor_copy(out=out_hi, in_=bk[64:128, :]), ins=[bk[64:128, :]], outs=[out_hi])

        for b in range(NB):
            dma("pool", wK[:, :, 0:512], wview(win_d, C_AK, C_AK + 512))
            dma("pool", wK[:, :, 512:1024], wview(win_d, C_AV, C_AV + 512))
            dma("pool", wK[:, :, 1024:1152], wview(win_d, C_BKV, C_BKV + 128))
            dma("pool", wK[:, :, 1152:1280], wik2_d.rearrange("(k p) f -> p k f", p=128))
            for g in range(NG):
                load_uT(b, g, xin, uT)
                for h in range(4):
                    proj_fm(wK, h * 128, uT, aKT[:, h, g * 512:(g + 1) * 512])
                proj_fm(wK, 1152, uT, iKT[:, g * 512:(g + 1) * 512])
                for tt in range(4):
                    bk = bank()
                    P.op("pe", [lambda e, k=k, bk=bk, tt=tt: e.matmul(bk[:], uT[:, k, tt * 128:(tt + 1) * 128],
                                                                  wK[:, k, 512:1024], start=(k == 0), stop=(k == 7))
                                for k in range(8)], ins=[uT[:], wK[:, :, 512:1024]], outs=[bk[:]])
                    evac(aV[:, g * 4 + tt, :], bk[:])
                bk = bank()
                fns = []
                for tt in range(4):
                    for k in range(8):
                        fns.append(lambda e, k=k, bk=bk, tt=tt: e.matmul(
                            bk[:, tt * 128:(tt + 1) * 128], uT[:, k, tt * 128:(tt + 1) * 128], wK[:, k, 1024:1152],
                            start=(k == 0), stop=(k == 7)))
                P.op("pe", fns, ins=[uT[:], wK[:, :, 1024:1152]], outs=[bk[:]])
                P.op("act", lambda e, bk=bk: e.activation(out=sq1[:], in_=bk[:], func=AF.Square), ins=[bk[:]], outs=[sq1[:]])
                ssq = small[:, 8:12]
                P.op("dve", lambda e: e.tensor_reduce(out=ssq, in_=sq1[:].rearrange("p (t r) -> p t r", t=4), axis=AX.X,
                                                      op=ALU.add), ins=[sq1[:]], outs=[ssq])
                P.op("dve", lambda e: e.tensor_scalar(out=ssq, in0=ssq, scalar1=1.0 / 128.0, scalar2=RMS_EPS,
                                                      op0=ALU.mult, op1=ALU.add), ins=[ssq], outs=[ssq])
                P.op("act", lambda e: e.activation(out=ssq, in_=ssq, func=AF.Sqrt), ins=[ssq], outs=[ssq])
                P.op("dve", lambda e: e.reciprocal(out=ssq, in_=ssq), ins=[ssq], outs=[ssq])
                P.op("dve", lambda e, bk=bk: e.tensor_tensor(
                    out=t1[:], in0=bk[:].rearrange("p (t r) -> p t r", t=4),
                    in1=ssq.unsqueeze(2).to_broadcast([128, 4, 128]), op=ALU.mult),
                    ins=[bk[:], ssq], outs=[t1[:]])
                P.op("pool", lambda e, g=g: e.tensor_tensor(
                    out=cKV[:, g * 4:(g + 1) * 4, :], in0=t1[:],
                    in1=kvg[:].unsqueeze(1).to_broadcast([128, 4, 128]), op=ALU.mult),
                    ins=[t1[:], kvg[:]], outs=[cKV[:, g * 4:(g + 1) * 4, :]])
                pbh = pb[:, (g % 2) * 512:(g % 2) * 512 + 512]
                P.op("pe", [lambda e, tt=tt, g=g, pbh=pbh: e.transpose(pbh[:, tt * 128:(tt + 1) * 128],
                                                                   cKV[:, g * 4 + tt, :], identb[:])
                            for tt in range(4)], ins=[cKV[:, g * 4:(g + 1) * 4, :], identb[:]], outs=[pbh])
                evac(cKVT[:, g * 512:(g + 1) * 512], pbh)
            dump(f"aKT{b}", aKT[:], [128, 4, S])
            dump(f"aV{b}", aV[:], [128, NT, 512])
            dump(f"cKV{b}", cKV[:], [128, NT, 128])
            dump(f"cKVT{b}", cKVT[:], [128, S])
            dump(f"iKT{b}", iKT[:], [128, S])
            checkpoint("p1")

            dma("pool", wQ[:, :, 0:512], wview(win_d, C_AQ, C_AQ + 512))
            dma("pool", wQ[:, :, 512:1024], wview(win_d, C_BQ, C_BQ + 512))
            dma("pool", wQ[:, :, 1024:1536], wview(win_d, C_IQ, C_IQ + 512))
            dma("pool", wQ[:, :, 1536:1544], wview(win_d, C_IW, C_IW + 8))
            P.op("pool", lambda e: e.memset(aQT[:], 0.0), outs=[aQT[:]])
            P.op("pool", lambda e: e.memset(iQT[:], 0.0), outs=[iQT[:]])
            for g in range(NG):
                T0 = 4 * g
                load_uT(b, g, xin2, uT2)
                for h in range(4):
                    proj_fm_split(wQ, h * 128, uT2, aQT[0:64, h, 0, :], aQT[64:128, h, 1, :])
                    proj_fm(wQ, 512 + h * 128, uT2, bQT[:, h, :])
                    proj_fm_split(wQ, 1024 + h * 128, uT2, iQT[0:64, 2 * h, :], iQT[64:128, 2 * h + 1, :])
                bk = bank()
                fns = []
                for tt in range(4):
                    for k in range(8):
                        fns.append(lambda e, k=k, bk=bk, tt=tt: e.matmul(
                            bk[:, tt * 8:(tt + 1) * 8], uT2[:, k, tt * 128:(tt + 1) * 128], wQ[:, k, 1536:1544],
                            start=(k == 0), stop=(k == 7)))
                P.op("pe", fns, ins=[uT2[:], wQ[:, :, 1536:1544]], outs=[bk[:, 0:32]])
                P.op("dve", lambda e, bk=bk: e.tensor_copy(out=iW[:].rearrange("p t h -> p (t h)"), in_=bk[:, 0:32]),
                     ins=[bk[:, 0:32]], outs=[iW[:]])
                for h in range(8):
                    bk = bank()
                    P.op("pe", lambda e, bk=bk, h=h: e.matmul(bk[:], wukT[:, h, :], bQT[:, h // 2, :],
                                                           start=True, stop=True),
                         ins=[wukT[:, h, :], bQT[:, h // 2, :]], outs=[bk[:]])
                    evac(qabsT[:, h, :], bk[:])
                if b == 0 and g == NG - 1:
                    dump("qabsT", qabsT[:], [128, 8, 512])
                    dump("iW", iW[:], [128, 4, 8])
                    checkpoint("p2a")
                mx, mn, rg, thr, cnt, ee, lo = [bis[:, i:i + 1] for i in range(7)]

                def index_gen(T0=T0, b=b, g=g):
                    scoreB = [score, score1]

                    def l_units(qt):
                        T = T0 + qt
                        nk = (T + 1) * 128
                        scb = scoreB[qt % 2]
                        units = []
                        for kc in range((nk + 511) // 512):
                            ncol = min(512, nk - kc * 512)
                            for h in range(8):
                                def unit(kc=kc, ncol=ncol, h=h, qt=qt, scb=scb):
                                    bk = pf[6 + h % 2]
                                    P.op("pe", lambda e: e.matmul(
                                        bk[:, 0:ncol], iQT[:, h, qt * 128:(qt + 1) * 128],
                                        iKT[:, kc * 512:kc * 512 + ncol], start=True, stop=True),
                                        ins=[iQT[:, h, qt * 128:(qt + 1) * 128], iKT[:, kc * 512:kc * 512 + ncol]],
                                        outs=[bk[:, 0:ncol]])
                                    r = rl[h % 2]
                                    P.op("act", lambda e: e.activation(out=r[:, 0:ncol], in_=bk[:, 0:ncol], func=AF.Relu),
                                         ins=[bk[:, 0:ncol]], outs=[r[:, 0:ncol]])
                                    sc = scb[:, kc * 512:kc * 512 + ncol]
                                    if h == 0:
                                        P.op("act", lambda e: e.activation(out=sc, in_=r[:, 0:ncol], func=AF.Identity,
                                                                           scale=iW[:, qt, 0:1]),
                                             ins=[r[:, 0:ncol], iW[:]], outs=[sc])
                                    else:
                                        P.op("act", lambda e: e.activation(out=r[:, 0:ncol], in_=r[:, 0:ncol], func=AF.Identity,
                                                                           scale=iW[:, qt, h:h + 1]),
                                             ins=[r[:, 0:ncol], iW[:]], outs=[r[:, 0:ncol]])
                                        P.op("pool", lambda e: e.tensor_tensor(out=sc, in0=sc, in1=r[:, 0:ncol], op=ALU.add),
                                             ins=[sc, r[:, 0:ncol]], outs=[sc])
                                units.append(unit)
                        return units

                    def bz_steps(qt):
                        T = T0 + qt
                        nk = (T + 1) * 128
                        sv = scoreB[qt % 2][:, 0:nk]
                        sd = scoreB[qt % 2][:, T * 128:(T + 1) * 128]
                        steps = []

                        def setup():
                            if T >= 2:
                                P.op("dve", lambda e: e.tensor_reduce(out=mx, in_=sv, axis=AX.X, op=ALU.max), ins=[sv], outs=[mx])
                                P.op("dve", lambda e: e.tensor_reduce(out=mn, in_=sv, axis=AX.X, op=ALU.min), ins=[sv], outs=[mn])
                            P.op("dve", lambda e: e.tensor_tensor(out=sd, in0=sd, in1=idxmask[:], op=ALU.add),
                                 ins=[sd, idxmask[:]], outs=[sd])
                            if T >= 2:
                                P.op("dve", lambda e: e.tensor_tensor(out=rg, in0=mx, in1=mn, op=ALU.subtract), ins=[mx, mn], outs=[rg])
                                P.op("dve", lambda e: e.tensor_scalar(out=wtab[:], in0=pow2[:], scalar1=rg, scalar2=None,
                                                                      op0=ALU.mult), ins=[pow2[:], rg], outs=[wtab[:]])
                                P.op("dve", lambda e: e.tensor_tensor(out=thr, in0=mn, in1=wtab[:, 0:1], op=ALU.add),
                                     ins=[mn, wtab[:, 0:1]], outs=[thr])
                        steps.append(setup)
                        if T >= 2:
                            for it in range(NIT):
                                def iteration(it=it):
                                    P.op("dve", lambda e: e.tensor_scalar(
                                        out=junk[:, 0:nk], in0=sv, scalar1=thr, scalar2=None, op0=ALU.is_ge, op1=ALU.add,
                                        accum_out=cnt), ins=[sv, thr], outs=[junk[:, 0:nk], cnt])
                                    P.op("dve", lambda e: e.tensor_scalar(out=ee, in0=cnt, scalar1=TOPK - 0.5, scalar2=0.5,
                                                                          op0=ALU.is_ge, op1=ALU.subtract), ins=[cnt], outs=[ee])
                                    P.op("dve", lambda e: e.scalar_tensor_tensor(out=thr, in0=ee, scalar=wtab[:, it:it + 1], in1=thr,
                                                                                 op0=ALU.mult, op1=ALU.add),
                                         ins=[ee, wtab[:, it:it + 1], thr], outs=[thr])
                                steps.append(iteration)

                        def final():
                            if T >= 2:
                                P.op("dve", lambda e: e.scalar_tensor_tensor(out=lo, in0=wtab[:, NIT:NIT + 1], scalar=-1.5, in1=thr,
                                                                             op0=ALU.mult, op1=ALU.add),
                                     ins=[wtab[:, NIT:NIT + 1], thr], outs=[lo])
                                P.op("dve", lambda e: e.tensor_scalar(out=maskQ[:, 0:nk], in0=sv, scalar1=lo, scalar2=NEG,
                                                                      op0=ALU.is_lt, op1=ALU.mult), ins=[sv, lo],
                                     outs=[maskQ[:, 0:nk]])
                            else:
                                P.op("dve", lambda e: e.tensor_scalar(out=maskQ[:, 0:nk], in0=sv, scalar1=-1e29, scalar2=NEG,
                                                                      op0=ALU.is_lt, op1=ALU.mult), ins=[sv],
                                     outs=[maskQ[:, 0:nk]])
                            for kb0 in range(0, T + 1, 4):
                                nb = min(4, T + 1 - kb0)
                                half = (kb0 // 4) % 2
                                pbh = pb[:, half * 512:half * 512 + nb * 128]
                                P.op("pe", [lambda e, i=i, kb0=kb0, pbh=pbh: e.transpose(
                                    pbh[:, i * 128:(i + 1) * 128], maskQ[:, (kb0 + i) * 128:(kb0 + i + 1) * 128], identb[:])
                                    for i in range(nb)], ins=[maskQ[:, kb0 * 128:(kb0 + nb) * 128], identb[:]], outs=[pbh])
                                mo = maskT[:, kb0:kb0 + nb, qt * 128:(qt + 1) * 128]
                                P.op("act", lambda e, mo=mo, pbh=pbh, nb=nb: e.activation(
                                    out=mo, in_=pbh.rearrange("p (n q) -> p n q", n=nb), func=AF.Copy), ins=[pbh], outs=[mo])
                        steps.append(final)
                        return steps

                    for u in l_units(0):
                        u()
                        yield
                    for qt in range(4):
                        nxt = l_units(qt + 1) if qt < 3 else []
                        steps = bz_steps(qt)
                        nu = 0
                        for i, st_ in enumerate(steps[:-1]):
                            st_()
                            den = max(1, (6 * (len(steps) - 1)) // 10)
                            want = (len(nxt) * (i + 1) + den - 1) // den
                            while nu < min(want, len(nxt)):
                                nxt[nu]()
                                nu += 1
                            yield
                        while nu < len(nxt):
                            nxt[nu]()
                            nu += 1
                            yield
                        steps[-1]()
                        yield

                blocks = [("off", kb, 0) for kb in range(T0)] + [("diag", T0 + j, j) for j in range(4)]
                nblk = len(blocks)
                gcol = slice(g * 512, (g + 1) * 512)
                passes = []
                for h in range(4):
                    for m in range(2):
                        passes.append(dict(kind="A", h=h, m=m, hh=h,
                                           qk_lhsT=lambda kb, h=h: aKT[:, h, kb * 128:(kb + 1) * 128],
                                           qk_rhs=lambda c0, h=h, m=m: aQT[:, h, m, c0:512],
                                           pv_lhsT=lambda kb, h=h: aV[:, kb, h * 128:(h + 1) * 128],
                                           masked=False))
                for h in range(8):
                    passes.append(dict(kind="B", h=h, hh=4 + h,
                                       qk_lhsT=lambda kb: cKVT[:, kb * 128:(kb + 1) * 128],
                                       qk_rhs=lambda c0, h=h: qabsT[:, h, c0:512],
                                       pv_lhsT=lambda kb: cKV[:, kb, :],
                                       masked=True))
                items = [(pi, bi) for pi in range(len(passes)) for bi in range(nblk)]
                NI = len(items)

                def geom(n):
                    pi, bi = items[n]
                    kind, kb, j = blocks[bi]
                    c0 = j * 128 if kind == "diag" else 0
                    a0 = c0 + 128 if kind == "diag" else 0
                    return pi, bi, passes[pi], kind, kb, j, c0, a0

                NA = 8 * nblk
                SB4 = [pf[0], pf[1], pf[6], pf[7]]

                def sbank(n):
                    return pf[n % 2] if n < NA else SB4[n % 4]

                def st_qk(n):
                    pi, bi, ps_, kind, kb, j, c0, a0 = geom(n)
                    sb_ = sbank(n)
                    hh = ps_["hh"]
                    kl, qr = ps_["qk_lhsT"](kb), ps_["qk_rhs"](c0)
                    msk = ps_["masked"]
                    dg_ = (kind == "diag")
                    more = msk or dg_
                    fns = [lambda e: e.matmul(sb_[:, c0:512], kl, qr, start=True, stop=(a0 >= 512 and not more))]
                    ins = [kl, qr]
                    if a0 < 512:
                        fns.append(lambda e: e.matmul(sb_[:, a0:512], augl[:], qaug[:, hh, a0:512], start=False, stop=not more))
                        ins += [augl[:], qaug[:, hh, a0:512]]
                    if dg_:
                        fns.append(lambda e: e.matmul(sb_[:, c0:c0 + 128], identb[:], dbias[:, 0, hh, :], start=False, stop=False))
                        fns.append(lambda e: e.matmul(sb_[:, c0:c0 + 128], identb[:], dbias[:, 1, hh, :], start=False,
                                                      stop=not msk))
                        ins += [identb[:], dbias[:, 0, hh, :], dbias[:, 1, hh, :]]
                    if msk:
                        mt_ = maskT[:, kb, c0:512]
                        fns.append(lambda e: e.matmul(sb_[:, c0:512], identb[:], mt_, start=False, stop=True))
                        ins += [identb[:], mt_]
                    P.op("pe", fns, ins=ins, outs=[sb_[:, c0:512]])

                def st_sm(n):
                    pi, bi, ps_, kind, kb, j, c0, a0 = geom(n)
                    sb_ = sbank(n)
                    hh = ps_["hh"]
                    pt = PT[n % 4]
                    if kind == "diag":
                        P.op("act", lambda e: e.activation(out=pt[:, c0:c0 + 128], in_=sb_[:, c0:c0 + 128], func=AF.Exp,
                                                           scale=0.125),
                             ins=[sb_[:, c0:c0 + 128]], outs=[pt[:, c0:c0 + 128]])
                    if a0 < 512:
                        dpi = (T0 - kb) + 3
                        P.op("act", lambda e: e.activation(
                            out=pt[:, a0:512], in_=sb_[:, a0:512], func=AF.Exp, scale=0.125,
                            bias=kbias[:, hh, dpi:dpi + 1]),
                            ins=[sb_[:, a0:512], kbias[:, hh, dpi:dpi + 1]], outs=[pt[:, a0:512]])

                def st_pv(n):
                    pi, bi, ps_, kind, kb, j, c0, a0 = geom(n)
                    src = PT[n % 4]
                    OA, RS = pf[2 + pi % 2], pf[4 + pi % 2]
                    first, last = (bi == 0), (bi == nblk - 1)
                    vl = ps_["pv_lhsT"](kb)
                    P.op("pe", [lambda e: e.matmul(OA[:, c0:512], vl, src[:, c0:512], start=first, stop=last),
                                lambda e: e.matmul(RS[:, c0:512], onesb[:], src[:, c0:512], start=first, stop=last)],
                         ins=[vl, src[:, c0:512], onesb[:]], outs=[OA[:, c0:512], RS[:, c0:512]])

                pending = []

                def fin_a(pi, n):
                    ps_ = passes[pi]
                    OA, RS = pf[2 + pi % 2], pf[4 + pi % 2]
                    P.op("act", lambda e: e.activation(out=rinv[:], in_=RS[:], func=AF.Ln), ins=[RS[:]], outs=[rinv[:]])
                    P.op("act", lambda e: e.activation(out=rinv[:], in_=rinv[:], func=AF.Exp, scale=-1.0),
                         ins=[rinv[:]], outs=[rinv[:]])
                    if ps_["kind"] == "A":
                        h, m = ps_["h"], ps_["m"]
                        om = o01[m]
                        od = o01[0]
                        dfr = 2 if nblk >= 8 else 0
                        sqb = o01[1] if dfr else rinv

                        def fin_dve():
                            P.op("dve", lambda e: e.tensor_tensor(out=om[:], in0=OA[:], in1=rinv[:], op=ALU.mult),
                                 ins=[OA[:], rinv[:]], outs=[om[:]])
                            if m == 1:
                                P.op("dve", lambda e: e.scalar_tensor_tensor(out=od[:], in0=o01[1][:], scalar=lamneg[:, 0:1],
                                                                             in1=od[:], op0=ALU.mult, op1=ALU.add),
                                     ins=[o01[1][:], lamneg[:], od[:]], outs=[od[:]])
                                P.op("act", lambda e: e.activation(out=sqb[:], in_=od[:], func=AF.Square),
                                     ins=[od[:]], outs=[sqb[:]])
                        if dfr:
                            pending.append((n + dfr, fin_dve))
                        else:
                            fin_dve()
                        if m == 1:
                            def fin_pe():
                                P.op("pe", lambda e: e.matmul(RS[:], onesf[:], sqb[:], start=True, stop=True),
                                     ins=[onesf[:], sqb[:]], outs=[RS[:]])

                            def fin_b():
                                P.op("act", lambda e: e.activation(out=sqb[:], in_=RS[:], func=AF.Ln, scale=1.0 / 128.0,
                                                                   bias=epsc[:, 0:1]),
                                     ins=[RS[:], epsc[:]], outs=[sqb[:]])
                                P.op("act", lambda e: e.activation(out=sqb[:], in_=sqb[:], func=AF.Exp, scale=-0.5),
                                     ins=[sqb[:]], outs=[sqb[:]])
                                yo = yAT[:, h, gcol]
                                P.op("dve", lambda e: e.scalar_tensor_tensor(out=yo, in0=od[:], scalar=gsc[:, 0:1], in1=sqb[:],
                                                                             op0=ALU.mult, op1=ALU.mult),
                                     ins=[od[:], gsc[:], sqb[:]], outs=[yo])
                            pending.append((n + 2 + dfr, fin_pe))
                            pending.append((n + 3 + dfr, fin_b))
                    else:
                        h = ps_["h"]
                        ot = olT[h % 2]
                        P.op("dve", lambda e: e.tensor_tensor(out=ot[:], in0=OA[:], in1=rinv[:], op=ALU.mult),
                             ins=[OA[:], rinv[:]], outs=[ot[:]])

                        def fin_pe():
                            P.op("pe", lambda e: e.matmul(RS[:], wuv[:, h, :], ot[:], start=True, stop=True),
                                 ins=[wuv[:, h, :], ot[:]], outs=[RS[:]])
                        pending.append((n + 2, fin_pe))

                        def fin_b():
                            p0 = (h % 2) * 64
                            yo = yBT[p0:p0 + 64, h // 2, gcol]
                            src_ = RS[p0:p0 + 64, :]
                            if h % 2 == 0:
                                P.op("act", lambda e: e.activation(out=yo, in_=src_, func=AF.Copy), ins=[src_], outs=[yo])
                            else:
                                P.op("dve", lambda e: e.tensor_copy(out=yo, in_=src_), ins=[src_], outs=[yo])
                        pending.append((n + 3, fin_b))

                def attn_gen():
                    st_qk(0)
                    nq = 1
                    for n in range(NI):
                        tgt = min(NI - 1, n + (3 if n >= NA else 1))
                        while nq <= tgt:
                            st_qk(nq)
                            nq += 1
                        st_sm(n)
                        st_pv(n)
                        if items[n][1] == nblk - 1:
                            fin_a(items[n][0], n)
                        while pending and pending[0][0] <= n:
                            pending.pop(0)[1]()
                        yield
                    while pending:
                        pending.pop(0)[1]()

                ig, ag = index_gen(), attn_gen()
                NY = 8 * (((T0 + 1) * 128 + 511) // 512) + sum((NIT + 2) if T0 + qt >= 2 else 2 for qt in range(4))
                ia = iy = 0
                for _ in ig:
                    iy += 1
                    while ia < NA - 1 and ia * NY < iy * NA:
                        next(ag)
                        ia += 1
                while ia < NA:
                    next(ag)
                    ia += 1
                for _ in ag:
                    pass
            dump(f"yAT{b}", yAT[:], [128, 4, S])
            dump(f"yBT{b}", yBT[:], [128, 4, S])
            checkpoint("p2")

            for blk in range(NBLK):
                dma("pool", wap[:], wap_d.rearrange("(k p) f -> p k f", p=128))
                dma("pool", wbp[:], wbp_d.rearrange("(k p) f -> p k f", p=128))
                for gl in range(2):
                    load_uT(b, blk * 2 + gl, xin3, uT3, ucol0=gl * 512)
                tok0 = blk * 1024
                def load_wgt(j):
                    wj = wgt[j % 2]
                    dma("pool", wj[:, :, 0, :], wview(win_d, C_G + j * 128, C_G + (j + 1) * 128))
                    dma("pool", wj[:, :, 1, :], wview(win_d, C_G + 1024 + j * 128, C_G + 1024 + (j + 1) * 128))

                load_wgt(0)
                load_wgt(1)
                for j in range(8):
                    wj = wgt[j % 2]
                    for gl in range(2):
                        gs = []
                        for gi in range(2):
                            bk = bank()
                            P.op("pe", [lambda e, k=k, bk=bk, wj=wj, gi=gi, gl=gl: e.matmul(
                                bk[:], wj[:, k, gi, :], uT3[:, k, gl * 512:(gl + 1) * 512], start=(k == 0), stop=(k == 7))
                                for k in range(8)], ins=[wj[:, :, gi, :], uT3[:, :, gl * 512:(gl + 1) * 512]], outs=[bk[:]])
                            gt = g01[(gl * 2 + gi) % 4]
                            P.op("act", lambda e, gt=gt, bk=bk: e.activation(out=gt[:], in_=bk[:], func=AF.Sigmoid),
                                 ins=[bk[:]], outs=[gt[:]])
                            gs.append(gt)
                        ts_ = []
                        for gi, (wp, yT) in enumerate(((wap, yAT), (wbp, yBT))):
                            bk = bank()
                            c_lo = tok0 + gl * 512
                            P.op("pe", [lambda e, c=c, bk=bk, wp=wp, yT=yT, c_lo=c_lo, j=j: e.matmul(
                                bk[:], wp[:, c, j * 128:(j + 1) * 128], yT[:, c, c_lo:c_lo + 512], start=(c == 0), stop=(c == 3))
                                for c in range(4)], ins=[wp[:, :, j * 128:(j + 1) * 128], yT[:, :, c_lo:c_lo + 512]],
                                outs=[bk[:]])
                            tt_ = tt01[gi]
                            P.op("dve", lambda e, tt_=tt_, bk=bk, gt=gs[gi]: e.tensor_tensor(out=tt_[:], in0=bk[:], in1=gt[:],
                                                                                          op=ALU.mult),
                                 ins=[bk[:], gs[gi][:]], outs=[tt_[:]])
                            ts_.append(tt_)
                        mo = mixedT[:, j, gl * 512:(gl + 1) * 512]
                        P.op("pool", lambda e, mo=mo, a=ts_[0], c=ts_[1]: e.tensor_tensor(out=mo, in0=a[:], in1=c[:], op=ALU.add),
                             ins=[ts_[0][:], ts_[1][:]], outs=[mo])
                    if j + 2 < 8:
                        load_wgt(j + 2)
                if b == 0 and blk == 0:
                    dump("mixedT", mixedT[:], [128, 8, 1024])
                dma("pool", wo[:], wo_d.rearrange("(k p) f -> p k f", p=128))
                dma("sp", lnv[:], lnbc_d[:, 0:2, :])
                for gi, ch0 in enumerate((16, 40)):
                    for half in range(2):
                        bk = bank()
                        for jj in range(4):
                            j = half * 4 + jj
                            dgt = dg[jj % 2]
                            P.op("dve", lambda e, dgt=dgt, j=j, ch0=ch0: e.tensor_scalar(
                                out=dgt[:], in0=identf[:], scalar1=modT[:, ch0 + j, b:b + 1], scalar2=None, op0=ALU.mult),
                                ins=[identf[:], modT[:]], outs=[dgt[:]])
                            P.op("pe", lambda e, dgt=dgt, bk=bk, jj=jj: e.matmul(bk[:, jj * 128:(jj + 1) * 128], onesf[:], dgt[:],
                                                                              start=True, stop=True),
                                 ins=[onesf[:], dgt[:]], outs=[bk[:, jj * 128:(jj + 1) * 128]])
                        evac(gatebc[:, gi, half * 512:(half + 1) * 512], bk[:])
                if blk == 0:
                    dump(f"gatebc{b}", gatebc[:], [128, 2, D])
                def layer_norm(src, gidx, out_ap, scratch, st):
                    s1, s2, mean, msq, var = st[0], st[1], st[2], st[3], st[4]
                    P.op("act", lambda e: e.activation(out=scratch, in_=src, func=AF.Copy, accum_out=s1),
                         ins=[src], outs=[scratch, s1])
                    P.op("act", lambda e: e.activation(out=scratch, in_=src, func=AF.Square, accum_out=s2),
                         ins=[src], outs=[scratch, s2])
                    yield
                    P.op("dve", lambda e: e.tensor_scalar(out=mean, in0=s1, scalar1=1.0 / D, scalar2=None, op0=ALU.mult),
                         ins=[s1], outs=[mean])
                    P.op("dve", lambda e: e.tensor_tensor(out=msq, in0=mean, in1=mean, op=ALU.mult), ins=[mean], outs=[msq])
                    P.op("dve", lambda e: e.scalar_tensor_tensor(out=var, in0=s2, scalar=1.0 / D, in1=msq, op0=ALU.mult,
                                                                 op1=ALU.subtract), ins=[s2, msq], outs=[var])
                    P.op("dve", lambda e: e.tensor_scalar(out=var, in0=var, scalar1=LN_EPS, scalar2=None, op0=ALU.add),
                         ins=[var], outs=[var])
                    P.op("act", lambda e: e.activation(out=var, in_=var, func=AF.Sqrt), ins=[var], outs=[var])
                    yield
                    P.op("dve", lambda e: e.reciprocal(out=var, in_=var), ins=[var], outs=[var])
                    P.op("dve", lambda e: e.tensor_scalar(out=src, in0=src, scalar1=mean, scalar2=var, op0=ALU.subtract,
                                                          op1=ALU.mult), ins=[src, mean, var], outs=[src])
                    P.op("dve", lambda e: e.tensor_tensor(out=src, in0=src, in1=lnv[:, 0, :], op=ALU.mult),
                         ins=[src, lnv[:, 0, :]], outs=[src])
                    P.op("pool", lambda e: e.tensor_tensor(out=out_ap, in0=src, in1=lnv[:, 1, :], op=ALU.add),
                         ins=[src, lnv[:, 1, :]], outs=[out_ap])
                    yield

                def tile_gen(t, b=b, tok0=tok0):
                    slot = t % NSLOT
                    row0 = b * S + tok0 + t * 128
                    xr = xrow[slot]
                    rt_ = r1[slot]
                    u2f = u2fs[slot]
                    rtr = rtrs[slot]
                    st = [small[:, 16 + 8 * slot + i:17 + 8 * slot + i] for i in range(8)]
                    dma("sp", xr[:], x_d[row0:row0 + 128, :])
                    for half in range(2):
                        bk = bank()
                        cs = slice(half * 512, (half + 1) * 512)
                        P.op("pe", [lambda e, k=k, bk=bk, t=t, cs=cs: e.matmul(
                            bk[:], mixedT[:, k, t * 128:(t + 1) * 128], wo[:, k, cs], start=(k == 0), stop=(k == 7))
                            for k in range(8)], ins=[mixedT[:, :, t * 128:(t + 1) * 128], wo[:, :, cs]], outs=[bk[:]])
                        P.op("dve", lambda e, bk=bk, rt_=rt_, cs=cs: e.tensor_tensor(out=rt_[:, cs], in0=bk[:],
                                                                                  in1=gatebc[:, 0, cs], op=ALU.mult),
                             ins=[bk[:], gatebc[:, 0, cs]], outs=[rt_[:, cs]])
                    P.op("dve", lambda e, xr=xr, rt_=rt_: e.scalar_tensor_tensor(out=rt_[:], in0=xr[:], scalar=ALPHA, in1=rt_[:],
                                                                                op0=ALU.mult, op1=ALU.add),
                         ins=[xr[:], rt_[:]], outs=[rt_[:]])
                    yield
                    yield from layer_norm(rt_[:], 0, x1blk[:, t, :], xr[:], st)
                    for kh in range(2):
                        bk = bank()
                        P.op("pe", [lambda e, kk=kk, bk=bk, t=t, kh=kh: e.transpose(
                            bk[:, kk * 128:(kk + 1) * 128], x1blk[:, t, (kh * 4 + kk) * 128:(kh * 4 + kk + 1) * 128], identf[:])
                            for kk in range(4)], ins=[x1blk[:, t, kh * 512:(kh + 1) * 512], identf[:]], outs=[bk[:]])
                        for kk in range(4):
                            k = kh * 4 + kk
                            P.op("act", lambda e, bk=bk, kk=kk, k=k: e.activation(
                                out=u2f[:, k, :], in_=bk[:, kk * 128:(kk + 1) * 128], func=AF.Identity,
                                bias=modT[:, 24 + k, b:b + 1], scale=modT[:, 32 + k, b:b + 1]),
                                ins=[bk[:, kk * 128:(kk + 1) * 128], modT[:]], outs=[u2f[:, k, :]])
                    P.op("pool", lambda e, t=t: e.tensor_copy(out=u2T[:, :, t * 128:(t + 1) * 128], in_=u2f[:]),
                         ins=[u2f[:]], outs=[u2T[:, :, t * 128:(t + 1) * 128]])
                    bk = bank()
                    P.op("pe", [lambda e, k=k, bk=bk: e.matmul(bk[:, 0:20], u2f[:, k, :], wrt[:, k, :], start=(k == 0),
                                                              stop=(k == 7)) for k in range(8)],
                         ins=[u2f[:], wrt[:]], outs=[bk[:, 0:20]])
                    yield
                    lg = rtr[:, 0:20]
                    GL, EL = rtr[:, 0:4], rtr[:, 4:20]
                    gmax, ngmax, gsum, gtop = rtr[:, 20:21], rtr[:, 21:22], rtr[:, 22:23], rtr[:, 23:24]
                    ohg, gexp = rtr[:, 24:28], rtr[:, 28:32]
                    t44, els, oh1, els2, oh2 = rtr[:, 32:48], rtr[:, 48:52], rtr[:, 52:56], rtr[:, 56:60], rtr[:, 60:64]
                    m1, m2, dd, ex, w1, w2 = [rtr[:, 64 + i:65 + i] for i in range(6)]
                    we = rtr[:, 72:76]
                    P.op("dve", lambda e, bk=bk: e.tensor_tensor(out=lg, in0=bk[:, 0:20], in1=brt[:], op=ALU.add),
                         ins=[bk[:, 0:20], brt[:]], outs=[lg])
                    P.op("dve", lambda e: e.tensor_reduce(out=gmax, in_=GL, axis=AX.X, op=ALU.max), ins=[GL], outs=[gmax])
                    P.op("dve", lambda e: e.tensor_scalar(out=ohg, in0=GL, scalar1=gmax, scalar2=None, op0=ALU.is_ge),
                         ins=[GL, gmax], outs=[ohg])
                    P.op("dve", lambda e: e.tensor_scalar(out=ngmax, in0=gmax, scalar1=-1.0, scalar2=None, op0=ALU.mult),
                         ins=[gmax], outs=[ngmax])
                    P.op("act", lambda e: e.activation(out=gexp, in_=GL, func=AF.Exp, bias=ngmax, scale=1.0, accum_out=gsum),
                         ins=[GL, ngmax], outs=[gexp, gsum])
                    yield
                    P.op("dve", lambda e: e.reciprocal(out=gtop, in_=gsum), ins=[gsum], outs=[gtop])
                    P.op("dve", lambda e: e.tensor_tensor(
                        out=t44.rearrange("p (g e) -> p g e", g=4), in0=EL.rearrange("p (g e) -> p g e", g=4),
                        in1=ohg.unsqueeze(2).to_broadcast([128, 4, 4]), op=ALU.mult), ins=[EL, ohg], outs=[t44])
                    P.op("dve", lambda e: e.tensor_reduce(out=els, in_=t44.rearrange("p (g e) -> p e g", g=4), axis=AX.X,
                                                          op=ALU.add), ins=[t44], outs=[els])
                    P.op("dve", lambda e: e.tensor_reduce(out=m1, in_=els, axis=AX.X, op=ALU.max), ins=[els], outs=[m1])
                    P.op("dve", lambda e: e.tensor_scalar(out=oh1, in0=els, scalar1=m1, scalar2=None, op0=ALU.is_ge),
                         ins=[els, m1], outs=[oh1])
                    P.op("dve", lambda e: e.scalar_tensor_tensor(out=els2, in0=oh1, scalar=-1e30, in1=els, op0=ALU.mult,
                                                                 op1=ALU.add), ins=[oh1, els], outs=[els2])
                    P.op("dve", lambda e: e.tensor_reduce(out=m2, in_=els2, axis=AX.X, op=ALU.max), ins=[els2], outs=[m2])
                    P.op("dve", lambda e: e.tensor_scalar(out=oh2, in0=els2, scalar1=m2, scalar2=None, op0=ALU.is_ge),
                         ins=[els2, m2], outs=[oh2])
                    P.op("dve", lambda e: e.tensor_tensor(out=dd, in0=m2, in1=m1, op=ALU.subtract), ins=[m2, m1], outs=[dd])
                    P.op("act", lambda e: e.activation(out=ex, in_=dd, func=AF.Exp), ins=[dd], outs=[ex])
                    P.op("dve", lambda e: e.tensor_scalar(out=w1, in0=ex, scalar1=1.0, scalar2=None, op0=ALU.add),
                         ins=[ex], outs=[w1])
                    P.op("dve", lambda e: e.reciprocal(out=w1, in_=w1), ins=[w1], outs=[w1])
                    P.op("dve", lambda e: e.tensor_tensor(out=w2, in0=ex, in1=w1, op=ALU.mult), ins=[ex, w1], outs=[w2])
                    P.op("dve", lambda e: e.tensor_scalar(out=we, in0=oh1, scalar1=w1, scalar2=None, op0=ALU.mult),
                         ins=[oh1, w1], outs=[we])
                    P.op("dve", lambda e: e.scalar_tensor_tensor(out=we, in0=oh2, scalar=w2, in1=we, op0=ALU.mult, op1=ALU.add),
                         ins=[oh2, w2, we], outs=[we])
                    P.op("dve", lambda e: e.tensor_scalar(out=we, in0=we, scalar1=gtop, scalar2=None, op0=ALU.mult),
                         ins=[we, gtop], outs=[we])
                    P.op("dve", lambda e, t=t: e.tensor_tensor(
                        out=comb[:, t, :].rearrange("p (g e) -> p g e", g=4),
                        in0=ohg.unsqueeze(2).to_broadcast([128, 4, 4]),
                        in1=we.unsqueeze(1).to_broadcast([128, 4, 4]), op=ALU.mult), ins=[ohg, we], outs=[comb[:, t, :]])
                    yield

                tgens = [tile_gen(t) for t in range(8)]
                active = []
                nstart = 0
                while nstart < 8 or active:
                    if nstart < 8 and len(active) < NSLOT:
                        active.append(tgens[nstart])
                        nstart += 1
                    for tg in list(active):
                        try:
                            next(tg)
                        except StopIteration:
                            active.remove(tg)
                if b == 0 and blk == 0:
                    dump("x1blk", x1blk[:], [128, 8, D])
                    dump("comb", comb[:], [128, 8, 16])
                    dump("u2T", u2T[:], [128, 8, 1024])
                    checkpoint("p3")

                def moe_gu(ex_):
                    wg_, wu_, wd_ = wg[ex_ % 2], wu[ex_ % 2], wd[ex_ % 2]
                    dma("pool", wg_[:], weg_d[ex_].rearrange("(k p) f -> p k f", p=128))
                    dma("pool", wu_[:], weu_d[ex_].rearrange("(k p) f -> p k f", p=128))
                    dma("pool", wd_[:], wed_d[ex_].rearrange("(k p) f -> p k f", p=128))
                    hT_ = hT[ex_ % 2]
                    for gl in range(2):
                        for fh in range(2):
                            bg, bu = bank(), bank()
                            for (bk, w_) in ((bg, wg_), (bu, wu_)):
                                P.op("pe", [lambda e, k=k, bk=bk, w_=w_, fh=fh, gl=gl: e.matmul(
                                    bk[:], w_[:, k, fh * 128:(fh + 1) * 128], u2T[:, k, gl * 512:(gl + 1) * 512],
                                    start=(k == 0), stop=(k == 7)) for k in range(8)],
                                    ins=[w_[:, :, fh * 128:(fh + 1) * 128], u2T[:, :, gl * 512:(gl + 1) * 512]], outs=[bk[:]])
                            s_ = sg[(gl * 2 + fh) % 2]
                            P.op("act", lambda e, s_=s_, bg=bg: e.activation(out=s_[:], in_=bg[:], func=AF.Silu),
                                 ins=[bg[:]], outs=[s_[:]])
                            ho = hT_[:, fh, gl * 512:(gl + 1) * 512]
                            P.op("dve", lambda e, ho=ho, s_=s_, bu=bu: e.tensor_tensor(out=ho, in0=bu[:], in1=s_[:], op=ALU.mult),
                                 ins=[bu[:], s_[:]], outs=[ho])

                def moe_dn(ex_):
                    wd_, hT_ = wd[ex_ % 2], hT[ex_ % 2]
                    for t in range(8):
                        for half in range(2):
                            bk = bank()
                            cs = slice(half * 512, (half + 1) * 512)
                            P.op("pe", [lambda e, fh=fh, bk=bk, t=t, cs=cs: e.matmul(
                                bk[:], hT_[:, fh, t * 128:(t + 1) * 128], wd_[:, fh, cs], start=(fh == 0), stop=(fh == 1))
                                for fh in range(2)], ins=[hT_[:, :, t * 128:(t + 1) * 128], wd_[:, :, cs]], outs=[bk[:]])
                            ya = yacc[:, t, cs]
                            if ex_ == 0:
                                P.op("dve", lambda e, ya=ya, bk=bk, t=t: e.tensor_scalar(
                                    out=ya, in0=bk[:], scalar1=comb[:, t, 0:1], scalar2=None, op0=ALU.mult),
                                    ins=[bk[:], comb[:, t, 0:1]], outs=[ya])
                            else:
                                P.op("dve", lambda e, ya=ya, bk=bk, t=t, ex_=ex_: e.scalar_tensor_tensor(
                                    out=ya, in0=bk[:], scalar=comb[:, t, ex_:ex_ + 1], in1=ya, op0=ALU.mult, op1=ALU.add),
                                    ins=[bk[:], comb[:, t, ex_:ex_ + 1], ya], outs=[ya])

                moe_gu(0)
                for ex_ in range(16):
                    if ex_ + 1 < 16:
                        moe_gu(ex_ + 1)
                    moe_dn(ex_)
                if b == 0 and blk == 0:
                    dump("yacc", yacc[:], [128, 8, D])
                dma("sp", lnv[:], lnbc_d[:, 2:4, :])
                def ln2_gen(t, b=b, tok0=tok0):
                    row0 = b * S + tok0 + t * 128
                    r_ = yacc[:, t, :]
                    ot = otile[t % 2]
                    st2 = [small[:, 40 + 8 * (t % 2) + i:41 + 8 * (t % 2) + i] for i in range(8)]
                    P.op("dve", lambda e: e.tensor_tensor(out=r_, in0=r_, in1=gatebc[:, 1, :], op=ALU.mult),
                         ins=[r_, gatebc[:, 1, :]], outs=[r_])
                    P.op("dve", lambda e: e.scalar_tensor_tensor(out=r_, in0=x1blk[:, t, :], scalar=ALPHA,
                                                                 in1=r_, op0=ALU.mult, op1=ALU.add),
                         ins=[x1blk[:, t, :], r_], outs=[r_])
                    yield
                    yield from layer_norm(r_, 2, ot[:], ot[:], st2)
                    fin = dma("sp", y_d[row0:row0 + 128, :], ot[:])
                    finals.append(fin)

                lgens = [ln2_gen(t) for t in range(8)]
                active = []
                nstart = 0
                while nstart < 8 or active:
                    if nstart < 8 and len(active) < 2:
                        active.append(lgens[nstart])
                        nstart += 1
                    for tg in list(active):
                        try:
                            next(tg)
                        except StopIteration:
                            active.remove(tg)
    except _Stop:
        pass
    P.emit(final_wait_ops=finals + list(dumps.values()))
    return nc, P, mem_report


finals = []


def prep_inputs(inputs, core, NB=2, S=2048):
    f = lambda a: np.ascontiguousarray(np.asarray(a, dtype=np.float32))
    b0 = core * NB
    x = f(inputs["x"])[b0:b0 + NB, :S].reshape(NB * S, D)
    c = f(inputs["c"])[b0:b0 + NB]
    cT = f(c.T.reshape(8, 128, NB).transpose(1, 0, 2))
    ada_b = f(inputs["ada_b"])[0]
    rep = lambda v: f(np.broadcast_to(np.asarray(v, np.float32)[None, :], (128, len(v))))
    w_in = f(inputs["w_in"])[0]
    w_uk = f(inputs["w_uk"])[0]
    w_uv = f(inputs["w_uv"])[0]
    w_ukTz = np.zeros((128, 8, 128), np.float32)
    w_uvz = np.zeros((128, 8, 128), np.float32)
    for h in range(8):
        w_ukTz[(h % 2) * 64:(h % 2) * 64 + 64, h, :] = w_uk[:, h, :].T
        w_uvz[:, h, (h % 2) * 64:(h % 2) * 64 + 64] = w_uv[:, h, :]
    d = dict(
        x=x, cT=cT, ada_w=f(inputs["ada_w"])[0], ada_bT=f(ada_b.reshape(48, 128).T),
        w_in=w_in, w_ik2=f(np.concatenate([w_in[:, C_IK:C_IK + 64], w_in[:, C_IK:C_IK + 64]], 1)),
        lamA=f(np.stack([rep(f(inputs["lambda_q1"])[0]), rep(f(inputs["lambda_q2"])[0])], 1)),
        lamB=f(np.stack([rep(f(inputs["lambda_k1"])[0]), rep(f(inputs["lambda_k2"])[0])], 1)),
        asub=f(f(inputs["a_subln_g"])[0].reshape(128, 1)),
        kvg_bc=rep(f(inputs["kv_norm_g"])[0]),
        w_ukTz=w_ukTz, w_uvz=w_uvz,
        w_a_proj=f(inputs["w_a_proj"])[0], w_b_proj=f(inputs["w_b_proj"])[0], w_o=f(inputs["w_o"])[0],
        ln_bc=f(np.stack([rep(f(inputs["ln1_g"])[0]), rep(f(inputs["ln1_b"])[0]),
                          rep(f(inputs["ln2_g"])[0]), rep(f(inputs["ln2_b"])[0])], 1)),
        w_router=f(np.concatenate([f(inputs["w_group"])[0], f(inputs["w_expert_router"])[0]], 1)),
        b_router_bc=rep(np.concatenate([f(inputs["b_group"])[0], f(inputs["b_expert_router"])[0]])),
        w_exp_gate=f(inputs["w_exp_gate"])[0], w_exp_up=f(inputs["w_exp_up"])[0], w_exp_down=f(inputs["w_exp_down"])[0],
    )
    d.update(host_constants())
    return d


_CACHE = {}


def kernel(**inputs):
    NB, S = 2, 2048
    if "nc" not in _CACHE:
        finals.clear()
        _CACHE["nc"] = build(NB, S)[0]
    nc = _CACHE["nc"]
    in_maps = [prep_inputs(inputs, c, NB, S) for c in range(8)]
    res = run_bass_kernel_spmd(nc, in_maps, core_ids=list(range(8)))
    out = np.concatenate([np.asarray(r["y"], dtype=np.float32).reshape(NB, S, D) for r in res.results], axis=0)
    return out
```

```python
import math
import types
import numpy as np
import ml_dtypes
import concourse.bass as bass
import concourse.mybir as mybir
from concourse.bass_utils import run_bass_kernel_spmd

F32 = mybir.dt.float32
BF16 = mybir.dt.bfloat16
ALU = mybir.AluOpType
AF = mybir.ActivationFunctionType
AX = mybir.AxisListType

SEM_WRAP = 30000
N_DMA_SLOTS = 16
CELL = 32
ENGS = ("pe", "act", "dve", "pool", "sp")
EIDX = {e: i for i, e in enumerate(ENGS)}


def _esize(dt):
    return {F32: 4, BF16: 2}.get(dt, 4)


def _freeze(fn):
    if not getattr(fn, "__closure__", None):
        return fn
    cells = []
    for c in fn.__closure__:
        try:
            cells.append(types.CellType(c.cell_contents))
        except ValueError:
            cells.append(c)
    g = types.FunctionType(fn.__code__, fn.__globals__, fn.__name__, fn.__defaults__, tuple(cells))
    g.__kwdefaults__ = fn.__kwdefaults__
    return g


class Prog:
    def __init__(self, nc):
        self.nc = nc
        self.ops = []
        self.reg = {}
        self.sb_top = 16512
        self.sb_limit = 229344
        self.spaces = {}
        self.seq = {e: 0 for e in ENGS}
        self.dma_reads = []
        self._space("sb", nc.SBUF_PARTITION_SIZE_BYTES)

    def _space(self, name, nbytes):
        n = (nbytes + CELL - 1) // CELL
        self.spaces[name] = dict(
            lw=np.full((len(ENGS), n), -1, np.int64),
            lr=np.full((len(ENGS), n), -1, np.int64),
            dw=np.full(n, -1, np.int64),
        )

    def sb(self, name, shape, dtype, offset=None):
        es = _esize(dtype)
        nbytes = int(np.prod(shape[1:])) * es
        if offset is None:
            offset = (self.sb_top + 63) // 64 * 64
            self.sb_top = offset + nbytes
        assert offset + nbytes <= self.sb_limit, (name, offset, nbytes)
        h = self.nc.alloc_sbuf_tensor_at(name, list(shape), dtype, offset=offset)
        self.reg[h.name] = ("sb", offset, es, int(np.prod(shape[1:])))
        return h

    def ps(self, name, shape, dtype=F32):
        es = _esize(dtype)
        h = self.nc.alloc_psum_tensor(name, list(shape), dtype)
        sp = "ps:" + h.name
        self._space(sp, int(np.prod(shape[1:])) * es)
        self.reg[h.name] = (sp, 0, es, int(np.prod(shape[1:])))
        return h

    def _ranges(self, ap):
        r = self.reg.get(ap.name)
        if r is None:
            return None
        space, base, es, row = r
        if space.startswith("ps:"):
            return space, [(0, (row * es + CELL - 1) // CELL)]
        off = int(ap.offset) % row
        dims = [(int(s), int(c)) for (s, c) in ap.ap[1:]]
        runs = []
        if len(dims) == 0:
            runs = [(off, off + 1)]
        else:
            inner = dims[-1]
            ext = abs(inner[0]) * (inner[1] - 1) + 1
            outer = dims[:-1]
            n_outer = int(np.prod([c for _, c in outer])) if outer else 1
            if n_outer > 64:
                lo = off
                hi = off + 1
                for s, c in dims:
                    if s >= 0:
                        hi += s * (c - 1)
                    else:
                        lo += s * (c - 1)
                runs = [(lo, hi)]
            else:
                offs = [off]
                for s, c in outer:
                    offs = [o + i * s for o in offs for i in range(c)]
                for o in offs:
                    if inner[0] >= 0:
                        runs.append((o, o + ext))
                    else:
                        runs.append((o - ext + 1, o + 1))
        out = []
        for lo, hi in runs:
            out.append(((base + lo * es) // CELL, (base + hi * es + CELL - 1) // CELL))
        out.sort()
        merged = []
        for lo, hi in out:
            if merged and lo <= merged[-1][1]:
                merged[-1] = (merged[-1][0], max(merged[-1][1], hi))
            else:
                merged.append((lo, hi))
        return space, merged

    def op(self, eng, fns, ins=(), outs=(), dma=False):
        if callable(fns):
            fns = [fns]
        fns = [_freeze(f) for f in fns]
        oid = len(self.ops)
        ei = EIDX[eng]
        waits = {}
        dwaits = set()
        rd = [self._ranges(a) for a in ins]
        wr = [self._ranges(a) for a in outs]

        def need(e_i, s):
            if s >= 0 and waits.get(e_i, -1) < s:
                waits[e_i] = s

        for fp in rd:
            if fp is None:
                continue
            st = self.spaces[fp[0]]
            for lo, hi in fp[1]:
                m = st["lw"][:, lo:hi].max(axis=1)
                for e_i in range(len(ENGS)):
                    need(e_i, int(m[e_i]))
                d = st["dw"][lo:hi]
                if (d >= 0).any():
                    dwaits.update(int(v) for v in np.unique(d[d >= 0]))
        for fp in wr:
            if fp is None:
                continue
            st = self.spaces[fp[0]]
            for lo, hi in fp[1]:
                m = np.maximum(st["lw"][:, lo:hi].max(axis=1), st["lr"][:, lo:hi].max(axis=1))
                for e_i in range(len(ENGS)):
                    need(e_i, int(m[e_i]))
                d = st["dw"][lo:hi]
                if (d >= 0).any():
                    dwaits.update(int(v) for v in np.unique(d[d >= 0]))
                if self.dma_reads:
                    keep = []
                    for (did, dsp, dlo, dhi) in self.dma_reads:
                        if dsp == fp[0] and dlo < hi and lo < dhi:
                            dwaits.add(did)
                        else:
                            keep.append((did, dsp, dlo, dhi))
                    self.dma_reads = keep
        if dma:
            for fp in rd:
                if fp is None:
                    continue
                for lo, hi in fp[1]:
                    self.dma_reads.append((oid, fp[0], lo, hi))
            for fp in wr:
                if fp is None:
                    continue
                st = self.spaces[fp[0]]
                for lo, hi in fp[1]:
                    st["dw"][lo:hi] = oid
                    st["lw"][:, lo:hi] = -1
                    st["lr"][:, lo:hi] = -1
            seq = None
        else:
            seq = self.seq[eng]
            self.seq[eng] += 1
            for fp in rd:
                if fp is None:
                    continue
                st = self.spaces[fp[0]]
                for lo, hi in fp[1]:
                    st["lr"][ei, lo:hi] = seq
            for fp in wr:
                if fp is None:
                    continue
                st = self.spaces[fp[0]]
                for lo, hi in fp[1]:
                    st["lw"][:, lo:hi] = -1
                    st["lr"][:, lo:hi] = -1
                    st["lw"][ei, lo:hi] = seq
                    st["dw"][lo:hi] = -1
        self.ops.append(dict(eng=eng, fns=fns, waits=waits, dwaits=dwaits, dma=dma, id=oid, seq=seq))
        return oid

    def emit(self, final_wait_ops=()):
        nc = self.nc
        ops = self.ops
        by_eng = {e: [o for o in ops if o["eng"] == e] for e in ENGS}
        dma_cnt = {e: 0 for e in ENGS}
        last_slot = {}
        for o in ops:
            if o["dma"]:
                k = dma_cnt[o["eng"]]
                dma_cnt[o["eng"]] += 1
                o["slot"] = k % N_DMA_SLOTS
                o["sval"] = 16 * (k // N_DMA_SLOTS + 1)
                key = (o["eng"], o["slot"])
                if key in last_slot:
                    o["dwaits"].add(last_slot[key])
                last_slot[key] = o["id"]
        signal = {e: set() for e in ENGS}
        for e in ENGS:
            waited = {}
            dwaited = {}
            for o in by_eng[e]:
                w2 = {}
                for e_i, s in o["waits"].items():
                    if waited.get(e_i, -1) >= s:
                        continue
                    w2[e_i] = s
                    waited[e_i] = s
                    signal[ENGS[e_i]].add(s)
                o["waits2"] = w2
                d2 = []
                for d in sorted(o["dwaits"]):
                    od = ops[d]
                    key = (od["eng"], od["slot"])
                    if dwaited.get(key, 0) >= od["sval"]:
                        continue
                    dwaited[key] = od["sval"]
                    d2.append(d)
                o["dwaits2"] = d2
        sig_sorted = {e: sorted(signal[e]) for e in ENGS}
        sig_rank = {e: {s: i for i, s in enumerate(sig_sorted[e])} for e in ENGS}
        sems = {}
        for e in ENGS:
            nsem = len(sig_sorted[e]) // SEM_WRAP + 1
            sems[e] = [nc.alloc_semaphore(f"s_{e}_{i}") for i in range(nsem)]
        dsems = {}
        for e in ENGS:
            if dma_cnt[e]:
                dsems[e] = [nc.alloc_semaphore(f"d_{e}_{i}") for i in range(min(N_DMA_SLOTS, dma_cnt[e]))]

        def csig(e, s):
            r = sig_rank[e][s]
            return sems[e][r // SEM_WRAP], r % SEM_WRAP + 1

        self.n_waits = 0
        self.n_sig = sum(len(v) for v in sig_sorted.values())
        self.n_ins = 0
        finals = [(dsems[ops[d]["eng"]][ops[d]["slot"]], ops[d]["sval"]) for d in final_wait_ops]
        with nc.Block() as block:
            hw = {"pe": block.tensor, "act": block.scalar, "dve": block.vector,
                  "pool": block.gpsimd, "sp": block.sync}

            def make(e):
                def body(eng):
                    for o in by_eng[e]:
                        for e_i, s in o["waits2"].items():
                            sem, val = csig(ENGS[e_i], s)
                            eng.wait_ge(sem, val)
                            self.n_waits += 1
                        for d in o["dwaits2"]:
                            od = ops[d]
                            eng.wait_ge(dsems[od["eng"]][od["slot"]], od["sval"])
                            self.n_waits += 1
                        ins = None
                        for fn in o["fns"]:
                            ins = fn(eng)
                            self.n_ins += 1
                        if o["dma"]:
                            ins.then_inc(dsems[e][o["slot"]], 16)
                        elif o["seq"] in sig_rank[e]:
                            sem, val = csig(e, o["seq"])
                            ins.then_inc(sem, 1)
                    if e == "sp":
                        for sem, val in finals:
                            eng.wait_ge(sem, val)
                return body

            for e in ENGS:
                if by_eng[e] or (e == "sp" and finals):
                    hw[e](make(e))


D = 1024
NIT = 20
LN_EPS = 1e-5
RMS_EPS = 1e-5
ALPHA = 2.0 ** 0.25
LAM_INIT = 0.8 - 0.6 * math.exp(0.0)
NEG = -30000.0
C_AQ, C_AK, C_AV, C_BQ, C_BKV, C_IQ, C_IK, C_IW, C_G = 0, 512, 1024, 1536, 2048, 2176, 2688, 2752, 2760


def _slopes():
    n = 12
    s = (2.0 ** (-8.0 * np.arange(1, n + 1) / n)).astype(np.float32).astype(np.float64)
    a_idx = np.arange(4) * 3
    b_idx = np.setdiff1d(np.arange(n), a_idx)
    return np.concatenate([s[a_idx], s[b_idx]])


def _bf16_split3(v):
    hi = v.astype(ml_dtypes.bfloat16).astype(np.float64)
    r = v - hi
    mid = r.astype(ml_dtypes.bfloat16).astype(np.float64)
    lo = (r - mid).astype(ml_dtypes.bfloat16).astype(np.float64)
    return np.stack([hi, mid, lo], 0)


def _hl(v):
    hi = v.astype(ml_dtypes.bfloat16).astype(np.float64)
    lo = (v - hi).astype(ml_dtypes.bfloat16).astype(np.float64)
    return np.stack([hi, lo], 1).astype(np.float32)


def host_constants():
    sl = _slopes()
    qrel = np.arange(512, dtype=np.float64)
    qaug = np.stack([_bf16_split3(-8.0 * sl[h] * qrel) for h in range(12)], 1)
    p = np.arange(128, dtype=np.float64)
    dp = np.arange(-3, 13, dtype=np.float64)
    kbias = sl[None, :, None] * (p[:, None, None] - 128.0 * dp[None, None, :])
    qq = np.arange(128)
    vis = (p[:, None].astype(int) // 64) <= (qq[None, :] // 64)
    dist = np.abs(qq[None, :] - p[:, None])
    dbias = np.where(vis[:, None, :], -sl[None, :, None] * dist[:, None, :], NEG)
    idxmask = np.where((p[None, :].astype(int) // 64) <= (qq[:, None] // 64), 0.0, -1e30)
    pow2 = np.tile((2.0 ** -(np.arange(NIT + 1) + 1.0))[None, :], (128, 1))
    return dict(
        ident=np.eye(128, dtype=np.float32),
        qaug=qaug.astype(np.float32),
        kbias=kbias.astype(np.float32),
        dbias=_hl(dbias * 8.0),
        idxmask=idxmask.astype(np.float32),
        pow2=pow2.astype(np.float32),
    )


def build(NB=2, S=2048, dbg=(), stop=None):
    nc = bass.Bass("TRN2", target_bir_lowering=False)
    P = Prog(nc)
    NT, NG, NBLK = S // 128, S // 512, S // 1024
    TOPK = min(256, S // 4)
    dumps = {}

    def din(name, shape):
        return nc.dram_tensor(name, list(shape), F32, kind="ExternalInput").ap()

    x_d = din("x", [NB * S, D])
    cT_d = din("cT", [128, 8, NB])
    adaw_d = din("ada_w", [D, 6 * D])
    adabT_d = din("ada_bT", [128, 48])
    win_d = din("w_in", [D, 4808])
    wik2_d = din("w_ik2", [D, 128])
    lamA_d = din("lamA", [128, 2, 64])
    lamB_d = din("lamB", [128, 2, 64])
    asub_d = din("asub", [128, 1])
    kvg_d = din("kvg_bc", [128, 128])
    wukT_d = din("w_ukTz", [128, 8, 128])
    wuv_d = din("w_uvz", [128, 8, 128])
    wap_d = din("w_a_proj", [512, D])
    wbp_d = din("w_b_proj", [512, D])
    wo_d = din("w_o", [D, D])
    lnbc_d = din("ln_bc", [128, 4, D])
    wrt_d = din("w_router", [D, 20])
    brt_d = din("b_router_bc", [128, 20])
    weg_d = din("w_exp_gate", [16, D, 256])
    weu_d = din("w_exp_up", [16, D, 256])
    wed_d = din("w_exp_down", [16, 256, D])
    ident_d = din("ident", [128, 128])
    qaug_d = din("qaug", [3, 12, 512])
    kbias_d = din("kbias", [128, 12, 16])
    dbias_d = din("dbias", [128, 2, 12, 128])
    idxmask_d = din("idxmask", [128, 128])
    pow2_d = din("pow2", [128, NIT + 1])
    y_d = nc.dram_tensor("y", [NB * S, D], F32, kind="ExternalOutput").ap()

    identf = P.sb("identf", [128, 128], F32)
    identb = P.sb("identb", [128, 128], BF16)
    onesb = P.sb("onesb", [128, 128], BF16)
    onesf = P.sb("onesf", [128, 128], F32)
    qaug = P.sb("qaug_sb", [128, 12, 512], BF16)
    augl = P.sb("augl", [128, 128], BF16)
    kbias = P.sb("kbias_sb", [128, 12, 16], F32)
    dbias = P.sb("dbias_sb", [128, 2, 12, 128], BF16)
    idxmask = P.sb("idxmask_sb", [128, 128], F32)
    pow2 = P.sb("pow2_sb", [128, NIT + 1], F32)
    modT = P.sb("modT", [128, 48, NB], F32)
    lamneg = P.sb("lamneg", [128, 1], F32)
    gsc = P.sb("gsc", [128, 1], F32)
    kvg = P.sb("kvg", [128, 128], F32)
    brt = P.sb("brt", [128, 20], F32)
    wukT = P.sb("wukT", [128, 8, 128], BF16)
    wuv = P.sb("wuv", [128, 8, 128], BF16)
    wrt = P.sb("wrt", [128, 8, 20], F32)
    small = P.sb("small", [128, 64], F32)
    epsc = P.sb("epsc", [128, 2], F32)
    R_K = (P.sb_top + 63) // 64 * 64
    aKT = P.sb("aKT", [128, 4, S], BF16)
    aV = P.sb("aV", [128, NT, 512], BF16)
    cKV = P.sb("cKV", [128, NT, 128], BF16)
    cKVT = P.sb("cKVT", [128, S], BF16)
    iKT = P.sb("iKT", [128, S], BF16)
    P.sb_top = max(P.sb_top, R_K + 8 * D * 4 + 2 * D * 4 + 128)
    yAT = P.sb("yAT", [128, 4, S], BF16)
    yBT = P.sb("yBT", [128, 4, S], BF16)
    R_W = (P.sb_top + 63) // 64 * 64

    def region(start):
        P.sb_top = start

    region(R_W)
    cT_sb = P.sb("cT_sb", [128, 8, NB], F32)
    condT = P.sb("condT", [128, 8, NB], BF16)
    adabT = P.sb("adabT", [128, 48], F32)
    lamA = P.sb("lamA_sb", [128, 2, 64], F32)
    lamB = P.sb("lamB_sb", [128, 2, 64], F32)
    asub = P.sb("asub_sb", [128, 1], F32)
    adaw = [P.sb(f"adaw{i}", [128, 8, 1536], BF16) for i in range(2)]
    region(R_W)
    xin = P.sb("xin", [128, 4, D], F32)
    uT = P.sb("uT", [128, 8, 512], BF16)
    R_12 = P.sb_top
    wK = P.sb("wK", [128, 8, 1280], BF16)
    sq1 = P.sb("sq1", [128, 512], F32)
    t1 = P.sb("t1", [128, 4, 128], F32)
    region(R_12)
    xin2, uT2 = xin, uT
    xin_off = P.reg[xin.name][1]
    score = P.sb("score", [128, S], F32, offset=xin_off)
    score1 = P.sb("score1", [128, S], F32, offset=P.reg[uT.name][1])
    assert S * 4 <= 8 * 512 * 2
    junk = P.sb("junk", [128, S], BF16, offset=xin_off + S * 4)
    maskQ = P.sb("maskQ", [128, S], BF16, offset=xin_off + S * 6)
    assert S * 8 <= 4 * D * 4
    wQ = P.sb("wQ", [128, 8, 1544], BF16)
    aQT = P.sb("aQT", [128, 4, 2, 512], BF16)
    ov1 = P.sb_top
    bQT = P.sb("bQT", [128, 4, 512], BF16)
    o01 = [P.sb(f"o01_{i}", [128, 512], F32, offset=(ov1 + 63) // 64 * 64 + i * 2048) for i in range(2)]
    iQT = P.sb("iQT", [128, 8, 512], BF16)
    qabsT = P.sb("qabsT", [128, 8, 512], BF16)
    iW = P.sb("iW", [128, 4, 8], F32)
    maskT = P.sb("maskT", [128, NT, 512], BF16)
    ov3 = P.sb_top
    rl = [P.sb(f"rl{i}", [128, 512], F32) for i in range(2)]
    ov3b = (ov3 + 63) // 64 * 64
    PTm = [P.sb(f"PTm{i}", [128, 512], BF16, offset=ov3b + i * 1024) for i in range(2)]
    olT = [P.sb(f"olT{i}", [128, 512], BF16, offset=ov3b + 2048 + i * 1024) for i in range(2)]
    dtmp = [P.sb(f"dtmp{i}", [128, 128], F32) for i in range(2)]
    rinv = P.sb("rinv", [128, 512], F32)
    wtab = P.sb("wtab", [128, NIT + 1], F32)
    bis = P.sb("bis", [128, 8], F32)
    PT = [P.sb(f"PT{i}", [128, 512], BF16) for i in range(4)]
    p2_end = P.sb_top
    region(R_K)
    x1blk = P.sb("x1blk", [128, 8, D], F32)
    lnv = P.sb("lnv", [128, 2, D], F32)
    assert P.sb_top <= P.reg[yAT.name][1]
    region(R_W)
    u2T = P.sb("u2T", [128, 8, 1024], BF16)
    comb = P.sb("comb", [128, 8, 16], F32)
    gatebc = P.sb("gatebc", [128, 2, D], F32)
    mixedT = P.sb("mixedT", [128, 8, 1024], BF16)
    R_34 = P.sb_top
    uT3 = P.sb("uT3", [128, 8, 1024], BF16)
    xin3 = P.sb("xin3", [128, 4, D], F32)
    wap = P.sb("wap", [128, 4, D], BF16)
    wbp = P.sb("wbp", [128, 4, D], BF16)
    wgt = [P.sb(f"wgt{i}", [128, 8, 2, 128], BF16) for i in range(2)]
    g01 = [P.sb(f"g01_{i}", [128, 512], BF16) for i in range(4)]
    tt01 = [P.sb(f"tt01_{i}", [128, 512], F32) for i in range(2)]
    p3a_end = P.sb_top
    region(R_34)
    wo = P.sb("wo", [128, 8, D], BF16)
    NSLOT = 3
    xrow = [P.sb(f"xrow{i}", [128, D], F32) for i in range(NSLOT)]
    r1 = [P.sb(f"r1_{i}", [128, D], F32) for i in range(NSLOT)]
    u2fs = [P.sb(f"u2f{i}", [128, 8, 128], F32) for i in range(NSLOT)]
    rtrs = [P.sb(f"rtr{i}", [128, 96], F32) for i in range(NSLOT)]
    dg = [P.sb(f"dg{i}", [128, 128], F32) for i in range(2)]
    p3_end = max(P.sb_top, p3a_end)
    region(P.reg[mixedT.name][1])
    yacc = P.sb("yacc", [128, 8, D], F32)
    wg = [P.sb(f"wg{i}", [128, 8, 256], BF16) for i in range(2)]
    wu = [P.sb(f"wu{i}", [128, 8, 256], BF16) for i in range(2)]
    wd = [P.sb(f"wd{i}", [128, 2, D], BF16) for i in range(2)]
    hT = [P.sb(f"hT{i}", [128, 2, 1024], BF16) for i in range(2)]
    sg = [P.sb(f"sg{i}", [128, 512], BF16) for i in range(2)]
    r2 = [P.sb("r2_0", [128, D], F32)]
    otile = [P.sb(f"otile{i}", [128, D], F32) for i in range(2)]
    p4_end = P.sb_top
    mem_report = dict(R_K=R_K, R_W=R_W, p2_end=p2_end, p3_end=p3_end, p4_end=p4_end, limit=P.sb_limit)

    pf = [P.ps(f"pf{i}", [128, 512], F32) for i in range(8)]
    pb = pf[7].bitcast(BF16)
    rr = {"i": 0}

    def bank():
        rr["i"] = (rr["i"] + 1) % 8
        return pf[rr["i"]]

    ev = {"i": 0}

    def evac(out, in_, scale=None):
        ev["i"] += 1
        if ev["i"] % 2 == 0:
            P.op("act", lambda e: e.activation(out=out, in_=in_, func=AF.Copy), ins=[in_], outs=[out])
        else:
            P.op("dve", lambda e: e.tensor_copy(out=out, in_=in_), ins=[in_], outs=[out])

    def dump(name, ap, shape):
        if name not in dbg:
            return
        t = nc.dram_tensor("dbg_" + name, list(shape), ap.dtype, kind="ExternalOutput").ap()
        dumps[name] = P.op("sp", lambda e: e.dma_start(out=t, in_=ap), ins=[ap], dma=True)

    def dma(eng, out, in_):
        if eng == "pool":
            return P.op(eng, lambda e: e.dma_start(out=out, in_=in_, max_dma_last_dim=8192), ins=[in_], outs=[out],
                        dma=True)
        return P.op(eng, lambda e: e.dma_start(out=out, in_=in_), ins=[in_], outs=[out], dma=True)

    class _Stop(Exception):
        pass

    hits = {}

    def checkpoint(name):
        hits[name] = hits.get(name, -1) + 1
        if stop == name or stop == f"{name}@{hits[name]}":
            raise _Stop()

    def wview(w_ap, c0, c1):
        return w_ap[:, c0:c1].rearrange("(k p) f -> p k f", p=128)

    try:
        dma("sp", identf[:], ident_d)
        dma("pool", identb[:], ident_d)
        P.op("pool", lambda e: e.memset(qaug[:], 0.0), outs=[qaug[:]])
        dma("pool", qaug[0:3, :, :], qaug_d)
        P.op("pool", lambda e: e.memset(augl[:], 0.0), outs=[augl[:]])
        P.op("pool", lambda e: e.memset(augl[0:3, :], 1.0), outs=[augl[0:3, :]])
        dma("sp", kbias[:], kbias_d)
        dma("pool", dbias[:], dbias_d)
        dma("sp", idxmask[:], idxmask_d)
        dma("sp", pow2[:], pow2_d)
        dma("sp", kvg[:], kvg_d)
        dma("sp", brt[:], brt_d)
        dma("pool", wukT[:], wukT_d)
        dma("pool", wuv[:], wuv_d)
        dma("sp", wrt[:], wrt_d.rearrange("(k p) f -> p k f", p=128))
        P.op("pool", lambda e: e.memset(onesb[:], 1.0), outs=[onesb[:]])
        P.op("pool", lambda e: e.memset(onesf[:], 1.0), outs=[onesf[:]])
        P.op("pool", lambda e: e.memset(epsc[:], RMS_EPS), outs=[epsc[:]])

        dma("sp", cT_sb[:], cT_d)
        dma("sp", adabT[:], adabT_d)
        dma("sp", lamA[:], lamA_d)
        dma("sp", lamB[:], lamB_d)
        dma("sp", asub[:], asub_d)
        P.op("act", lambda e: e.activation(out=condT[:], in_=cT_sb[:], func=AF.Silu), ins=[cT_sb[:]], outs=[condT[:]])
        for pc in range(4):
            aw = adaw[pc % 2]
            dma("pool", aw[:], wview(adaw_d, pc * 1536, (pc + 1) * 1536))
            bk = bank()
            fns = []
            for jj in range(12):
                for k in range(8):
                    fns.append(lambda e, jj=jj, k=k, aw=aw, bk=bk: e.matmul(
                        bk[:, jj * NB:(jj + 1) * NB], aw[:, k, jj * 128:(jj + 1) * 128], condT[:, k, :],
                        start=(k == 0), stop=(k == 7)))
            P.op("pe", fns, ins=[aw[:], condT[:]], outs=[bk[:, 0:12 * NB]])
            j0 = pc * 12
            P.op("dve", lambda e, bk=bk, j0=j0: e.tensor_tensor(
                out=modT[:, j0:j0 + 12, :], in0=bk[:, 0:12 * NB].rearrange("p (j b) -> p j b", b=NB),
                in1=adabT[:, j0:j0 + 12].unsqueeze(2).to_broadcast([128, 12, NB]), op=ALU.add),
                ins=[bk[:, 0:12 * NB], adabT[:]], outs=[modT[:, j0:j0 + 12, :]])
        for (a, b_) in ((8, 16), (32, 40)):
            P.op("dve", lambda e, a=a, b_=b_: e.tensor_scalar(out=modT[:, a:b_, :], in0=modT[:, a:b_, :], scalar1=1.0,
                                                             scalar2=None, op0=ALU.add),
                 ins=[modT[:, a:b_, :]], outs=[modT[:, a:b_, :]])
        dump("modT", modT[:], [128, 48, NB])
        checkpoint("p0")
        lp = small[:, 0:2]
        P.op("dve", lambda e: e.tensor_tensor(out=lamA[:], in0=lamA[:], in1=lamB[:], op=ALU.mult),
             ins=[lamA[:], lamB[:]], outs=[lamA[:]])
        P.op("dve", lambda e: e.tensor_reduce(out=lp, in_=lamA[:], axis=AX.X, op=ALU.add), ins=[lamA[:]], outs=[lp])
        P.op("act", lambda e: e.activation(out=small[:, 2:4], in_=lp, func=AF.Exp), ins=[lp], outs=[small[:, 2:4]])
        P.op("dve", lambda e: e.tensor_tensor(out=small[:, 4:5], in0=small[:, 3:4], in1=small[:, 2:3], op=ALU.subtract),
             ins=[small[:, 2:4]], outs=[small[:, 4:5]])
        P.op("dve", lambda e: e.tensor_scalar(out=lamneg[:], in0=small[:, 4:5], scalar1=-LAM_INIT, scalar2=None, op0=ALU.add),
             ins=[small[:, 4:5]], outs=[lamneg[:]])
        P.op("dve", lambda e: e.tensor_scalar(out=gsc[:], in0=asub[:], scalar1=1.0 - LAM_INIT, scalar2=None, op0=ALU.mult),
             ins=[asub[:]], outs=[gsc[:]])

        def load_uT(b, g, xt, ut, ucol0=0):
            r0 = b * S + g * 512
            dma("sp", xt[:], x_d[r0:r0 + 512, :].rearrange("(t p) d -> p t d", p=128))
            for k in range(8):
                bk = bank()
                P.op("pe", [lambda e, tt=tt, k=k, bk=bk: e.transpose(bk[:, tt * 128:(tt + 1) * 128],
                                                                  xt[:, tt, k * 128:(k + 1) * 128], identf[:])
                            for tt in range(4)], ins=[xt[:, :, k * 128:(k + 1) * 128], identf[:]], outs=[bk[:]])
                P.op("act", lambda e, k=k, bk=bk: e.activation(
                    out=ut[:, k, ucol0:ucol0 + 512], in_=bk[:], func=AF.Identity,
                    bias=modT[:, k, b:b + 1], scale=modT[:, 8 + k, b:b + 1]),
                    ins=[bk[:], modT[:]], outs=[ut[:, k, ucol0:ucol0 + 512]])

        def proj_fm(w, c0, ut, out_ap, ucol0=0, act=None):
            bk = bank()
            P.op("pe", [lambda e, k=k, bk=bk: e.matmul(bk[:], w[:, k, c0:c0 + 128], ut[:, k, ucol0:ucol0 + 512],
                                                    start=(k == 0), stop=(k == 7)) for k in range(8)],
                 ins=[w[:, :, c0:c0 + 128], ut[:, :, ucol0:ucol0 + 512]], outs=[bk[:]])
            if act is None:
                evac(out_ap, bk[:])
            else:
                P.op("act", lambda e: e.activation(out=out_ap, in_=bk[:], func=act), ins=[bk[:]], outs=[out_ap])

        def proj_fm_split(w, c0, ut, out_lo, out_hi):
            bk = bank()
            P.op("pe", [lambda e, k=k, bk=bk: e.matmul(bk[:], w[:, k, c0:c0 + 128], ut[:, k, 0:512],
                                                    start=(k == 0), stop=(k == 7)) for k in range(8)],
                 ins=[w[:, :, c0:c0 + 128], ut[:, :, 0:512]], outs=[bk[:]])
            P.op("act", lambda e: e.activation(out=out_lo, in_=bk[0:64, :], func=AF.Copy), ins=[bk[0:64, :]], outs=[out_lo])
            P.op("dve", lambda e: e.tensor_copy(out=out_hi, in_=bk[64:128, :]), ins=[bk[64:128, :]], outs=[out_hi])

        for b in range(NB):
            dma("pool", wK[:, :, 0:512], wview(win_d, C_AK, C_AK + 512))
            dma("pool", wK[:, :, 512:1024], wview(win_d, C_AV, C_AV + 512))
            dma("pool", wK[:, :, 1024:1152], wview(win_d, C_BKV, C_BKV + 128))
            dma("pool", wK[:, :, 1152:1280], wik2_d.rearrange("(k p) f -> p k f", p=128))
            for g in range(NG):
                load_uT(b, g, xin, uT)
                for h in range(4):
                    proj_fm(wK, h * 128, uT, aKT[:, h, g * 512:(g + 1) * 512])
                proj_fm(wK, 1152, uT, iKT[:, g * 512:(g + 1) * 512])
                for tt in range(4):
                    bk = bank()
                    P.op("pe", [lambda e, k=k, bk=bk, tt=tt: e.matmul(bk[:], uT[:, k, tt * 128:(tt + 1) * 128],
                                                                  wK[:, k, 512:1024], start=(k == 0), stop=(k == 7))
                                for k in range(8)], ins=[uT[:], wK[:, :, 512:1024]], outs=[bk[:]])
                    evac(aV[:, g * 4 + tt, :], bk[:])
                bk = bank()
                fns = []
                for tt in range(4):
                    for k in range(8):
                        fns.append(lambda e, k=k, bk=bk, tt=tt: e.matmul(
                            bk[:, tt * 128:(tt + 1) * 128], uT[:, k, tt * 128:(tt + 1) * 128], wK[:, k, 1024:1152],
                            start=(k == 0), stop=(k == 7)))
                P.op("pe", fns, ins=[uT[:], wK[:, :, 1024:1152]], outs=[bk[:]])
                P.op("act", lambda e, bk=bk: e.activation(out=sq1[:], in_=bk[:], func=AF.Square), ins=[bk[:]], outs=[sq1[:]])
                ssq = small[:, 8:12]
                P.op("dve", lambda e: e.tensor_reduce(out=ssq, in_=sq1[:].rearrange("p (t r) -> p t r", t=4), axis=AX.X,
                                                      op=ALU.add), ins=[sq1[:]], outs=[ssq])
                P.op("dve", lambda e: e.tensor_scalar(out=ssq, in0=ssq, scalar1=1.0 / 128.0, scalar2=RMS_EPS,
                                                      op0=ALU.mult, op1=ALU.add), ins=[ssq], outs=[ssq])
                P.op("act", lambda e: e.activation(out=ssq, in_=ssq, func=AF.Sqrt), ins=[ssq], outs=[ssq])
                P.op("dve", lambda e: e.reciprocal(out=ssq, in_=ssq), ins=[ssq], outs=[ssq])
                P.op("dve", lambda e, bk=bk: e.tensor_tensor(
                    out=t1[:], in0=bk[:].rearrange("p (t r) -> p t r", t=4),
                    in1=ssq.unsqueeze(2).to_broadcast([128, 4, 128]), op=ALU.mult),
                    ins=[bk[:], ssq], outs=[t1[:]])
                P.op("pool", lambda e, g=g: e.tensor_tensor(
                    out=cKV[:, g * 4:(g + 1) * 4, :], in0=t1[:],
                    in1=kvg[:].unsqueeze(1).to_broadcast([128, 4, 128]), op=ALU.mult),
                    ins=[t1[:], kvg[:]], outs=[cKV[:, g * 4:(g + 1) * 4, :]])
                pbh = pb[:, (g % 2) * 512:(g % 2) * 512 + 512]
                P.op("pe", [lambda e, tt=tt, g=g, pbh=pbh: e.transpose(pbh[:, tt * 128:(tt + 1) * 128],
                                                                   cKV[:, g * 4 + tt, :], identb[:])
                            for tt in range(4)], ins=[cKV[:, g * 4:(g + 1) * 4, :], identb[:]], outs=[pbh])
                evac(cKVT[:, g * 512:(g + 1) * 512], pbh)
            dump(f"aKT{b}", aKT[:], [128, 4, S])
            dump(f"aV{b}", aV[:], [128, NT, 512])
            dump(f"cKV{b}", cKV[:], [128, NT, 128])
            dump(f"cKVT{b}", cKVT[:], [128, S])
            dump(f"iKT{b}", iKT[:], [128, S])
            checkpoint("p1")

            dma("pool", wQ[:, :, 0:512], wview(win_d, C_AQ, C_AQ + 512))
            dma("pool", wQ[:, :, 512:1024], wview(win_d, C_BQ, C_BQ + 512))
            dma("pool", wQ[:, :, 1024:1536], wview(win_d, C_IQ, C_IQ + 512))
            dma("pool", wQ[:, :, 1536:1544], wview(win_d, C_IW, C_IW + 8))
            P.op("pool", lambda e: e.memset(aQT[:], 0.0), outs=[aQT[:]])
            P.op("pool", lambda e: e.memset(iQT[:], 0.0), outs=[iQT[:]])
            for g in range(NG):
                T0 = 4 * g
                load_uT(b, g, xin2, uT2)
                for h in range(4):
                    proj_fm_split(wQ, h * 128, uT2, aQT[0:64, h, 0, :], aQT[64:128, h, 1, :])
                    proj_fm(wQ, 512 + h * 128, uT2, bQT[:, h, :])
                    proj_fm_split(wQ, 1024 + h * 128, uT2, iQT[0:64, 2 * h, :], iQT[64:128, 2 * h + 1, :])
                bk = bank()
                fns = []
                for tt in range(4):
                    for k in range(8):
                        fns.append(lambda e, k=k, bk=bk, tt=tt: e.matmul(
                            bk[:, tt * 8:(tt + 1) * 8], uT2[:, k, tt * 128:(tt + 1) * 128], wQ[:, k, 1536:1544],
                            start=(k == 0), stop=(k == 7)))
                P.op("pe", fns, ins=[uT2[:], wQ[:, :, 1536:1544]], outs=[bk[:, 0:32]])
                P.op("dve", lambda e, bk=bk: e.tensor_copy(out=iW[:].rearrange("p t h -> p (t h)"), in_=bk[:, 0:32]),
                     ins=[bk[:, 0:32]], outs=[iW[:]])
                for h in range(8):
                    bk = bank()
                    P.op("pe", lambda e, bk=bk, h=h: e.matmul(bk[:], wukT[:, h, :], bQT[:, h // 2, :],
                                                           start=True, stop=True),
                         ins=[wukT[:, h, :], bQT[:, h // 2, :]], outs=[bk[:]])
                    evac(qabsT[:, h, :], bk[:])
                if b == 0 and g == NG - 1:
                    dump("qabsT", qabsT[:], [128, 8, 512])
                    dump("iW", iW[:], [128, 4, 8])
                    checkpoint("p2a")
                mx, mn, rg, thr, cnt, ee, lo = [bis[:, i:i + 1] for i in range(7)]

                def index_gen(T0=T0, b=b, g=g):
                    scoreB = [score, score1]

                    def l_units(qt):
                        T = T0 + qt
                        nk = (T + 1) * 128
                        scb = scoreB[qt % 2]
                        units = []
                        for kc in range((nk + 511) // 512):
                            ncol = min(512, nk - kc * 512)
                            for h in range(8):
                                def unit(kc=kc, ncol=ncol, h=h, qt=qt, scb=scb):
                                    bk = pf[6 + h % 2]
                                    P.op("pe", lambda e: e.matmul(
                                        bk[:, 0:ncol], iQT[:, h, qt * 128:(qt + 1) * 128],
                                        iKT[:, kc * 512:kc * 512 + ncol], start=True, stop=True),
                                        ins=[iQT[:, h, qt * 128:(qt + 1) * 128], iKT[:, kc * 512:kc * 512 + ncol]],
                                        outs=[bk[:, 0:ncol]])
                                    r = rl[h % 2]
                                    P.op("act", lambda e: e.activation(out=r[:, 0:ncol], in_=bk[:, 0:ncol], func=AF.Relu),
                                         ins=[bk[:, 0:ncol]], outs=[r[:, 0:ncol]])
                                    sc = scb[:, kc * 512:kc * 512 + ncol]
                                    if qt == 0:
                                        if h == 0:
                                            P.op("dve", lambda e: e.tensor_scalar(
                                                out=sc, in0=r[:, 0:ncol], scalar1=iW[:, qt, 0:1], scalar2=None, op0=ALU.mult),
                                                ins=[r[:, 0:ncol], iW[:]], outs=[sc])
                                        else:
                                            P.op("dve", lambda e: e.scalar_tensor_tensor(
                                                out=sc, in0=r[:, 0:ncol], scalar=iW[:, qt, h:h + 1], in1=sc,
                                                op0=ALU.mult, op1=ALU.add), ins=[r[:, 0:ncol], iW[:], sc], outs=[sc])
                                    elif h == 0:
                                        P.op("act", lambda e: e.activation(out=sc, in_=r[:, 0:ncol], func=AF.Identity,
                                                                           scale=iW[:, qt, 0:1]),
                                             ins=[r[:, 0:ncol], iW[:]], outs=[sc])
                                    else:
                                        P.op("act", lambda e: e.activation(out=r[:, 0:ncol], in_=r[:, 0:ncol], func=AF.Identity,
                                                                           scale=iW[:, qt, h:h + 1]),
                                             ins=[r[:, 0:ncol], iW[:]], outs=[r[:, 0:ncol]])
                                        P.op("pool", lambda e: e.tensor_tensor(out=sc, in0=sc, in1=r[:, 0:ncol], op=ALU.add),
                                             ins=[sc, r[:, 0:ncol]], outs=[sc])
                                units.append(unit)
                        return units

                    def bz_steps(qt):
                        T = T0 + qt
                        nk = (T + 1) * 128
                        sv = scoreB[qt % 2][:, 0:nk]
                        sd = scoreB[qt % 2][:, T * 128:(T + 1) * 128]
                        steps = []

                        def setup():
                            if T >= 2:
                                P.op("dve", lambda e: e.tensor_reduce(out=mx, in_=sv, axis=AX.X, op=ALU.max), ins=[sv], outs=[mx])
                                P.op("dve", lambda e: e.tensor_reduce(out=mn, in_=sv, axis=AX.X, op=ALU.min), ins=[sv], outs=[mn])
                            P.op("dve", lambda e: e.tensor_tensor(out=sd, in0=sd, in1=idxmask[:], op=ALU.add),
                                 ins=[sd, idxmask[:]], outs=[sd])
                            if T >= 2:
                                P.op("dve", lambda e: e.tensor_tensor(out=rg, in0=mx, in1=mn, op=ALU.subtract), ins=[mx, mn], outs=[rg])
                                P.op("dve", lambda e: e.tensor_scalar(out=wtab[:], in0=pow2[:], scalar1=rg, scalar2=None,
                                                                      op0=ALU.mult), ins=[pow2[:], rg], outs=[wtab[:]])
                                P.op("dve", lambda e: e.tensor_tensor(out=thr, in0=mn, in1=wtab[:, 0:1], op=ALU.add),
                                     ins=[mn, wtab[:, 0:1]], outs=[thr])
                        steps.append(setup)
                        if T >= 2:
                            for it in range(NIT):
                                def iteration(it=it):
                                    P.op("dve", lambda e: e.tensor_scalar(
                                        out=junk[:, 0:nk], in0=sv, scalar1=thr, scalar2=None, op0=ALU.is_ge, op1=ALU.add,
                                        accum_out=cnt), ins=[sv, thr], outs=[junk[:, 0:nk], cnt])
                                    P.op("dve", lambda e: e.tensor_scalar(out=ee, in0=cnt, scalar1=TOPK - 0.5, scalar2=0.5,
                                                                          op0=ALU.is_ge, op1=ALU.subtract), ins=[cnt], outs=[ee])
                                    P.op("dve", lambda e: e.scalar_tensor_tensor(out=thr, in0=ee, scalar=wtab[:, it:it + 1], in1=thr,
                                                                                 op0=ALU.mult, op1=ALU.add),
                                         ins=[ee, wtab[:, it:it + 1], thr], outs=[thr])
                                steps.append(iteration)

                        def final():
                            if T >= 2:
                                P.op("dve", lambda e: e.scalar_tensor_tensor(out=lo, in0=wtab[:, NIT:NIT + 1], scalar=-1.5, in1=thr,
                                                                             op0=ALU.mult, op1=ALU.add),
                                     ins=[wtab[:, NIT:NIT + 1], thr], outs=[lo])
                                P.op("dve", lambda e: e.tensor_scalar(out=maskQ[:, 0:nk], in0=sv, scalar1=lo, scalar2=NEG,
                                                                      op0=ALU.is_lt, op1=ALU.mult), ins=[sv, lo],
                                     outs=[maskQ[:, 0:nk]])
                            else:
                                P.op("dve", lambda e: e.tensor_scalar(out=maskQ[:, 0:nk], in0=sv, scalar1=-1e29, scalar2=NEG,
                                                                      op0=ALU.is_lt, op1=ALU.mult), ins=[sv],
                                     outs=[maskQ[:, 0:nk]])
                            for kb0 in range(0, T + 1, 4):
                                nb = min(4, T + 1 - kb0)
                                half = (kb0 // 4) % 2
                                pbh = pb[:, half * 512:half * 512 + nb * 128]
                                P.op("pe", [lambda e, i=i, kb0=kb0, pbh=pbh: e.transpose(
                                    pbh[:, i * 128:(i + 1) * 128], maskQ[:, (kb0 + i) * 128:(kb0 + i + 1) * 128], identb[:])
                                    for i in range(nb)], ins=[maskQ[:, kb0 * 128:(kb0 + nb) * 128], identb[:]], outs=[pbh])
                                mo = maskT[:, kb0:kb0 + nb, qt * 128:(qt + 1) * 128]
                                P.op("act", lambda e, mo=mo, pbh=pbh, nb=nb: e.activation(
                                    out=mo, in_=pbh.rearrange("p (n q) -> p n q", n=nb), func=AF.Copy), ins=[pbh], outs=[mo])
                        steps.append(final)
                        return steps

                    for u in l_units(0):
                        u()
                        yield
                    for qt in range(4):
                        nxt = l_units(qt + 1) if qt < 3 else []
                        steps = bz_steps(qt)
                        nu = 0
                        for i, st_ in enumerate(steps[:-1]):
                            st_()
                            den = max(1, (6 * (len(steps) - 1)) // 10)
                            want = (len(nxt) * (i + 1) + den - 1) // den
                            while nu < min(want, len(nxt)):
                                nxt[nu]()
                                nu += 1
                            yield
                        while nu < len(nxt):
                            nxt[nu]()
                            nu += 1
                            yield
                        steps[-1]()
                        yield

                blocks = [("off", kb, 0) for kb in range(T0)] + [("diag", T0 + j, j) for j in range(4)]
                nblk = len(blocks)
                gcol = slice(g * 512, (g + 1) * 512)
                passes = []
                for h in range(4):
                    for m in range(2):
                        passes.append(dict(kind="A", h=h, m=m, hh=h,
                                           qk_lhsT=lambda kb, h=h: aKT[:, h, kb * 128:(kb + 1) * 128],
                                           qk_rhs=lambda c0, h=h, m=m: aQT[:, h, m, c0:512],
                                           pv_lhsT=lambda kb, h=h: aV[:, kb, h * 128:(h + 1) * 128],
                                           masked=False))
                for h in range(8):
                    passes.append(dict(kind="B", h=h, hh=4 + h,
                                       qk_lhsT=lambda kb: cKVT[:, kb * 128:(kb + 1) * 128],
                                       qk_rhs=lambda c0, h=h: qabsT[:, h, c0:512],
                                       pv_lhsT=lambda kb: cKV[:, kb, :],
                                       masked=True))
                items = [(pi, bi) for pi in range(len(passes)) for bi in range(nblk)]
                NI = len(items)

                def geom(n):
                    pi, bi = items[n]
                    kind, kb, j = blocks[bi]
                    c0 = j * 128 if kind == "diag" else 0
                    a0 = c0 + 128 if kind == "diag" else 0
                    return pi, bi, passes[pi], kind, kb, j, c0, a0

                NA = 8 * nblk
                SB4 = [pf[0], pf[1], pf[6], pf[7]]

                def sbank(n):
                    return pf[n % 2] if n < NA else SB4[n % 4]

                def st_qk(n):
                    pi, bi, ps_, kind, kb, j, c0, a0 = geom(n)
                    sb_ = sbank(n)
                    hh = ps_["hh"]
                    kl, qr = ps_["qk_lhsT"](kb), ps_["qk_rhs"](c0)
                    msk = ps_["masked"]
                    dg_ = (kind == "diag")
                    more = msk or dg_
                    fns = [lambda e: e.matmul(sb_[:, c0:512], kl, qr, start=True, stop=(a0 >= 512 and not more))]
                    ins = [kl, qr]
                    if a0 < 512:
                        fns.append(lambda e: e.matmul(sb_[:, a0:512], augl[:], qaug[:, hh, a0:512], start=False, stop=not more))
                        ins += [augl[:], qaug[:, hh, a0:512]]
                    if dg_:
                        fns.append(lambda e: e.matmul(sb_[:, c0:c0 + 128], identb[:], dbias[:, 0, hh, :], start=False, stop=False))
                        fns.append(lambda e: e.matmul(sb_[:, c0:c0 + 128], identb[:], dbias[:, 1, hh, :], start=False,
                                                      stop=not msk))
                        ins += [identb[:], dbias[:, 0, hh, :], dbias[:, 1, hh, :]]
                    if msk:
                        mt_ = maskT[:, kb, c0:512]
                        fns.append(lambda e: e.matmul(sb_[:, c0:512], identb[:], mt_, start=False, stop=True))
                        ins += [identb[:], mt_]
                    P.op("pe", fns, ins=ins, outs=[sb_[:, c0:512]])

                def st_sm(n):
                    pi, bi, ps_, kind, kb, j, c0, a0 = geom(n)
                    sb_ = sbank(n)
                    hh = ps_["hh"]
                    pt = PT[n % 4]
                    if kind == "diag":
                        P.op("act", lambda e: e.activation(out=pt[:, c0:c0 + 128], in_=sb_[:, c0:c0 + 128], func=AF.Exp,
                                                           scale=0.125),
                             ins=[sb_[:, c0:c0 + 128]], outs=[pt[:, c0:c0 + 128]])
                    if a0 < 512:
                        dpi = (T0 - kb) + 3
                        P.op("act", lambda e: e.activation(
                            out=pt[:, a0:512], in_=sb_[:, a0:512], func=AF.Exp, scale=0.125,
                            bias=kbias[:, hh, dpi:dpi + 1]),
                            ins=[sb_[:, a0:512], kbias[:, hh, dpi:dpi + 1]], outs=[pt[:, a0:512]])

                def st_pv(n):
                    pi, bi, ps_, kind, kb, j, c0, a0 = geom(n)
                    src = PT[n % 4]
                    OA, RS = pf[2 + pi % 2], pf[4 + pi % 2]
                    first, last = (bi == 0), (bi == nblk - 1)
                    vl = ps_["pv_lhsT"](kb)
                    P.op("pe", [lambda e: e.matmul(OA[:, c0:512], vl, src[:, c0:512], start=first, stop=last),
                                lambda e: e.matmul(RS[:, c0:512], onesb[:], src[:, c0:512], start=first, stop=last)],
                         ins=[vl, src[:, c0:512], onesb[:]], outs=[OA[:, c0:512], RS[:, c0:512]])

                pending = []

                def fin_a(pi, n):
                    ps_ = passes[pi]
                    OA, RS = pf[2 + pi % 2], pf[4 + pi % 2]
                    P.op("act", lambda e: e.activation(out=rinv[:], in_=RS[:], func=AF.Ln), ins=[RS[:]], outs=[rinv[:]])
                    P.op("act", lambda e: e.activation(out=rinv[:], in_=rinv[:], func=AF.Exp, scale=-1.0),
                         ins=[rinv[:]], outs=[rinv[:]])
                    if ps_["kind"] == "A":
                        h, m = ps_["h"], ps_["m"]
                        om = o01[m]
                        od = o01[0]
                        dfr = 2 if nblk >= 8 else 0
                        sqb = o01[1] if dfr else rinv

                        def fin_dve():
                            P.op("dve", lambda e: e.tensor_tensor(out=om[:], in0=OA[:], in1=rinv[:], op=ALU.mult),
                                 ins=[OA[:], rinv[:]], outs=[om[:]])
                            if m == 1:
                                P.op("dve", lambda e: e.scalar_tensor_tensor(out=od[:], in0=o01[1][:], scalar=lamneg[:, 0:1],
                                                                             in1=od[:], op0=ALU.mult, op1=ALU.add),
                                     ins=[o01[1][:], lamneg[:], od[:]], outs=[od[:]])
                                P.op("act", lambda e: e.activation(out=sqb[:], in_=od[:], func=AF.Square),
                                     ins=[od[:]], outs=[sqb[:]])
                        if dfr:
                            pending.append((n + dfr, fin_dve))
                        else:
                            fin_dve()
                        if m == 1:
                            def fin_pe():
                                P.op("pe", lambda e: e.matmul(RS[:], onesf[:], sqb[:], start=True, stop=True),
                                     ins=[onesf[:], sqb[:]], outs=[RS[:]])

                            def fin_b():
                                P.op("act", lambda e: e.activation(out=sqb[:], in_=RS[:], func=AF.Ln, scale=1.0 / 128.0,
                                                                   bias=epsc[:, 0:1]),
                                     ins=[RS[:], epsc[:]], outs=[sqb[:]])
                                P.op("act", lambda e: e.activation(out=sqb[:], in_=sqb[:], func=AF.Exp, scale=-0.5),
                                     ins=[sqb[:]], outs=[sqb[:]])
                                yo = yAT[:, h, gcol]
                                P.op("dve", lambda e: e.scalar_tensor_tensor(out=yo, in0=od[:], scalar=gsc[:, 0:1], in1=sqb[:],
                                                                             op0=ALU.mult, op1=ALU.mult),
                                     ins=[od[:], gsc[:], sqb[:]], outs=[yo])
                            pending.append((n + 2 + dfr, fin_pe))
                            pending.append((n + 3 + dfr, fin_b))
                    else:
                        h = ps_["h"]
                        ot = olT[h % 2]
                        P.op("dve", lambda e: e.tensor_tensor(out=ot[:], in0=OA[:], in1=rinv[:], op=ALU.mult),
                             ins=[OA[:], rinv[:]], outs=[ot[:]])

                        def fin_pe():
                            P.op("pe", lambda e: e.matmul(RS[:], wuv[:, h, :], ot[:], start=True, stop=True),
                                 ins=[wuv[:, h, :], ot[:]], outs=[RS[:]])
                        pending.append((n + 2, fin_pe))

                        def fin_b():
                            p0 = (h % 2) * 64
                            yo = yBT[p0:p0 + 64, h // 2, gcol]
                            src_ = RS[p0:p0 + 64, :]
                            if h % 2 == 0:
                                P.op("act", lambda e: e.activation(out=yo, in_=src_, func=AF.Copy), ins=[src_], outs=[yo])
                            else:
                                P.op("dve", lambda e: e.tensor_copy(out=yo, in_=src_), ins=[src_], outs=[yo])
                        pending.append((n + 3, fin_b))

                def attn_gen():
                    st_qk(0)
                    nq = 1
                    for n in range(NI):
                        tgt = min(NI - 1, n + (3 if n >= NA else 1))
                        while nq <= tgt:
                            st_qk(nq)
                            nq += 1
                        st_sm(n)
                        st_pv(n)
                        if items[n][1] == nblk - 1:
                            fin_a(items[n][0], n)
                        while pending and pending[0][0] <= n:
                            pending.pop(0)[1]()
                        yield
                    while pending:
                        pending.pop(0)[1]()

                ig, ag = index_gen(), attn_gen()
                NY = 8 * (((T0 + 1) * 128 + 511) // 512) + sum((NIT + 2) if T0 + qt >= 2 else 2 for qt in range(4))
                ia = iy = 0
                for _ in ig:
                    iy += 1
                    while ia < NA - 1 and ia * NY < iy * NA:
                        next(ag)
                        ia += 1
                while ia < NA:
                    next(ag)
                    ia += 1
                for _ in ag:
                    pass
            dump(f"yAT{b}", yAT[:], [128, 4, S])
            dump(f"yBT{b}", yBT[:], [128, 4, S])
            checkpoint("p2")

            for blk in range(NBLK):
                dma("pool", wap[:], wap_d.rearrange("(k p) f -> p k f", p=128))
                dma("pool", wbp[:], wbp_d.rearrange("(k p) f -> p k f", p=128))
                for gl in range(2):
                    load_uT(b, blk * 2 + gl, xin3, uT3, ucol0=gl * 512)
                tok0 = blk * 1024
                def load_wgt(j):
                    wj = wgt[j % 2]
                    dma("pool", wj[:, :, 0, :], wview(win_d, C_G + j * 128, C_G + (j + 1) * 128))
                    dma("pool", wj[:, :, 1, :], wview(win_d, C_G + 1024 + j * 128, C_G + 1024 + (j + 1) * 128))

                load_wgt(0)
                load_wgt(1)
                for j in range(8):
                    wj = wgt[j % 2]
                    for gl in range(2):
                        gs = []
                        for gi in range(2):
                            bk = bank()
                            P.op("pe", [lambda e, k=k, bk=bk, wj=wj, gi=gi, gl=gl: e.matmul(
                                bk[:], wj[:, k, gi, :], uT3[:, k, gl * 512:(gl + 1) * 512], start=(k == 0), stop=(k == 7))
                                for k in range(8)], ins=[wj[:, :, gi, :], uT3[:, :, gl * 512:(gl + 1) * 512]], outs=[bk[:]])
                            gt = g01[(gl * 2 + gi) % 4]
                            P.op("act", lambda e, gt=gt, bk=bk: e.activation(out=gt[:], in_=bk[:], func=AF.Sigmoid),
                                 ins=[bk[:]], outs=[gt[:]])
                            gs.append(gt)
                        ts_ = []
                        for gi, (wp, yT) in enumerate(((wap, yAT), (wbp, yBT))):
                            bk = bank()
                            c_lo = tok0 + gl * 512
                            P.op("pe", [lambda e, c=c, bk=bk, wp=wp, yT=yT, c_lo=c_lo, j=j: e.matmul(
                                bk[:], wp[:, c, j * 128:(j + 1) * 128], yT[:, c, c_lo:c_lo + 512], start=(c == 0), stop=(c == 3))
                                for c in range(4)], ins=[wp[:, :, j * 128:(j + 1) * 128], yT[:, :, c_lo:c_lo + 512]],
                                outs=[bk[:]])
                            tt_ = tt01[gi]
                            P.op("dve", lambda e, tt_=tt_, bk=bk, gt=gs[gi]: e.tensor_tensor(out=tt_[:], in0=bk[:], in1=gt[:],
                                                                                          op=ALU.mult),
                                 ins=[bk[:], gs[gi][:]], outs=[tt_[:]])
                            ts_.append(tt_)
                        mo = mixedT[:, j, gl * 512:(gl + 1) * 512]
                        P.op("pool", lambda e, mo=mo, a=ts_[0], c=ts_[1]: e.tensor_tensor(out=mo, in0=a[:], in1=c[:], op=ALU.add),
                             ins=[ts_[0][:], ts_[1][:]], outs=[mo])
                    if j + 2 < 8:
                        load_wgt(j + 2)
                if b == 0 and blk == 0:
                    dump("mixedT", mixedT[:], [128, 8, 1024])
                dma("pool", wo[:], wo_d.rearrange("(k p) f -> p k f", p=128))
                dma("sp", lnv[:], lnbc_d[:, 0:2, :])
                for gi, ch0 in enumerate((16, 40)):
                    for half in range(2):
                        bk = bank()
                        for jj in range(4):
                            j = half * 4 + jj
                            dgt = dg[jj % 2]
                            P.op("dve", lambda e, dgt=dgt, j=j, ch0=ch0: e.tensor_scalar(
                                out=dgt[:], in0=identf[:], scalar1=modT[:, ch0 + j, b:b + 1], scalar2=None, op0=ALU.mult),
                                ins=[identf[:], modT[:]], outs=[dgt[:]])
                            P.op("pe", lambda e, dgt=dgt, bk=bk, jj=jj: e.matmul(bk[:, jj * 128:(jj + 1) * 128], onesf[:], dgt[:],
                                                                              start=True, stop=True),
                                 ins=[onesf[:], dgt[:]], outs=[bk[:, jj * 128:(jj + 1) * 128]])
                        evac(gatebc[:, gi, half * 512:(half + 1) * 512], bk[:])
                if blk == 0:
                    dump(f"gatebc{b}", gatebc[:], [128, 2, D])
                def layer_norm(src, gidx, out_ap, scratch, st):
                    s1, s2, mean, msq, var = st[0], st[1], st[2], st[3], st[4]
                    P.op("act", lambda e: e.activation(out=scratch, in_=src, func=AF.Copy, accum_out=s1),
                         ins=[src], outs=[scratch, s1])
                    P.op("act", lambda e: e.activation(out=scratch, in_=src, func=AF.Square, accum_out=s2),
                         ins=[src], outs=[scratch, s2])
                    yield
                    P.op("dve", lambda e: e.tensor_scalar(out=mean, in0=s1, scalar1=1.0 / D, scalar2=None, op0=ALU.mult),
                         ins=[s1], outs=[mean])
                    P.op("dve", lambda e: e.tensor_tensor(out=msq, in0=mean, in1=mean, op=ALU.mult), ins=[mean], outs=[msq])
                    P.op("dve", lambda e: e.scalar_tensor_tensor(out=var, in0=s2, scalar=1.0 / D, in1=msq, op0=ALU.mult,
                                                                 op1=ALU.subtract), ins=[s2, msq], outs=[var])
                    P.op("dve", lambda e: e.tensor_scalar(out=var, in0=var, scalar1=LN_EPS, scalar2=None, op0=ALU.add),
                         ins=[var], outs=[var])
                    P.op("act", lambda e: e.activation(out=var, in_=var, func=AF.Sqrt), ins=[var], outs=[var])
                    yield
                    P.op("dve", lambda e: e.reciprocal(out=var, in_=var), ins=[var], outs=[var])
                    P.op("dve", lambda e: e.tensor_scalar(out=src, in0=src, scalar1=mean, scalar2=var, op0=ALU.subtract,
                                                          op1=ALU.mult), ins=[src, mean, var], outs=[src])
                    P.op("dve", lambda e: e.tensor_tensor(out=src, in0=src, in1=lnv[:, 0, :], op=ALU.mult),
                         ins=[src, lnv[:, 0, :]], outs=[src])
                    P.op("pool", lambda e: e.tensor_tensor(out=out_ap, in0=src, in1=lnv[:, 1, :], op=ALU.add),
                         ins=[src, lnv[:, 1, :]], outs=[out_ap])
                    yield

                def tile_gen(t, b=b, tok0=tok0):
                    slot = t % NSLOT
                    row0 = b * S + tok0 + t * 128
                    xr = xrow[slot]
                    rt_ = r1[slot]
                    u2f = u2fs[slot]
                    rtr = rtrs[slot]
                    st = [small[:, 16 + 8 * slot + i:17 + 8 * slot + i] for i in range(8)]
                    dma("sp", xr[:], x_d[row0:row0 + 128, :])
                    for half in range(2):
                        bk = bank()
                        cs = slice(half * 512, (half + 1) * 512)
                        P.op("pe", [lambda e, k=k, bk=bk, t=t, cs=cs: e.matmul(
                            bk[:], mixedT[:, k, t * 128:(t + 1) * 128], wo[:, k, cs], start=(k == 0), stop=(k == 7))
                            for k in range(8)], ins=[mixedT[:, :, t * 128:(t + 1) * 128], wo[:, :, cs]], outs=[bk[:]])
                        P.op("dve", lambda e, bk=bk, rt_=rt_, cs=cs: e.tensor_tensor(out=rt_[:, cs], in0=bk[:],
                                                                                  in1=gatebc[:, 0, cs], op=ALU.mult),
                             ins=[bk[:], gatebc[:, 0, cs]], outs=[rt_[:, cs]])
                    P.op("dve", lambda e, xr=xr, rt_=rt_: e.scalar_tensor_tensor(out=rt_[:], in0=xr[:], scalar=ALPHA, in1=rt_[:],
                                                                                op0=ALU.mult, op1=ALU.add),
                         ins=[xr[:], rt_[:]], outs=[rt_[:]])
                    yield
                    yield from layer_norm(rt_[:], 0, x1blk[:, t, :], xr[:], st)
                    for kh in range(2):
                        bk = bank()
                        P.op("pe", [lambda e, kk=kk, bk=bk, t=t, kh=kh: e.transpose(
                            bk[:, kk * 128:(kk + 1) * 128], x1blk[:, t, (kh * 4 + kk) * 128:(kh * 4 + kk + 1) * 128], identf[:])
                            for kk in range(4)], ins=[x1blk[:, t, kh * 512:(kh + 1) * 512], identf[:]], outs=[bk[:]])
                        for kk in range(4):
                            k = kh * 4 + kk
                            P.op("act", lambda e, bk=bk, kk=kk, k=k: e.activation(
                                out=u2f[:, k, :], in_=bk[:, kk * 128:(kk + 1) * 128], func=AF.Identity,
                                bias=modT[:, 24 + k, b:b + 1], scale=modT[:, 32 + k, b:b + 1]),
                                ins=[bk[:, kk * 128:(kk + 1) * 128], modT[:]], outs=[u2f[:, k, :]])
                    P.op("pool", lambda e, t=t: e.tensor_copy(out=u2T[:, :, t * 128:(t + 1) * 128], in_=u2f[:]),
                         ins=[u2f[:]], outs=[u2T[:, :, t * 128:(t + 1) * 128]])
                    bk = bank()
                    P.op("pe", [lambda e, k=k, bk=bk: e.matmul(bk[:, 0:20], u2f[:, k, :], wrt[:, k, :], start=(k == 0),
                                                              stop=(k == 7)) for k in range(8)],
                         ins=[u2f[:], wrt[:]], outs=[bk[:, 0:20]])
                    yield
                    lg = rtr[:, 0:20]
                    GL, EL = rtr[:, 0:4], rtr[:, 4:20]
                    gmax, ngmax, gsum, gtop = rtr[:, 20:21], rtr[:, 21:22], rtr[:, 22:23], rtr[:, 23:24]
                    ohg, gexp = rtr[:, 24:28], rtr[:, 28:32]
                    t44, els, oh1, els2, oh2 = rtr[:, 32:48], rtr[:, 48:52], rtr[:, 52:56], rtr[:, 56:60], rtr[:, 60:64]
                    m1, m2, dd, ex, w1, w2 = [rtr[:, 64 + i:65 + i] for i in range(6)]
                    we = rtr[:, 72:76]
                    P.op("dve", lambda e, bk=bk: e.tensor_tensor(out=lg, in0=bk[:, 0:20], in1=brt[:], op=ALU.add),
                         ins=[bk[:, 0:20], brt[:]], outs=[lg])
                    P.op("dve", lambda e: e.tensor_reduce(out=gmax, in_=GL, axis=AX.X, op=ALU.max), ins=[GL], outs=[gmax])
                    P.op("dve", lambda e: e.tensor_scalar(out=ohg, in0=GL, scalar1=gmax, scalar2=None, op0=ALU.is_ge),
                         ins=[GL, gmax], outs=[ohg])
                    P.op("dve", lambda e: e.tensor_scalar(out=ngmax, in0=gmax, scalar1=-1.0, scalar2=None, op0=ALU.mult),
                         ins=[gmax], outs=[ngmax])
                    P.op("act", lambda e: e.activation(out=gexp, in_=GL, func=AF.Exp, bias=ngmax, scale=1.0, accum_out=gsum),
                         ins=[GL, ngmax], outs=[gexp, gsum])
                    yield
                    P.op("dve", lambda e: e.reciprocal(out=gtop, in_=gsum), ins=[gsum], outs=[gtop])
                    P.op("dve", lambda e: e.tensor_tensor(
                        out=t44.rearrange("p (g e) -> p g e", g=4), in0=EL.rearrange("p (g e) -> p g e", g=4),
                        in1=ohg.unsqueeze(2).to_broadcast([128, 4, 4]), op=ALU.mult), ins=[EL, ohg], outs=[t44])
                    P.op("dve", lambda e: e.tensor_reduce(out=els, in_=t44.rearrange("p (g e) -> p e g", g=4), axis=AX.X,
                                                          op=ALU.add), ins=[t44], outs=[els])
                    P.op("dve", lambda e: e.tensor_reduce(out=m1, in_=els, axis=AX.X, op=ALU.max), ins=[els], outs=[m1])
                    P.op("dve", lambda e: e.tensor_scalar(out=oh1, in0=els, scalar1=m1, scalar2=None, op0=ALU.is_ge),
                         ins=[els, m1], outs=[oh1])
                    P.op("dve", lambda e: e.scalar_tensor_tensor(out=els2, in0=oh1, scalar=-1e30, in1=els, op0=ALU.mult,
                                                                 op1=ALU.add), ins=[oh1, els], outs=[els2])
                    P.op("dve", lambda e: e.tensor_reduce(out=m2, in_=els2, axis=AX.X, op=ALU.max), ins=[els2], outs=[m2])
                    P.op("dve", lambda e: e.tensor_scalar(out=oh2, in0=els2, scalar1=m2, scalar2=None, op0=ALU.is_ge),
                         ins=[els2, m2], outs=[oh2])
                    P.op("dve", lambda e: e.tensor_tensor(out=dd, in0=m2, in1=m1, op=ALU.subtract), ins=[m2, m1], outs=[dd])
                    P.op("act", lambda e: e.activation(out=ex, in_=dd, func=AF.Exp), ins=[dd], outs=[ex])
                    P.op("dve", lambda e: e.tensor_scalar(out=w1, in0=ex, scalar1=1.0, scalar2=None, op0=ALU.add),
                         ins=[ex], outs=[w1])
                    P.op("dve", lambda e: e.reciprocal(out=w1, in_=w1), ins=[w1], outs=[w1])
                    P.op("dve", lambda e: e.tensor_tensor(out=w2, in0=ex, in1=w1, op=ALU.mult), ins=[ex, w1], outs=[w2])
                    P.op("dve", lambda e: e.tensor_scalar(out=we, in0=oh1, scalar1=w1, scalar2=None, op0=ALU.mult),
                         ins=[oh1, w1], outs=[we])
                    P.op("dve", lambda e: e.scalar_tensor_tensor(out=we, in0=oh2, scalar=w2, in1=we, op0=ALU.mult, op1=ALU.add),
                         ins=[oh2, w2, we], outs=[we])
                    P.op("dve", lambda e: e.tensor_scalar(out=we, in0=we, scalar1=gtop, scalar2=None, op0=ALU.mult),
                         ins=[we, gtop], outs=[we])
                    P.op("dve", lambda e, t=t: e.tensor_tensor(
                        out=comb[:, t, :].rearrange("p (g e) -> p g e", g=4),
                        in0=ohg.unsqueeze(2).to_broadcast([128, 4, 4]),
                        in1=we.unsqueeze(1).to_broadcast([128, 4, 4]), op=ALU.mult), ins=[ohg, we], outs=[comb[:, t, :]])
                    yield

                tgens = [tile_gen(t) for t in range(8)]
                active = []
                nstart = 0
                while nstart < 8 or active:
                    if nstart < 8 and len(active) < NSLOT:
                        active.append(tgens[nstart])
                        nstart += 1
                    for tg in list(active):
                        try:
                            next(tg)
                        except StopIteration:
                            active.remove(tg)
                if b == 0 and blk == 0:
                    dump("x1blk", x1blk[:], [128, 8, D])
                    dump("comb", comb[:], [128, 8, 16])
                    dump("u2T", u2T[:], [128, 8, 1024])
                    checkpoint("p3")

                def moe_gu(ex_):
                    wg_, wu_, wd_ = wg[ex_ % 2], wu[ex_ % 2], wd[ex_ % 2]
                    dma("pool", wg_[:], weg_d[ex_].rearrange("(k p) f -> p k f", p=128))
                    dma("pool", wu_[:], weu_d[ex_].rearrange("(k p) f -> p k f", p=128))
                    dma("pool", wd_[:], wed_d[ex_].rearrange("(k p) f -> p k f", p=128))
                    hT_ = hT[ex_ % 2]
                    for gl in range(2):
                        for fh in range(2):
                            bg, bu = bank(), bank()
                            for (bk, w_) in ((bg, wg_), (bu, wu_)):
                                P.op("pe", [lambda e, k=k, bk=bk, w_=w_, fh=fh, gl=gl: e.matmul(
                                    bk[:], w_[:, k, fh * 128:(fh + 1) * 128], u2T[:, k, gl * 512:(gl + 1) * 512],
                                    start=(k == 0), stop=(k == 7)) for k in range(8)],
                                    ins=[w_[:, :, fh * 128:(fh + 1) * 128], u2T[:, :, gl * 512:(gl + 1) * 512]], outs=[bk[:]])
                            s_ = sg[(gl * 2 + fh) % 2]
                            P.op("act", lambda e, s_=s_, bg=bg: e.activation(out=s_[:], in_=bg[:], func=AF.Silu),
                                 ins=[bg[:]], outs=[s_[:]])
                            ho = hT_[:, fh, gl * 512:(gl + 1) * 512]
                            P.op("dve", lambda e, ho=ho, s_=s_, bu=bu: e.tensor_tensor(out=ho, in0=bu[:], in1=s_[:], op=ALU.mult),
                                 ins=[bu[:], s_[:]], outs=[ho])

                def moe_dn(ex_):
                    wd_, hT_ = wd[ex_ % 2], hT[ex_ % 2]
                    for t in range(8):
                        for half in range(2):
                            bk = bank()
                            cs = slice(half * 512, (half + 1) * 512)
                            P.op("pe", [lambda e, fh=fh, bk=bk, t=t, cs=cs: e.matmul(
                                bk[:], hT_[:, fh, t * 128:(t + 1) * 128], wd_[:, fh, cs], start=(fh == 0), stop=(fh == 1))
                                for fh in range(2)], ins=[hT_[:, :, t * 128:(t + 1) * 128], wd_[:, :, cs]], outs=[bk[:]])
                            ya = yacc[:, t, cs]
                            if ex_ == 0:
                                P.op("dve", lambda e, ya=ya, bk=bk, t=t: e.tensor_scalar(
                                    out=ya, in0=bk[:], scalar1=comb[:, t, 0:1], scalar2=None, op0=ALU.mult),
                                    ins=[bk[:], comb[:, t, 0:1]], outs=[ya])
                            else:
                                P.op("dve", lambda e, ya=ya, bk=bk, t=t, ex_=ex_: e.scalar_tensor_tensor(
                                    out=ya, in0=bk[:], scalar=comb[:, t, ex_:ex_ + 1], in1=ya, op0=ALU.mult, op1=ALU.add),
                                    ins=[bk[:], comb[:, t, ex_:ex_ + 1], ya], outs=[ya])

                moe_gu(0)
                for ex_ in range(16):
                    if ex_ + 1 < 16:
                        moe_gu(ex_ + 1)
                    moe_dn(ex_)
                if b == 0 and blk == 0:
                    dump("yacc", yacc[:], [128, 8, D])
                dma("sp", lnv[:], lnbc_d[:, 2:4, :])
                def ln2_gen(t, b=b, tok0=tok0):
                    row0 = b * S + tok0 + t * 128
                    r_ = yacc[:, t, :]
                    ot = otile[t % 2]
                    st2 = [small[:, 40 + 8 * (t % 2) + i:41 + 8 * (t % 2) + i] for i in range(8)]
                    P.op("dve", lambda e: e.tensor_tensor(out=r_, in0=r_, in1=gatebc[:, 1, :], op=ALU.mult),
                         ins=[r_, gatebc[:, 1, :]], outs=[r_])
                    P.op("dve", lambda e: e.scalar_tensor_tensor(out=r_, in0=x1blk[:, t, :], scalar=ALPHA,
                                                                 in1=r_, op0=ALU.mult, op1=ALU.add),
                         ins=[x1blk[:, t, :], r_], outs=[r_])
                    yield
                    yield from layer_norm(r_, 2, ot[:], ot[:], st2)
                    fin = dma("sp", y_d[row0:row0 + 128, :], ot[:])
                    finals.append(fin)

                lgens = [ln2_gen(t) for t in range(8)]
                active = []
                nstart = 0
                while nstart < 8 or active:
                    if nstart < 8 and len(active) < 2:
                        active.append(lgens[nstart])
                        nstart += 1
                    for tg in list(active):
                        try:
                            next(tg)
                        except StopIteration:
                            active.remove(tg)
    except _Stop:
        pass
    P.emit(final_wait_ops=finals + list(dumps.values()))
    return nc, P, mem_report


finals = []


def prep_inputs(inputs, core, NB=2, S=2048):
    f = lambda a: np.ascontiguousarray(np.asarray(a, dtype=np.float32))
    b0 = core * NB
    x = f(inputs["x"])[b0:b0 + NB, :S].reshape(NB * S, D)
    c = f(inputs["c"])[b0:b0 + NB]
    cT = f(c.T.reshape(8, 128, NB).transpose(1, 0, 2))
    ada_b = f(inputs["ada_b"])[0]
    rep = lambda v: f(np.broadcast_to(np.asarray(v, np.float32)[None, :], (128, len(v))))
    w_in = f(inputs["w_in"])[0]
    w_uk = f(inputs["w_uk"])[0]
    w_uv = f(inputs["w_uv"])[0]
    w_ukTz = np.zeros((128, 8, 128), np.float32)
    w_uvz = np.zeros((128, 8, 128), np.float32)
    for h in range(8):
        w_ukTz[(h % 2) * 64:(h % 2) * 64 + 64, h, :] = w_uk[:, h, :].T
        w_uvz[:, h, (h % 2) * 64:(h % 2) * 64 + 64] = w_uv[:, h, :]
    d = dict(
        x=x, cT=cT, ada_w=f(inputs["ada_w"])[0], ada_bT=f(ada_b.reshape(48, 128).T),
        w_in=w_in, w_ik2=f(np.concatenate([w_in[:, C_IK:C_IK + 64], w_in[:, C_IK:C_IK + 64]], 1)),
        lamA=f(np.stack([rep(f(inputs["lambda_q1"])[0]), rep(f(inputs["lambda_q2"])[0])], 1)),
        lamB=f(np.stack([rep(f(inputs["lambda_k1"])[0]), rep(f(inputs["lambda_k2"])[0])], 1)),
        asub=f(f(inputs["a_subln_g"])[0].reshape(128, 1)),
        kvg_bc=rep(f(inputs["kv_norm_g"])[0]),
        w_ukTz=w_ukTz, w_uvz=w_uvz,
        w_a_proj=f(inputs["w_a_proj"])[0], w_b_proj=f(inputs["w_b_proj"])[0], w_o=f(inputs["w_o"])[0],
        ln_bc=f(np.stack([rep(f(inputs["ln1_g"])[0]), rep(f(inputs["ln1_b"])[0]),
                          rep(f(inputs["ln2_g"])[0]), rep(f(inputs["ln2_b"])[0])], 1)),
        w_router=f(np.concatenate([f(inputs["w_group"])[0], f(inputs["w_expert_router"])[0]], 1)),
        b_router_bc=rep(np.concatenate([f(inputs["b_group"])[0], f(inputs["b_expert_router"])[0]])),
        w_exp_gate=f(inputs["w_exp_gate"])[0], w_exp_up=f(inputs["w_exp_up"])[0], w_exp_down=f(inputs["w_exp_down"])[0],
    )
    d.update(host_constants())
    return d


_CACHE = {}


def kernel(**inputs):
    NB, S = 2, 2048
    if "nc" not in _CACHE:
        finals.clear()
        _CACHE["nc"] = build(NB, S)[0]
    nc = _CACHE["nc"]
    in_maps = [prep_inputs(inputs, c, NB, S) for c in range(8)]
    res = run_bass_kernel_spmd(nc, in_maps, core_ids=list(range(8)))
    out = np.concatenate([np.asarray(r["y"], dtype=np.float32).reshape(NB, S, D) for r in res.results], axis=0)
    return out
```
